# Optimizing a Trainium2 kernel written in Bass

```python
import math
import jax
import jax.numpy as jnp
from jax import lax
import numpy as np


D_MODEL = 1024
BATCH = 8
SEQ = 4096
DEPTH = 2

HEAD_DIM = D_MODEL // 16
DIL_GROUPS = ((128, 1), (512, 4), (2048, 16))
DIL_HEADS_PER_GROUP = 4
DIL_HEADS = len(DIL_GROUPS) * DIL_HEADS_PER_GROUP
DIL_WIDTH = DIL_HEADS * HEAD_DIM
DIL_OUT = DIL_HEADS_PER_GROUP * HEAD_DIM
DIL_BLOCK = 128
SB_HEADS = 8
SB_WIDTH = SB_HEADS * HEAD_DIM
SB_BLOCK = 128
CONV_CH = D_MODEL // 2
CONV_WIDTH = 31
N_BRANCH = 3
IN_SPLITS = (3 * DIL_WIDTH, 3 * DIL_WIDTH + 3 * SB_WIDTH, 3 * DIL_WIDTH + 3 * SB_WIDTH + 2 * CONV_CH)
IN_COLS = IN_SPLITS[-1] + N_BRANCH * D_MODEL
D_FF = ((8 * D_MODEL // 3 + 127) // 128) * 128
N_EXPERTS = 8
TOP_K = 2
D_FF_EXPERT = D_FF
MOE_BLOCK = 256
N_DENSE = (DEPTH + 1) // 2
N_MOE = DEPTH // 2
EPS = 1e-6
ALIBI_MAX_BIAS = 8.0

kernel_name = 'hybrid_dilated_stickbreak_conformer_moe'


def rms_norm(x, g):
    xf = x.astype(jnp.float32)
    y = xf * lax.rsqrt(jnp.mean(xf * xf, axis=-1, keepdims=True) + EPS)
    return y.astype(x.dtype) * g


def layer_norm(x, g, b):
    xf = x.astype(jnp.float32)
    mu = jnp.mean(xf, axis=-1, keepdims=True)
    var = jnp.mean(jnp.square(xf - mu), axis=-1, keepdims=True)
    return ((xf - mu) * lax.rsqrt(var + EPS)).astype(x.dtype) * g + b


def alibi_slopes(n_heads):
    return 2.0 ** (-ALIBI_MAX_BIAS * jnp.arange(1, n_heads + 1, dtype=jnp.float32) / n_heads)


def dilated_window_attention(q, k, v, slopes, window, dilation):
    bsz, seq, nh, dh = q.shape
    reach = window // dilation
    span = DIL_BLOCK * dilation
    seq_pad = -(-seq // span) * span
    sub_len = seq_pad // dilation
    nb = sub_len // DIL_BLOCK

    def to_blocks(t):
        t = jnp.pad(t, ((0, 0), (0, seq_pad - seq), (0, 0), (0, 0)))
        t = t.reshape(bsz, sub_len, dilation, nh, dh).transpose(0, 2, 3, 1, 4)
        return t.reshape(bsz, dilation, nh, nb, DIL_BLOCK, dh)

    def with_prev(t):
        prev = jnp.concatenate([jnp.zeros_like(t[:, :, :, :1]), t[:, :, :, :-1]], axis=3)
        return jnp.concatenate([prev, t], axis=4)

    qb = to_blocks(q)
    kk = with_prev(to_blocks(k))
    vv = with_prev(to_blocks(v))
    s = jnp.einsum('brhnqc,brhnkc->brhnqk', qb, kk).astype(jnp.float32) / math.sqrt(dh)
    qi = jnp.arange(DIL_BLOCK)[:, None] + DIL_BLOCK
    ki = jnp.arange(2 * DIL_BLOCK)[None, :]
    dist = qi - ki
    first = (jnp.arange(nb) == 0)[:, None, None] & (ki < DIL_BLOCK)[None]
    valid = (dist >= 0) & (dist <= reach) & ~first
    bias = -slopes[:, None, None, None] * (dist * dilation).astype(jnp.float32)
    s = jnp.where(valid, s + bias, -jnp.inf)
    m = jnp.max(s, axis=-1, keepdims=True)
    p = jnp.exp(s - m)
    denom = jnp.sum(p, axis=-1, keepdims=True)
    o = jnp.einsum('brhnqk,brhnkc->brhnqc', (p / denom).astype(v.dtype), vv)
    lse = (m + jnp.log(denom))[..., 0]

    def unblock(t):
        t = t.reshape(bsz, dilation, nh, sub_len, *t.shape[5:])
        t = jnp.moveaxis(t, 3, 1)
        return t.reshape(bsz, seq_pad, nh, *t.shape[4:])[:, :seq]

    return unblock(o), unblock(lse)


def stick_breaking_attention(q, k, v):
    bsz, seq, nh, dh = q.shape
    n_blocks = seq // SB_BLOCK
    qb = q.reshape(bsz, n_blocks, SB_BLOCK, nh, dh).transpose(1, 0, 3, 2, 4)
    kt = k.transpose(0, 2, 1, 3)
    vt = v.transpose(0, 2, 1, 3)
    kpos = jnp.arange(seq)
    scale = 1.0 / math.sqrt(dh)

    def one_block(args):
        q_blk, blk = args
        z = jnp.einsum('bhqc,bhkc->bhqk', q_blk, kt).astype(jnp.float32) * scale
        qpos = blk * SB_BLOCK + jnp.arange(SB_BLOCK)
        before = kpos[None, :] < qpos[:, None]
        log_keep = jnp.where(before, jax.nn.log_sigmoid(-z), 0.0)
        log_later = lax.cumsum(log_keep, axis=3, reverse=True) - log_keep
        a = jnp.where(before, jnp.exp(jax.nn.log_sigmoid(z) + log_later), 0.0)
        return jnp.einsum('bhqk,bhkc->bhqc', a.astype(vt.dtype), vt)

    o = lax.map(one_block, (qb, jnp.arange(n_blocks)))
    return o.transpose(1, 0, 3, 2, 4).reshape(bsz, seq, nh * dh)


def conformer_conv(u_val, u_gate, conv_w, conv_b, norm_g, norm_b):
    u = u_val * jax.nn.sigmoid(u_gate)
    u = lax.conv_general_dilated(u, conv_w[:, None, :], window_strides=(1,),
                                 padding=[(CONV_WIDTH - 1, 0)],
                                 dimension_numbers=('NWC', 'WIO', 'NWC'),
                                 feature_group_count=CONV_CH) + conv_b
    return jax.nn.silu(layer_norm(u, norm_g, norm_b))


def hybrid_mixer(h, w_in, q_norm_g, k_norm_g, conv_w, conv_b, conv_norm_g, conv_norm_b,
                 w_branch_a, w_branch_b, w_branch_c, w_out):
    bsz, seq, _ = h.shape
    proj = jnp.einsum('bsd,de->bse', h, w_in)
    qkv_a, qkv_b, glu, gate_logits = jnp.split(proj, list(IN_SPLITS), axis=-1)

    qkv_a = qkv_a.reshape(bsz, seq, 3, DIL_HEADS, HEAD_DIM)
    qa = rms_norm(qkv_a[:, :, 0], q_norm_g)
    ka = rms_norm(qkv_a[:, :, 1], k_norm_g)
    va = qkv_a[:, :, 2]
    slopes = alibi_slopes(DIL_HEADS)
    outs, lses = [], []
    for g, (window, dilation) in enumerate(DIL_GROUPS):
        sl = slice(g * DIL_HEADS_PER_GROUP, (g + 1) * DIL_HEADS_PER_GROUP)
        o_g, lse_g = dilated_window_attention(qa[:, :, sl], ka[:, :, sl], va[:, :, sl],
                                              slopes[sl], window, dilation)
        outs.append(o_g)
        lses.append(lse_g)
    o_groups = jnp.stack(outs)
    w_groups = jax.nn.softmax(jnp.stack(lses), axis=0)
    o_a = jnp.einsum('gbsh,gbshc->bshc', w_groups.astype(o_groups.dtype), o_groups)
    o_a = o_a.reshape(bsz, seq, DIL_OUT)

    qkv_b = qkv_b.reshape(bsz, seq, 3, SB_HEADS, HEAD_DIM)
    o_b = stick_breaking_attention(qkv_b[:, :, 0], qkv_b[:, :, 1], qkv_b[:, :, 2])

    glu_val, glu_gate = jnp.split(glu, 2, axis=-1)
    o_c = conformer_conv(glu_val, glu_gate, conv_w, conv_b, conv_norm_g, conv_norm_b)

    gates = jax.nn.sigmoid(gate_logits.reshape(bsz, seq, N_BRANCH, D_MODEL))
    merged = (gates[:, :, 0] * jnp.einsum('bsc,cd->bsd', o_a, w_branch_a)
              + gates[:, :, 1] * jnp.einsum('bsc,cd->bsd', o_b, w_branch_b)
              + gates[:, :, 2] * jnp.einsum('bsc,cd->bsd', o_c, w_branch_c))
    return jnp.einsum('bsd,de->bse', merged, w_out)


def swiglu(h, w_gate, w_up, w_down):
    a = jnp.einsum('bsd,df->bsf', h, w_gate)
    u = jnp.einsum('bsd,df->bsf', h, w_up)
    return jnp.einsum('bsf,fd->bsd', jax.nn.silu(a) * u, w_down)


def routed_swiglu(h, w_router, b_router, w_exp_gate, w_exp_up, w_exp_down):
    bsz, seq, d = h.shape
    t = h.reshape(-1, d)
    n = t.shape[0]
    logits = jnp.einsum('nd,de->ne', t, w_router).astype(jnp.float32) + b_router
    top_logits, top_idx = lax.top_k(logits, TOP_K)
    gates = jax.nn.softmax(top_logits, axis=-1).astype(h.dtype)
    e_flat = top_idx.reshape(-1)
    g_flat = gates.reshape(-1)
    tok_flat = jnp.arange(n * TOP_K) // TOP_K
    order = jnp.argsort(e_flat)
    e_sorted, tok_sorted, g_sorted = e_flat[order], tok_flat[order], g_flat[order]
    counts = jnp.bincount(e_flat, length=N_EXPERTS)
    start = jnp.cumsum(counts) - counts
    padded = (counts + MOE_BLOCK - 1) // MOE_BLOCK * MOE_BLOCK
    pend = jnp.cumsum(padded)
    pstart = pend - padded
    dest = pstart[e_sorted] + (jnp.arange(n * TOP_K) - start[e_sorted])
    n_blocks = -(-(n * TOP_K) // MOE_BLOCK) + N_EXPERTS
    rows = n_blocks * MOE_BLOCK
    row_tok = jnp.zeros((rows,), jnp.int32).at[dest].set(tok_sorted.astype(jnp.int32))
    row_gate = jnp.zeros((rows,), h.dtype).at[dest].set(g_sorted)
    block_expert = jnp.clip(jnp.searchsorted(pend, jnp.arange(n_blocks) * MOE_BLOCK, side='right'),
                            0, N_EXPERTS - 1)
    xs = t[row_tok].reshape(n_blocks, MOE_BLOCK, d)

    def expert_block(args):
        xb, e = args
        a = xb @ w_exp_gate[e]
        u = xb @ w_exp_up[e]
        return (jax.nn.silu(a) * u) @ w_exp_down[e]

    ys = lax.map(expert_block, (xs, block_expert)).reshape(rows, d) * row_gate[:, None]
    out = jax.ops.segment_sum(ys, row_tok, num_segments=n)
    return out.reshape(bsz, seq, d)


def setup_inputs(seed: int = 0) -> dict:
    key = jax.random.key(seed)
    ks = jax.random.split(key, 22)
    L = DEPTH

    def nrm(k, shape, scale):
        return jax.random.normal(k, shape, jnp.float32) * scale

    def gain(k, shape):
        return 1.0 + 0.02 * jax.random.normal(k, shape, jnp.float32)

    return {
        'x': nrm(ks[0], (BATCH, SEQ, D_MODEL), 1.0),
        'attn_norm_g': gain(ks[1], (L, D_MODEL)),
        'w_in': nrm(ks[2], (L, D_MODEL, IN_COLS), D_MODEL ** -0.5),
        'q_norm_g': gain(ks[3], (L, HEAD_DIM)),
        'k_norm_g': gain(ks[4], (L, HEAD_DIM)),
        'conv_w': nrm(ks[5], (L, CONV_WIDTH, CONV_CH), CONV_WIDTH ** -0.5),
        'conv_b': nrm(ks[6], (L, CONV_CH), 0.02),
        'conv_norm_g': gain(ks[7], (L, CONV_CH)),
        'conv_norm_b': nrm(ks[8], (L, CONV_CH), 0.02),
        'w_branch_a': nrm(ks[9], (L, DIL_OUT, D_MODEL), DIL_OUT ** -0.5),
        'w_branch_b': nrm(ks[10], (L, SB_WIDTH, D_MODEL), SB_WIDTH ** -0.5),
        'w_branch_c': nrm(ks[11], (L, CONV_CH, D_MODEL), CONV_CH ** -0.5),
        'w_out': nrm(ks[12], (L, D_MODEL, D_MODEL), D_MODEL ** -0.5),
        'ffn_norm_g': gain(ks[13], (L, D_MODEL)),
        'w_ffn_gate': nrm(ks[14], (N_DENSE, D_MODEL, D_FF), D_MODEL ** -0.5),
        'w_ffn_up': nrm(ks[15], (N_DENSE, D_MODEL, D_FF), D_MODEL ** -0.5),
        'w_ffn_down': nrm(ks[16], (N_DENSE, D_FF, D_MODEL), D_FF ** -0.5),
        'w_router': nrm(ks[17], (N_MOE, D_MODEL, N_EXPERTS), D_MODEL ** -0.5),
        'b_router': nrm(ks[18], (N_MOE, N_EXPERTS), 0.01),
        'w_exp_gate': nrm(ks[19], (N_MOE, N_EXPERTS, D_MODEL, D_FF_EXPERT), D_MODEL ** -0.5),
        'w_exp_up': nrm(ks[20], (N_MOE, N_EXPERTS, D_MODEL, D_FF_EXPERT), D_MODEL ** -0.5),
        'w_exp_down': nrm(ks[21], (N_MOE, N_EXPERTS, D_FF_EXPERT, D_MODEL), D_FF_EXPERT ** -0.5),
    }


def reference(x, attn_norm_g, w_in, q_norm_g, k_norm_g, conv_w, conv_b, conv_norm_g, conv_norm_b,
              w_branch_a, w_branch_b, w_branch_c, w_out, ffn_norm_g, w_ffn_gate, w_ffn_up,
              w_ffn_down, w_router, b_router, w_exp_gate, w_exp_up, w_exp_down):
    for layer in range(DEPTH):
        h = rms_norm(x, attn_norm_g[layer])
        x = x + hybrid_mixer(h, w_in[layer], q_norm_g[layer], k_norm_g[layer], conv_w[layer],
                             conv_b[layer], conv_norm_g[layer], conv_norm_b[layer],
                             w_branch_a[layer], w_branch_b[layer], w_branch_c[layer], w_out[layer])
        h = rms_norm(x, ffn_norm_g[layer])
        i = layer // 2
        if layer % 2 == 0:
            x = x + swiglu(h, w_ffn_gate[i], w_ffn_up[i], w_ffn_down[i])
        else:
            x = x + routed_swiglu(h, w_router[i], b_router[i], w_exp_gate[i], w_exp_up[i],
                                  w_exp_down[i])
    return x
```

```python
import numpy as np
import ml_dtypes
import concourse.bass as bass
import concourse.mybir as mybir
from concourse.bass_utils import run_bass_kernel_spmd

F32 = mybir.dt.float32
BF16 = mybir.dt.bfloat16
I32 = mybir.dt.int32
ALU = mybir.AluOpType
AF = mybir.ActivationFunctionType
AX = mybir.AxisListType

D = 1024
SEQ = 4096
DEPTH = 2
IN_COLS = 7936
DFF = 2816
NF = DFF // 128
NEXP = 8
DIL = (1, 4, 16)
QA, KA, VA = 0, 768, 1536
QB, KB_, VB = 2304, 2816, 3328
GV, GG = 3840, 4352
GATE0 = 4864
NEG = -30000.0


class Tl:
    _n = 0

    def __init__(self, h, name):
        self.h = h
        self.name = name
        self.w = set()
        self.r = set()
        Tl._n += 1
        self.id = Tl._n
        self.born = 0
        self.freed = None

    def __getitem__(self, idx):
        return self.h[idx]


class Prog:
    STREAMS = ("pe", "act", "dve", "pool", "sp")

    def __init__(self, nc):
        self.nc = nc
        self.ops = []
        self.streams = {s: [] for s in self.STREAMS}
        self.semorder = {}
        self.n_t = 0
        self.tiles = {}
        self.ghost = {}
        self.scopes = []

    def sb(self, name, shape, dt):
        self.n_t += 1
        t = Tl(self.nc.alloc_sbuf_tensor(f"{name}_{self.n_t}", list(shape), dt), name)
        t.w = set(self.ghost.values())
        t.born = len(self.ops)
        self.tiles[t.id] = t
        if self.scopes:
            self.scopes[-1][1].append(t)
        return t

    def scope_enter(self):
        g = self.nc.reset_on_exit()
        g.__enter__()
        self.scopes.append((g, []))

    def scope_exit(self):
        g, tiles = self.scopes.pop()
        g.__exit__(None, None, None)
        for t in tiles:
            t.freed = len(self.ops)
            for oid in (t.w | t.r):
                op = self.ops[oid]
                k = op["semkey"]
                if k not in self.ghost or self.ops[self.ghost[k]]["pos"] < op["pos"]:
                    self.ghost[k] = oid

    def ps(self, name, shape, dt=F32):
        self.n_t += 1
        t = Tl(self.nc.alloc_psum_tensor(f"{name}_{self.n_t}", list(shape), dt), name)
        t.excl = True
        return t

    def dram(self, name, shape, dt, kind="Internal"):
        return Tl(self.nc.dram_tensor(name, list(shape), dt, kind=kind), name)

    def _add(self, stream, fn, reads, writes, semkey, inc):
        oid = len(self.ops)
        raw = set()
        rar = set()
        for t in reads:
            raw |= t.w
            if getattr(t, "excl", False):
                rar |= t.r
        deps = set(raw) | rar
        waw, war = set(), set()
        for t in writes:
            waw |= t.w
            war |= t.r
        deps |= waw | war
        is_dma = inc == 16
        keep = set()
        for d in deps:
            od = self.ops[d]
            if od["semkey"] == semkey and d not in raw:
                if not is_dma:
                    continue
                if d in waw and d not in war:
                    continue
            keep.add(d)
        op = dict(id=oid, stream=stream, fn=fn, deps=keep, semkey=semkey, inc=inc)
        self.ops.append(op)
        self.streams[stream].append(oid)
        self.semorder.setdefault(semkey, []).append(oid)
        op["pos"] = len(self.semorder[semkey]) - 1
        for t in reads:
            t.r.add(oid)
        for t in writes:
            t.w = {oid}
            t.r = set()
        return oid

    def c(self, eng, fn, reads=(), writes=()):
        return self._add(eng, fn, reads, writes, eng, 1)

    def dma(self, stream, out_ap, in_ap, semtile, reads=(), writes=(), **kw):
        fn = lambda e: e.dma_start(out=out_ap, in_=in_ap, **kw)
        return self._add(stream, fn, reads, writes, ("dma", semtile.id, stream == "pool"), 16)

    def idma(self, out_ap, out_off, in_ap, in_off, semtile, reads=(), writes=()):
        fn = lambda e: e.indirect_dma_start(out=out_ap, out_offset=out_off, in_=in_ap, in_offset=in_off)
        return self._add("pool", fn, reads, writes, ("dma", semtile.id, True), 16)

    def finish(self, stream, tiles):
        oid = self._add(stream, None, list(tiles), [], ("fin", stream), 0)
        for k, lst in self.semorder.items():
            if not isinstance(k, str) and k[0] == "dma":
                self.ops[oid]["deps"].add(lst[-1])
        return oid

    def emit(self):
        nc = self.nc
        ops = self.ops
        seen = {s: {} for s in self.STREAMS}
        needed = {}
        for op in ops:
            waits = {}
            for d in op["deps"]:
                od = ops[d]
                k = od["semkey"]
                if od["pos"] > waits.get(k, -1):
                    waits[k] = od["pos"]
            sn = seen[op["stream"]]
            w2 = []
            for k, p in waits.items():
                if sn.get(k, -1) >= p:
                    continue
                sn[k] = p
                needed.setdefault(k, set()).add(p)
                w2.append((k, p))
            op["waits"] = w2
        semval = {}
        sems = {}
        for k, lst in self.semorder.items():
            if not isinstance(k, str) or k not in needed:
                continue
            sems[k] = nc.alloc_semaphore("s_%s" % k)
            cnt = 0
            nd = needed[k]
            for p, oid in enumerate(lst):
                if p in nd:
                    cnt += 1
                    ops[oid]["do_inc"] = True
                semval[(k, p)] = cnt
        INF = 1 << 60
        dkeys = [k for k in self.semorder if not isinstance(k, str) and k[0] == "dma"]
        dkeys.sort(key=lambda k: self.tiles[k[1]].born)
        pools = {True: [], False: []}
        nphys = 0
        for k in dkeys:
            tl = self.tiles[k[1]]
            ent = None
            for cand in pools[k[2]]:
                if cand[2] <= tl.born:
                    ent = cand
                    break
            if ent is None:
                ent = [nc.alloc_semaphore("s_d%d" % nphys), 0, INF]
                nphys += 1
                pools[k[2]].append(ent)
            sems[k] = ent[0]
            base = ent[1]
            lst = self.semorder[k]
            for p, oid in enumerate(lst):
                ops[oid]["do_inc"] = True
                semval[(k, p)] = base + 16 * (p + 1)
            ent[1] = base + 16 * len(lst)
            ent[2] = tl.freed if tl.freed is not None else INF
        self.n_sems = len(sems)
        self.max_semval = max(semval.values()) if semval else 0
        engmap = dict(pe="tensor", act="scalar", dve="vector", pool="gpsimd", sp="sync")

        def run_stream(s):
            def body(e):
                for oid in self.streams[s]:
                    op = ops[oid]
                    for k, p in op["waits"]:
                        e.wait_ge(sems[k], semval[(k, p)])
                    if op["fn"] is None:
                        continue
                    ins = op["fn"](e)
                    if op.get("do_inc"):
                        ins.then_inc(sems[op["semkey"]], op["inc"])
            return body

        with nc.Block() as block:
            for s in self.STREAMS:
                if self.streams[s]:
                    getattr(block, engmap[s])(run_stream(s))


def sl(start, count, step=1):
    return slice(start, start + (count - 1) * step + 1, step)


def rr(gens):
    gens = list(gens)
    while gens:
        nxt = []
        for g in gens:
            try:
                next(g)
                nxt.append(g)
            except StopIteration:
                pass
        gens = nxt


class K:
    def __init__(self, T=SEQ, cfg=None):
        self.T = T
        self.cfg = cfg or {}
        self.nc = bass.Bass("TRN2", target_bir_lowering=False)
        self.P = Prog(self.nc)
        self.NT = T // 128
        self.NG = T // 512
        self._wq = 0
        self.setup()

    def setup(self):
        P, T = self.P, self.T
        L = DEPTH
        DFF = self.dff = self.cfg.get("dff", 2816)
        ein = lambda n, s, d: P.dram(n, s, d, kind="ExternalInput")
        self.x_in = ein("x", [T, D], F32)
        self.w_in = ein("w_in", [L, D, IN_COLS], F32)
        self.w_ba = ein("w_branch_a", [L, 256, D], F32)
        self.w_bb = ein("w_branch_b", [L, 512, D], F32)
        self.w_bc = ein("w_branch_c", [L, 512, D], F32)
        self.w_out = ein("w_out", [L, D, D], F32)
        self.w_fg = ein("w_ffn_gate", [1, D, DFF], F32)
        self.w_fu = ein("w_ffn_up", [1, D, DFF], F32)
        self.w_fd = ein("w_ffn_down", [1, DFF, D], F32)
        if self.cfg.get("moe", True):
            self.w_eg = ein("w_exp_gate", [1, NEXP, D, DFF], F32)
            self.w_eu = ein("w_exp_up", [1, NEXP, D, DFF], F32)
            self.w_ed = ein("w_exp_down", [1, NEXP, DFF, D], F32)
        self.d_gcols = ein("gcols", [128, L * 2 * 8], F32)
        self.d_qkg = ein("qkg", [128, L * 2], F32)
        self.d_qkrow = ein("qkrow", [128, L * 2 * 64], F32)
        self.d_convw = ein("convw", [128, L * 4 * 31], F32)
        self.d_convp = ein("convp", [128, L * 3 * 4], F32)
        self.d_wr = ein("wr_bc", [128, NEXP * D], F32)
        self.d_br = ein("br_bc", [128, NEXP], F32)
        self.d_grow = ein("grow", [128, D], F32)
        self.d_ebase = ein("ebase", [128, NEXP], F32)
        self.d_cb = ein("cbf", [128, 7 * 128], BF16)
        self.d_bt = ein("btab", [128, 12 * 256], F32)
        self.out = P.dram("out", [T, D], F32, kind="ExternalOutput")
        self.xs = [P.dram("xs0", [T, D], F32), P.dram("xs1", [T, D], F32)]
        self.mT_d = P.dram("mT_d", [D, T], BF16)
        self.oT_d = P.dram("oT_d", [1280, T], BF16, kind="ExternalOutput" if self.cfg.get("dbg_oT") else "Internal")
        self.dbg = {}

        def ld(name, d, shape, dt):
            t = P.sb(name, shape, dt)
            P.dma("sp", t[:], d[:], t, reads=[d], writes=[t])
            return t

        self.gcols = ld("gcols", self.d_gcols, [128, L * 2 * 8], F32)
        self.qkg = ld("qkg", self.d_qkg, [128, L * 2], F32)
        self.qkrow = ld("qkrow", self.d_qkrow, [128, L * 2 * 64], F32)
        self.convw = ld("convw", self.d_convw, [128, L * 4 * 31], F32)
        self.convp = ld("convp", self.d_convp, [128, L * 3 * 4], F32)
        self.cb = ld("cb", self.d_cb, [128, 7 * 128], BF16)
        self.bt = ld("bt", self.d_bt, [128, 12 * 256], F32)
        cbv = lambda i: (lambda: self.cb[:, i * 128:(i + 1) * 128])
        self.ident, self.tri, self.negones, self.ones, self.blockones, self.zeros, self.sbmask = [cbv(i) for i in range(7)]
        self.cst = P.sb("cst", [128, 4], F32)
        P.c("pool", lambda e: e.memset(self.cst[:, 0:1], 1e-6), writes=[self.cst])
        P.c("pool", lambda e: e.memset(self.cst[:, 1:2], 1.0), writes=[self.cst])
        P.c("pool", lambda e: e.memset(self.cst[:, 2:3], 0.0), writes=[self.cst])
        self.bank = [P.ps("bank%d" % i, [128, 512], F32) for i in range(8)]
        self.wbufs = [P.sb("wb", [128, 8, 128], BF16) for _ in range(6)]

    def build_full(self, n_layers=DEPTH):
        P = self.P
        x_cur = self.x_in
        for l in range(n_layers):
            P.scope_enter()
            self.hT = P.sb("hT", [128, 8, self.T], BF16)
            self.phase_norm(x_cur, (l * 2 + 0) * 8)
            self.phase_sb(l)
            self.phase_dil(l)
            self.phase_conv(l)
            x1_d = self.xs[0]
            self.phase_merge(l, x_cur, x1_d)
            xo = self.out if l == n_layers - 1 else self.xs[1]
            if l % 2 == 0:
                self.phase_norm(x1_d, (l * 2 + 1) * 8)
                self.phase_ffn_dense(x1_d, xo)
                P.scope_exit()
            else:
                P.scope_exit()
                self.phase_moe(l, x1_d, xo)
            x_cur = xo
        P.finish("sp", [self.out])
        P.emit()

    def dbg_out(self, name, shape, dt=F32):
        t = self.P.dram(name, shape, dt, kind="ExternalOutput")
        self.dbg[name] = t
        return t

    def wchunk(self, src_tl, src_ap):
        wb = self.wbufs[self._wq % len(self.wbufs)]
        self._wq += 1
        self.P.dma("pool", wb[:], src_ap, wb, reads=[src_tl], writes=[wb])
        return wb

    def win_cols(self, l, c0, n=128):
        return self.w_in.h[l].rearrange("(c p) n -> p c n", p=128)[:, :, c0:c0 + n]

    def phase_norm(self, x_d, gcol_off):
        P, T = self.P, self.T
        P.scope_enter()
        na = dict(
            xt=[P.sb("xt", [128, D], F32) for _ in range(2)],
            sq=P.sb("sq", [128, D], F32),
            ss=[P.sb("ss", [128, 1], F32) for _ in range(2)],
            rs=[P.sb("rs", [128, 1], F32) for _ in range(2)],
            hb=[P.sb("hb", [128, D], BF16) for _ in range(2)],
        )
        hT, gcols, cst = self.hT, self.gcols, self.cst
        for i in range(self.NT):
            b = i % 2
            xt, ss, rs, hb, sq = na["xt"][b], na["ss"][b], na["rs"][b], na["hb"][b], na["sq"]
            P.dma("sp", xt[:], x_d[i * 128:(i + 1) * 128, :], xt, reads=[x_d], writes=[xt])
            P.c("act", lambda e, xt=xt, ss=ss: e.activation(out=sq[:], in_=xt[:], func=AF.Square, accum_out=ss[:]),
                reads=[xt], writes=[sq, ss])
            P.c("act", lambda e, ss=ss, rs=rs: e.activation(out=rs[:], in_=ss[:], func=AF.Ln, scale=1.0 / D, bias=cst[:, 0:1]),
                reads=[ss, cst], writes=[rs])
            P.c("act", lambda e, rs=rs: e.activation(out=rs[:], in_=rs[:], func=AF.Exp, scale=-0.5),
                reads=[rs], writes=[rs])
            P.c("dve", lambda e, xt=xt, rs=rs, hb=hb: e.tensor_scalar(out=hb[:], in0=xt[:], scalar1=rs[:, 0:1], scalar2=None,
                                                                   op0=ALU.mult), reads=[xt, rs], writes=[hb])
            pt = self.bank[i % 2]
            ptv = pt[:].bitcast(BF16)
            for c in range(8):
                P.c("pe", lambda e, ptv=ptv, c=c, hb=hb: e.transpose(out=ptv[:, c * 128:(c + 1) * 128],
                                                                     in_=hb[:, c * 128:(c + 1) * 128], identity=self.ident()),
                    reads=[hb, self.cb], writes=[pt])
            for c in range(8):
                eng = "dve" if c % 2 == 0 else "pool"
                eng = "dve"
                P.c(eng, lambda e, ptv=ptv, c=c, i=i: e.tensor_scalar(
                    out=hT[:, c, i * 128:(i + 1) * 128], in0=ptv[:, c * 128:(c + 1) * 128],
                    scalar1=gcols[:, gcol_off + c:gcol_off + c + 1], scalar2=None, op0=ALU.mult),
                    reads=[pt, gcols], writes=[hT])
        P.scope_exit()

    def proj_fm(self, wb, pm, tg, wcol0=0, m=128):
        P, hT = self.P, self.hT
        for c in range(8):
            P.c("pe", lambda e, c=c: e.matmul(pm[0:m, :], lhsT=wb[:, c, wcol0:wcol0 + m], rhs=hT[:, c, tg * 512:(tg + 1) * 512],
                                              start=(c == 0), stop=(c == 7)), reads=[wb, hT], writes=[pm])

    def phase_sb(self, l):
        P, T, NT, NG = self.P, self.T, self.NT, self.NG
        hT, bank = self.hT, self.bank
        P.scope_enter()
        if True:
            NCH = 3
            self.sbb = dict(
                vall=P.sb("vall", [128, NT, 512], BF16),
                wv=P.sb("wv", [128, 8, 512], BF16),
                qT=P.sb("qT", [128, T], BF16),
                kT=P.sb("kT", [128, T], BF16),
                obT=P.sb("obT", [128, T], BF16),
                ch=[dict(e=P.sb("e", [128, 512], F32),
                         sp=[P.sb("sp", [128, 512], BF16) for _ in range(2)],
                         a=[P.sb("a", [128, 512], BF16) for _ in range(2)],
                         r32=P.sb("r32", [128, 512], F32),
                         rb=[P.sb("rb", [128, 512], BF16) for _ in range(2)],
                         zb=bank[2 + 2 * i], ob=bank[3 + 2 * i]) for i in range(NCH)],
            )
        S = self.sbb
        vall, wv, qT, kT, obT = S["vall"], S["wv"], S["qT"], S["kT"], S["obT"]
        cst = self.cst
        P.dma("pool", wv[:], self.win_cols(l, VB, 512), wv, reads=[self.w_in], writes=[wv])
        for i in range(NT):
            pm = bank[i % 2]
            for c in range(8):
                P.c("pe", lambda e, c=c, i=i, pm=pm: e.matmul(pm[:, :], lhsT=hT[:, c, i * 128:(i + 1) * 128], rhs=wv[:, c, :],
                                                             start=(c == 0), stop=(c == 7)), reads=[hT, wv], writes=[pm])
            if i % 2 == 0:
                P.c("act", lambda e, i=i, pm=pm: e.copy(out=vall[:, i, :], in_=pm[:, :]), reads=[pm], writes=[vall])
            else:
                P.c("dve", lambda e, i=i, pm=pm: e.tensor_copy(out=vall[:, i, :], in_=pm[:, :]), reads=[pm], writes=[vall])

        def chain(h, Q, C):
            prow = (h % 2) * 64
            hp = h // 2
            zb, ob, esb, r32 = C["zb"], C["ob"], C["e"], C["r32"]
            rows = slice(prow, prow + 64)
            q0 = Q * 512
            P.c("pe", lambda e: e.matmul(ob[:, :], lhsT=self.zeros(), rhs=qT[:, q0:q0 + 512], start=True, stop=False),
                reads=[self.cb, qT], writes=[ob])
            P.c("pool", lambda e: e.memset(r32[:], 0.0), writes=[r32])
            yield
            kbs = list(range(4 * Q + 3, -1, -1))
            for ti, kb in enumerate(kbs):
                j = max(kb - 4 * Q, 0)
                c0 = 128 * j
                diag = kb >= 4 * Q
                first = ti == 0
                last = kb == 0
                sp, a, rb = C["sp"][ti % 2], C["a"][ti % 2], C["rb"][ti % 2]
                rbp = C["rb"][(ti + 1) % 2]
                def qk(stop_after, kb=kb, c0=c0, diag=diag):
                    st = bool(stop_after and not diag)
                    P.c("pe", lambda e, kb=kb, c0=c0, st=st: e.matmul(zb[:, c0:512], lhsT=kT[rows, kb * 128:(kb + 1) * 128],
                                                                      rhs=qT[rows, q0 + c0:q0 + 512], start=True, stop=st),
                        reads=[kT, qT], writes=[zb])
                    if diag:
                        P.c("pe", lambda e, c0=c0, sa=bool(stop_after): e.matmul(zb[:, c0:c0 + 128], lhsT=self.ident(), rhs=self.sbmask(),
                                                                                 start=False, stop=sa), reads=[self.cb], writes=[zb])
                qk(True)
                yield
                P.c("act", lambda e, c0=c0: e.activation(out=esb[:, c0:512], in_=zb[:, c0:512], func=AF.Exp),
                    reads=[zb], writes=[esb])
                yield
                P.c("act", lambda e, c0=c0, sp=sp: e.activation(out=sp[:, c0:512], in_=esb[:, c0:512], func=AF.Ln, bias=cst[:, 1:2]),
                    reads=[esb, cst], writes=[sp])
                yield
                qk(False)
                P.c("pe", lambda e, c0=c0, sp=sp, first=first: e.matmul(zb[:, c0:512], lhsT=self.tri(), rhs=sp[:, c0:512],
                                                                        start=False, stop=first),
                    reads=[self.cb, sp], writes=[zb])
                if not first:
                    P.c("pe", lambda e, c0=c0, rbp=rbp: e.matmul(zb[:, c0:512], lhsT=self.negones(), rhs=rbp[:, c0:512],
                                                                 start=False, stop=True),
                        reads=[self.cb, rbp], writes=[zb])
                if not last:
                    P.c("dve", lambda e, c0=c0, sp=sp: e.tensor_tensor(out=r32[:, c0:512], in0=r32[:, c0:512], in1=sp[:, c0:512],
                                                                       op=ALU.add), reads=[r32, sp], writes=[r32])
                    P.c("dve", lambda e, rb=rb: e.tensor_copy(out=rb[:, :], in_=r32[:, :]), reads=[r32], writes=[rb])
                yield
                P.c("act", lambda e, c0=c0, a=a: e.activation(out=a[:, c0:512], in_=zb[:, c0:512], func=AF.Exp),
                    reads=[zb], writes=[a])
                yield
                P.c("pe", lambda e, c0=c0, a=a, kb=kb, last=last: e.matmul(ob[:, c0:512], lhsT=vall[:, kb, hp * 128:(hp + 1) * 128],
                                                                          rhs=a[:, c0:512], start=False, stop=last),
                    reads=[vall, a], writes=[ob])
                yield
            P.c("dve", lambda e: e.tensor_copy(out=obT[rows, q0:q0 + 512], in_=ob[rows, :]), reads=[ob], writes=[obT])
            yield

        for hp in range(4):
            wq = self.wchunk(self.w_in, self.win_cols(l, QB + hp * 128))
            wk = self.wchunk(self.w_in, self.win_cols(l, KB_ + hp * 128))
            for tg in range(NG):
                pm = bank[0]
                self.proj_fm(wq, pm, tg)
                P.c("act", lambda e, tg=tg, pm=pm: e.mul(out=qT[:, tg * 512:(tg + 1) * 512], in_=pm[:, :], mul=0.125),
                    reads=[pm], writes=[qT])
                pm = bank[1]
                self.proj_fm(wk, pm, tg)
                P.c("dve", lambda e, tg=tg, pm=pm: e.tensor_copy(out=kT[:, tg * 512:(tg + 1) * 512], in_=pm[:, :]),
                    reads=[pm], writes=[kT])
            jobs = [(2 * hp + hh, Q) for Q in range(NG - 1, -1, -1) for hh in range(2)]
            nch = len(S["ch"])
            slots = [[] for _ in range(nch)]
            for ji, jb in enumerate(jobs):
                slots[ji % nch].append(jb)

            def slot_gen(si):
                for (h, Q) in slots[si]:
                    yield from chain(h, Q, S["ch"][si])
            rr([slot_gen(si) for si in range(nch)])
            P.dma("sp", self.oT_d[256 + hp * 128:256 + (hp + 1) * 128, :], obT[:, :], obT, reads=[obT], writes=[self.oT_d])
        P.scope_exit()

    def phase_dil(self, l):
        P, T, NT, NG = self.P, self.T, self.NT, self.NG
        hT, bank, cst = self.hT, self.bank, self.cst
        P.scope_enter()
        qn = P.sb("qn", [128, T], BF16)
        kn = P.sb("kn", [128, T], BF16)
        vp = P.sb("vp", [128, NT, 128], BF16)
        vT = P.sb("vT", [128, T], BF16)
        acc = P.sb("acc", [128, 2, T], F32)
        raw = [P.sb("raw", [128, 512], F32) for _ in range(2)]
        sq = [P.sb("sqd", [128, 512], BF16) for _ in range(2)]
        rst = [P.sb("rst", [128, 512], F32) for _ in range(2)]
        tmp = [P.sb("tmpd", [128, 512], F32) for _ in range(2)]
        pT = [P.sb("pT", [128, 512], BF16) for _ in range(3)]
        oa = P.sb("oa", [128, T], BF16)
        sm = P.sb("smalld", [128, 136], F32)
        bt = P.sb("bt", [128, 12 * 256], F32)
        P.dma("sp", bt[:], self.d_bt[:], bt, reads=[self.d_bt], writes=[bt])
        qkrow, qkg = self.qkrow, self.qkg
        P.c("dve", lambda e: e.tensor_tensor(out=sm[:, 0:128], in0=qkrow[:, l * 128:(l + 1) * 128], in1=qkrow[:, l * 128:(l + 1) * 128],
                                             op=ALU.mult), reads=[qkrow], writes=[sm])
        P.c("dve", lambda e: e.reduce_max(out=sm[:, 128:129], in_=sm[:, 0:64], axis=AX.X), reads=[sm], writes=[sm])
        P.c("dve", lambda e: e.reduce_max(out=sm[:, 129:130], in_=sm[:, 64:128], axis=AX.X), reads=[sm], writes=[sm])
        P.c("dve", lambda e: e.tensor_tensor(out=sm[:, 130:131], in0=sm[:, 128:129], in1=sm[:, 129:130], op=ALU.add),
            reads=[sm], writes=[sm])
        P.c("dve", lambda e: e.tensor_scalar(out=sm[:, 130:131], in0=sm[:, 130:131], scalar1=-4.0, scalar2=None, op0=ALU.mult),
            reads=[sm], writes=[sm])
        P.c("dve", lambda e: e.tensor_scalar(out=sm[:, 131:132], in0=qkg[:, 2 * l:2 * l + 1], scalar1=0.125, scalar2=None, op0=ALU.mult),
            reads=[qkg], writes=[sm])
        negc = lambda: sm[:, 130:131]
        g8 = lambda: sm[:, 131:132]
        gk = lambda: qkg[:, 2 * l + 1:2 * l + 2]
        cnt = [0]
        def group(s, g):
            if True:
                d = DIL[g]
                nb = T // (128 * d)
                H = 4 * g + 2 * s
                wq = self.wchunk(self.w_in, self.win_cols(l, QA + H * 64))
                wk = self.wchunk(self.w_in, self.win_cols(l, KA + H * 64))
                wvv = self.wchunk(self.w_in, self.win_cols(l, VA + H * 64))
                if self.cfg.get("dil_stop", 99) <= 0.1:
                    return
                for (w, dst, gain) in ((wq, qn, g8), (wk, kn, gk)):
                    for tg in range(NG):
                        i2 = cnt[0] % 2
                        cnt[0] += 1
                        pm, pm2 = bank[i2], bank[2 + i2]
                        rw, sqq, rs_ = raw[i2], sq[i2], rst[i2]
                        self.proj_fm(w, pm, tg)
                        P.c("dve", lambda e, pm=pm, rw=rw: e.tensor_copy(out=rw[:], in_=pm[:, :]), reads=[pm], writes=[rw])
                        P.c("act", lambda e, pm=pm, sqq=sqq: e.activation(out=sqq[:], in_=pm[:, :], func=AF.Square), reads=[pm], writes=[sqq])
                        P.c("pe", lambda e, pm2=pm2, sqq=sqq: e.matmul(pm2[:, :], lhsT=self.blockones(), rhs=sqq[:], start=True, stop=True),
                            reads=[self.cb, sqq], writes=[pm2])
                        P.c("act", lambda e, pm2=pm2, rs_=rs_: e.activation(out=rs_[:], in_=pm2[:, :], func=AF.Ln, scale=1.0 / 64, bias=cst[:, 0:1]),
                            reads=[pm2, cst], writes=[rs_])
                        P.c("act", lambda e, rs_=rs_: e.activation(out=rs_[:], in_=rs_[:], func=AF.Exp, scale=-0.5), reads=[rs_], writes=[rs_])
                        if self.cfg.get("dil_stop", 99) <= 0.5:
                            continue
                        P.c("dve", lambda e, dst=dst, tg=tg, rw=rw, rs_=rs_, gain=gain: e.scalar_tensor_tensor(
                            out=dst[:, :].rearrange("p (r i) -> p r i", r=d)[:, :, tg * (512 // d):(tg + 1) * (512 // d)],
                            in0=rw[:].rearrange("p (i r) -> p r i", r=d), scalar=gain(),
                            in1=rs_[:].rearrange("p (i r) -> p r i", r=d), op0=ALU.mult, op1=ALU.mult),
                            reads=[rw, rs_, sm, qkg], writes=[dst])
                if self.cfg.get("dil_stop", 99) <= 1:
                    return
                for tg in range(NG):
                    pm = bank[tg % 2]
                    self.proj_fm(wvv, pm, tg)
                    P.c("act", lambda e, pm=pm, tg=tg: e.copy(
                        out=vT[:, :].rearrange("p (r i) -> p r i", r=d)[:, :, tg * (512 // d):(tg + 1) * (512 // d)],
                        in_=pm[:, :].rearrange("p (i r) -> p r i", r=d)), reads=[pm], writes=[vT])
                for b in range(NT):
                    pm = bank[4 + (b // 4) % 2]
                    pmv = pm[:].bitcast(BF16)
                    P.c("pe", lambda e, pmv=pmv, b=b: e.transpose(out=pmv[:, (b % 4) * 128:(b % 4 + 1) * 128], in_=vT[:, b * 128:(b + 1) * 128],
                                                                 identity=self.ident()), reads=[vT, self.cb], writes=[pm])
                    if b % 4 == 3:
                        P.c("dve", lambda e, pmv=pmv, b=b: e.tensor_copy(out=vp[:, b - 3:b + 1, :],
                                                                        in_=pmv[:, 0:512].rearrange("p (a n) -> p a n", a=4)),
                            reads=[pm], writes=[vp])
                if self.cfg.get("dil_stop", 99) <= 2:
                    return
                def head(hh):
                    rows = slice(hh * 64, hh * 64 + 64)
                    Hh = H + hh
                    btH = lambda Hh=Hh: bt[:, Hh * 256:(Hh + 1) * 256]
                    batches = [(r, mb) for r in range(d) for mb in range(nb // 2)]

                    def stage1(bi):
                        r, mb = batches[bi]
                        sbk = bank[4 + bi % 2]
                        tm = tmp[bi % 2]
                        pt = pT[bi % 3]
                        width = 0
                        for i in range(2):
                            m = 2 * mb + i
                            nq = 256 if m < nb - 1 else 128
                            P.c("pe", lambda e, i=i, m=m, nq=nq: e.matmul(
                                sbk[:, i * 256:i * 256 + nq], lhsT=kn[rows, (r * nb + m) * 128:(r * nb + m + 1) * 128],
                                rhs=qn[rows, (r * nb + m) * 128:(r * nb + m) * 128 + nq], start=True, stop=True), reads=[kn, qn], writes=[sbk])
                            P.c("dve", lambda e, i=i, nq=nq: e.tensor_tensor(out=tm[:, i * 256:i * 256 + nq], in0=sbk[:, i * 256:i * 256 + nq],
                                                                            in1=btH()[:, 0:nq], op=ALU.add), reads=[sbk, bt], writes=[tm])
                            width = i * 256 + nq
                        P.c("act", lambda e, width=width: e.activation(out=pt[:, 0:width], in_=tm[:, 0:width], func=AF.Exp, bias=negc()),
                            reads=[tm, sm], writes=[pt])

                    def stage2(bi):
                        r, mb = batches[bi]
                        ub = bank[6 + bi % 2]
                        pt = pT[bi % 3]
                        ptp = pT[(bi - 1) % 3]
                        m0, m1 = 2 * mb, 2 * mb + 1
                        blk = lambda m: r * nb + m
                        contribs = [[], []]
                        if m0 > 0:
                            contribs[0].append((blk(m0 - 1), ptp, 384))
                        contribs[0].append((blk(m0), pt, 0))
                        contribs[1].append((blk(m0), pt, 128))
                        contribs[1].append((blk(m1), pt, 256))
                        for ni in range(2):
                            for ud in range(2):
                                n = len(contribs[ni])
                                for ci, (kb, ptile, c0) in enumerate(contribs[ni]):
                                    lhs = (lambda kb=kb: vp[:, kb, :]) if ud == 0 else self.ones
                                    P.c("pe", lambda e, lhs=lhs, ptile=ptile, c0=c0, ni=ni, ud=ud, ci=ci, n=n: e.matmul(
                                        ub[:, ud * 256 + ni * 128:ud * 256 + (ni + 1) * 128], lhsT=lhs(), rhs=ptile[:, c0:c0 + 128],
                                        start=(ci == 0), stop=(ci == n - 1)), reads=[vp, self.cb, ptile], writes=[ub])
                        ov = lambda: acc[rows, :, sl(m0 * 128 * d + r, 256, d)]
                        iv = lambda: ub[rows, :].rearrange("p (a n) -> p a n", a=2)
                        if g == 0:
                            P.c("dve", lambda e: e.tensor_copy(out=ov(), in_=iv()), reads=[ub], writes=[acc])
                        else:
                            P.c("dve", lambda e: e.tensor_tensor(out=ov(), in0=iv(), in1=ov(), op=ALU.add), reads=[ub, acc], writes=[acc])

                    for bi in range(len(batches) + 1):
                        if bi < len(batches):
                            stage1(bi)
                        if bi >= 1:
                            stage2(bi - 1)
                for hh in range(2):
                    head(hh)

        for s in range(2):
            for g in range(3):
                group(s, g)
            if self.cfg.get("dil_stop", 99) <= 3:
                continue
            P.c("dve", lambda e: e.reciprocal(out=acc[:, 1, :], in_=acc[:, 1, :]), reads=[acc], writes=[acc])
            P.c("dve", lambda e: e.tensor_tensor(out=oa[:, :], in0=acc[:, 0, :], in1=acc[:, 1, :], op=ALU.mult), reads=[acc], writes=[oa])
            P.dma("sp", self.oT_d[s * 128:(s + 1) * 128, :], oa[:, :], oa, reads=[oa], writes=[self.oT_d])
        P.scope_exit()

    def phase_conv(self, l):
        P, T, NT, NG = self.P, self.T, self.NT, self.NG
        hT, bank, cst = self.hT, self.bank, self.cst
        P.scope_enter()
        uT = P.sb("uT", [128, 4, 32 + T], BF16)
        dg = P.sb("dg", [128, 4, 31, 128], BF16)
        idf = P.sb("idf", [128, 128], F32)
        cv = [P.sb("cv", [128, 512], F32) for _ in range(4)]
        xc = [P.sb("xc", [128, 512], F32) for _ in range(4)]
        sqc = [P.sb("sqc", [128, 512], F32) for _ in range(2)]
        sig = [P.sb("sig", [128, 512], F32) for _ in range(2)]
        rsd = P.sb("rsd", [128, 512], F32)
        ost = [P.sb("ost", [128, 4, 512], BF16) for _ in range(2)]
        onesf = P.sb("onesf", [128, 128], F32)
        convw, convp = self.convw, self.convp
        P.c("pool", lambda e: e.memset(onesf[:], 1.0), writes=[onesf])
        P.c("dve", lambda e: e.tensor_copy(out=idf[:], in_=self.ident()), reads=[self.cb], writes=[idf])
        P.c("pool", lambda e: e.memset(uT[:, :, 0:32], 0.0), writes=[uT])
        for cc in range(4):
            for k in range(31):
                col = (l * 4 + cc) * 31 + k
                P.c("dve", lambda e, cc=cc, k=k, col=col: e.tensor_scalar(out=dg[:, cc, k, :], in0=idf[:], scalar1=convw[:, col:col + 1],
                                                                        scalar2=None, op0=ALU.mult), reads=[idf, convw], writes=[dg])
        for cc in range(4):
            wv_ = self.wchunk(self.w_in, self.win_cols(l, GV + cc * 128))
            wg_ = self.wchunk(self.w_in, self.win_cols(l, GG + cc * 128))
            for tg in range(NG):
                pmv, pmg = bank[tg % 2], bank[2 + tg % 2]
                sg = sig[tg % 2]
                self.proj_fm(wg_, pmg, tg)
                self.proj_fm(wv_, pmv, tg)
                P.c("act", lambda e, pmg=pmg, sg=sg: e.activation(out=sg[:], in_=pmg[:, :], func=AF.Sigmoid), reads=[pmg], writes=[sg])
                P.c("dve", lambda e, pmv=pmv, sg=sg, cc=cc, tg=tg: e.tensor_tensor(out=uT[:, cc, 32 + tg * 512:32 + (tg + 1) * 512], in0=pmv[:, :],
                                                                                 in1=sg[:], op=ALU.mult), reads=[pmv, sg], writes=[uT])
        pcol = lambda which, cc: convp[:, (l * 3 + which) * 4 + cc:(l * 3 + which) * 4 + cc + 1]
        def conv_tile(tg):
            for cc in range(4):
                pm = bank[4 + cc % 2]
                for k in range(31):
                    P.c("pe", lambda e, cc=cc, k=k, pm=pm: e.matmul(pm[:, :], lhsT=dg[:, cc, k, :],
                                                                   rhs=uT[:, cc, 2 + tg * 512 + k:2 + tg * 512 + k + 512],
                                                                   start=(k == 0), stop=(k == 30)), reads=[dg, uT], writes=[pm])
                P.c("act", lambda e, cc=cc, pm=pm: e.activation(out=cv[cc][:], in_=pm[:, :], func=AF.Identity, bias=pcol(0, cc)),
                    reads=[pm, convp], writes=[cv[cc]])
            pmm = bank[6]
            for cc in range(4):
                P.c("pe", lambda e, cc=cc: e.matmul(pmm[:, :], lhsT=onesf[:], rhs=cv[cc][:], start=(cc == 0), stop=(cc == 3)),
                    reads=[onesf, cv[cc]], writes=[pmm])
            for cc in range(4):
                P.c("dve", lambda e, cc=cc: e.scalar_tensor_tensor(out=xc[cc][:], in0=pmm[:, :], scalar=-1.0 / 512, in1=cv[cc][:],
                                                                   op0=ALU.mult, op1=ALU.add), reads=[pmm, cv[cc]], writes=[xc[cc]])
            pmv = bank[7]
            for cc in range(4):
                sq_ = sqc[cc % 2]
                P.c("act", lambda e, cc=cc, sq_=sq_: e.activation(out=sq_[:], in_=xc[cc][:], func=AF.Square), reads=[xc[cc]], writes=[sq_])
                P.c("pe", lambda e, cc=cc, sq_=sq_: e.matmul(pmv[:, :], lhsT=onesf[:], rhs=sq_[:], start=(cc == 0), stop=(cc == 3)),
                    reads=[onesf, sq_], writes=[pmv])
            P.c("act", lambda e: e.activation(out=rsd[:], in_=pmv[:, :], func=AF.Ln, scale=1.0 / 512, bias=cst[:, 0:1]),
                reads=[pmv, cst], writes=[rsd])
            P.c("act", lambda e: e.activation(out=rsd[:], in_=rsd[:], func=AF.Exp, scale=-0.5), reads=[rsd], writes=[rsd])
            os_ = ost[tg % 2]
            for cc in range(4):
                P.c("dve", lambda e, cc=cc: e.tensor_tensor(out=xc[cc][:], in0=xc[cc][:], in1=rsd[:], op=ALU.mult),
                    reads=[xc[cc], rsd], writes=[xc[cc]])
                P.c("dve", lambda e, cc=cc: e.tensor_scalar(out=xc[cc][:], in0=xc[cc][:], scalar1=pcol(1, cc), scalar2=pcol(2, cc),
                                                            op0=ALU.mult, op1=ALU.add), reads=[xc[cc], convp], writes=[xc[cc]])
                P.c("act", lambda e, cc=cc, os_=os_: e.activation(out=os_[:, cc, :], in_=xc[cc][:], func=AF.Silu), reads=[xc[cc]], writes=[os_])
            P.dma("sp", self.oT_d.h[768:1280, tg * 512:(tg + 1) * 512].rearrange("(c p) t -> p c t", p=128), os_[:], os_,
                  reads=[os_], writes=[self.oT_d])

        for tg in range(NG):
            conv_tile(tg)
        P.scope_exit()

    def phase_merge(self, l, x_d, x1_d):
        P, T, NT, NG = self.P, self.T, self.NT, self.NG
        hT, bank = self.hT, self.bank
        P.scope_enter()
        oTt = [P.sb("oTt", [128, 10, 512], BF16) for _ in range(2)]
        sg = [P.sb("sg", [128, 512], F32) for _ in range(3)]
        t1 = [P.sb("t1", [128, 512], F32) for _ in range(3)]
        mst = [P.sb("mst", [128, 512], BF16) for _ in range(2)]
        wbr = [P.sb("wbr", [128, 10, 128], BF16) for _ in range(2)]
        mT_d = self.mT_d
        wsrc = ((self.w_ba, 0, 2), (self.w_bb, 2, 4), (self.w_bc, 6, 4))

        def one(dmc, tg, wb_, wg):
            ot = oTt[tg % 2]
            P.dma("sp", ot[:], self.oT_d.h[:, tg * 512:(tg + 1) * 512].rearrange("(c p) t -> p c t", p=128), ot,
                  reads=[self.oT_d], writes=[ot])
            for b in range(3):
                self.proj_fm(wg[b], bank[b], tg)
                P.c("act", lambda e, b=b: e.activation(out=sg[b][:], in_=bank[b][:, :], func=AF.Sigmoid), reads=[bank[b]], writes=[sg[b]])
            for b, (wt, c0, nchunk) in enumerate(wsrc):
                pm = bank[3 + b]
                for ci in range(nchunk):
                    P.c("pe", lambda e, pm=pm, c0=c0, ci=ci, nchunk=nchunk: e.matmul(pm[:, :], lhsT=wb_[:, c0 + ci, :], rhs=ot[:, c0 + ci, :],
                                                                                   start=(ci == 0), stop=(ci == nchunk - 1)),
                        reads=[wb_, ot], writes=[pm])
                P.c("dve", lambda e, pm=pm, b=b: e.tensor_tensor(out=t1[b][:], in0=pm[:, :], in1=sg[b][:], op=ALU.mult),
                    reads=[pm, sg[b]], writes=[t1[b]])
            P.c("pool", lambda e: e.tensor_tensor(out=t1[0][:], in0=t1[0][:], in1=t1[1][:], op=ALU.add), reads=[t1[0], t1[1]], writes=[t1[0]])
            ms = mst[tg % 2]
            P.c("dve", lambda e, ms=ms: e.tensor_tensor(out=ms[:], in0=t1[0][:], in1=t1[2][:], op=ALU.add), reads=[t1[0], t1[2]], writes=[ms])
            P.dma("act", mT_d[dmc * 128:(dmc + 1) * 128, tg * 512:(tg + 1) * 512], ms[:], ms, reads=[ms], writes=[mT_d])

        for dmc in range(8):
            wb_ = wbr[dmc % 2]
            for (wt, c0, nchunk) in wsrc:
                P.dma("pool", wb_[:, c0:c0 + nchunk, :], wt.h[l].rearrange("(c p) n -> p c n", p=128)[:, :, dmc * 128:(dmc + 1) * 128], wb_,
                      reads=[wt], writes=[wb_])
            wg = [self.wchunk(self.w_in, self.win_cols(l, GATE0 + b * 1024 + dmc * 128)) for b in range(3)]
            for tg in range(NG):
                one(dmc, tg, wb_, wg)
        P.scope_exit()
        P.scope_enter()
        wo = P.sb("wo", [128, 8, 1024], BF16)
        mt = [P.sb("mt", [128, 8, 512], BF16) for _ in range(2)]
        xt = [P.sb("xt2", [128, D], F32) for _ in range(3)]
        for c in range(8):
            P.dma("pool", wo[:, c, :], self.w_out.h[l][c * 128:(c + 1) * 128, :], wo, reads=[self.w_out], writes=[wo])

        def tile(tg, j, mtt):
            i = tg * 4 + j
            x_ = xt[i % 3]
            P.dma("sp", x_[:], x_d[i * 128:(i + 1) * 128, :], x_, reads=[x_d], writes=[x_])
            for half in range(2):
                pm = bank[(2 * i + half) % 4]
                for c in range(8):
                    P.c("pe", lambda e, pm=pm, c=c, half=half: e.matmul(pm[:, :], lhsT=mtt[:, c, j * 128:(j + 1) * 128],
                                                                       rhs=wo[:, c, half * 512:(half + 1) * 512], start=(c == 0), stop=(c == 7)),
                        reads=[mtt, wo], writes=[pm])
                P.c("dve", lambda e, pm=pm, half=half: e.tensor_tensor(out=x_[:, half * 512:(half + 1) * 512], in0=pm[:, :],
                                                                      in1=x_[:, half * 512:(half + 1) * 512], op=ALU.add),
                    reads=[pm, x_], writes=[x_])
            P.dma("act", x1_d[i * 128:(i + 1) * 128, :], x_[:], x_, reads=[x_], writes=[x1_d])

        for tg in range(NG):
            mtt = mt[tg % 2]
            P.dma("sp", mtt[:], mT_d.h[:, tg * 512:(tg + 1) * 512].rearrange("(c p) t -> p c t", p=128), mtt, reads=[mT_d], writes=[mtt])
            for j in range(4):
                tile(tg, j, mtt)
        P.scope_exit()

    def ffn_core(self, xT, xoff, R, wg_src, wu_src, wd_src, sink):
        P, bank = self.P, self.bank
        nf = self.dff // 128
        hid = P.sb("hid", [128, nf, R], BF16)
        wd = [P.sb("wd", [128, nf, 512], BF16) for _ in range(2)]
        sil = [P.sb("sil", [128, 512], F32) for _ in range(2)]
        for half in range(2):
            for f0 in range(0, nf, 11):
                f1 = min(nf, f0 + 11)
                P.dma("pool", wd[half][:, f0:f1, :], wd_src[1][f0 * 128:f1 * 128, half * 512:(half + 1) * 512].rearrange("(f p) n -> p f n", p=128),
                      wd[half], reads=[wd_src[0]], writes=[wd[half]])
        segs = [(r0, min(512, R - r0)) for r0 in range(0, R, 512)]
        cnt = 0
        for f in range(nf):
            wg = self.wchunk(wg_src[0], wg_src[1].rearrange("(c p) n -> p c n", p=128)[:, :, f * 128:(f + 1) * 128])
            wu = self.wchunk(wu_src[0], wu_src[1].rearrange("(c p) n -> p c n", p=128)[:, :, f * 128:(f + 1) * 128])
            for (r0, w) in segs:
                pa, pu = bank[cnt % 2], bank[2 + cnt % 2]
                sl_ = sil[cnt % 2]
                cnt += 1
                for (wt, pm) in ((wg, pa), (wu, pu)):
                    for c in range(8):
                        P.c("pe", lambda e, wt=wt, pm=pm, c=c, r0=r0, w=w: e.matmul(pm[:, 0:w], lhsT=wt[:, c, :], rhs=xT[:, c, xoff + r0:xoff + r0 + w],
                                                                                   start=(c == 0), stop=(c == 7)), reads=[wt, xT], writes=[pm])
                P.c("act", lambda e, pa=pa, sl_=sl_, w=w: e.activation(out=sl_[:, 0:w], in_=pa[:, 0:w], func=AF.Silu), reads=[pa], writes=[sl_])
                P.c("dve", lambda e, pu=pu, sl_=sl_, w=w, r0=r0, f=f: e.tensor_tensor(out=hid[:, f, r0:r0 + w], in0=pu[:, 0:w], in1=sl_[:, 0:w],
                                                                                     op=ALU.mult), reads=[pu, sl_], writes=[hid])
        for j in range(R // 128):
            for half in range(2):
                pm = bank[4 + (2 * j + half) % 4]
                for f in range(nf):
                    P.c("pe", lambda e, pm=pm, f=f, j=j, half=half: e.matmul(pm[:, :], lhsT=hid[:, f, j * 128:(j + 1) * 128], rhs=wd[half][:, f, :],
                                                                            start=(f == 0), stop=(f == nf - 1)), reads=[hid, wd[half]], writes=[pm])
                sink(j, half, pm)

    def phase_ffn_dense(self, x1_d, xo_d):
        P, T = self.P, self.T
        RG = min(1024, T)
        for rg in range(T // RG):
            P.scope_enter()
            xt = [P.sb("xt3", [128, D], F32) for _ in range(3)]

            def sink(j, half, pm, rg=rg, xt=xt):
                i = rg * (RG // 128) + j
                x_ = xt[i % 3]
                if half == 0:
                    P.dma("sp", x_[:], x1_d[i * 128:(i + 1) * 128, :], x_, reads=[x1_d], writes=[x_])
                P.c("dve", lambda e: e.tensor_tensor(out=x_[:, half * 512:(half + 1) * 512], in0=pm[:, :],
                                                     in1=x_[:, half * 512:(half + 1) * 512], op=ALU.add), reads=[pm, x_], writes=[x_])
                if half == 1:
                    P.dma("act", xo_d[i * 128:(i + 1) * 128, :], x_[:], x_, reads=[x_], writes=[xo_d])

            self.ffn_core(self.hT, rg * RG, RG, (self.w_fg, self.w_fg.h[0]), (self.w_fu, self.w_fu.h[0]), (self.w_fd, self.w_fd.h[0]), sink)
            P.scope_exit()

    def phase_moe(self, l, x1_d, xo_d):
        P, T, NT, bank, cst = self.P, self.T, self.NT, self.bank, self.cst
        CAP = self.cfg.get("cap", 1536)
        XS_d = P.dram("XS_d", [NEXP * CAP, D], BF16)
        YS_d = P.dram("YS_d", [NEXP * CAP, D], F32)
        gsm = P.sb("gsm", [128, NT, 2], F32)
        idxs = P.sb("idxs", [128, NT, 2], I32)
        P.scope_enter()
        grow = P.sb("grow", [128, D], F32)
        wrg = P.sb("wrg", [128, NEXP, D], F32)
        brt = P.sb("brt", [128, NEXP], F32)
        ebase = P.sb("ebase", [128, NEXP], F32)
        zt = P.sb("zt", [128, 2, D], BF16)
        xt = [P.sb("xtm", [128, D], F32) for _ in range(2)]
        junk = P.sb("junk", [128, D], F32)
        h2b = [P.sb("h2b", [128, D], BF16) for _ in range(2)]
        sm = [P.sb("smm", [128, 96], F32) for _ in range(2)]
        selb = [P.sb("selb", [128, NEXP], BF16) for _ in range(2)]
        selcum = [P.sb("selcum", [128, NEXP], BF16) for _ in range(2)]
        P.dma("sp", grow[:], self.d_grow[:], grow, reads=[self.d_grow], writes=[grow])
        P.dma("sp", wrg[:], self.d_wr.h.rearrange("p (e d) -> p e d", e=NEXP), wrg, reads=[self.d_wr], writes=[wrg])
        P.dma("sp", brt[:], self.d_br[:], brt, reads=[self.d_br], writes=[brt])
        P.dma("sp", ebase[:], self.d_ebase[:], ebase, reads=[self.d_ebase], writes=[ebase])
        for e_ in range(NEXP):
            P.c("pool", lambda e, e_=e_: e.tensor_tensor(out=wrg[:, e_, :], in0=wrg[:, e_, :], in1=grow[:], op=ALU.mult),
                reads=[wrg, grow], writes=[wrg])
        P.c("pool", lambda e: e.memset(zt[:], 0.0), writes=[zt])
        P.c("pool", lambda e: e.memset(selcum[1][:], 0.0), writes=[selcum[1]])
        for a in range(NEXP * CAP // 256):
            P.dma("sp", XS_d.h[a * 256:(a + 1) * 256, :].rearrange("(a p) n -> p a n", p=128), zt[:], zt, reads=[zt], writes=[XS_d])

        def route(i):
            x_, hb_, s_, sb_ = xt[i % 2], h2b[i % 2], sm[i % 2], selb[i % 2]
            sc_new, sc_old = selcum[i % 2], selcum[(i + 1) % 2]
            col = lambda a, b=None: s_[:, a:(a + 1 if b is None else b)]
            P.dma("sp", x_[:], x1_d[i * 128:(i + 1) * 128, :], x_, reads=[x1_d], writes=[x_])
            P.c("act", lambda e: e.activation(out=junk[:], in_=x_[:], func=AF.Square, accum_out=col(0)), reads=[x_], writes=[junk, s_])
            P.c("act", lambda e: e.activation(out=col(1), in_=col(0), func=AF.Ln, scale=1.0 / D, bias=cst[:, 0:1]), reads=[s_, cst], writes=[s_])
            P.c("act", lambda e: e.activation(out=col(1), in_=col(1), func=AF.Exp, scale=-0.5), reads=[s_], writes=[s_])
            P.c("dve", lambda e: e.scalar_tensor_tensor(out=hb_[:], in0=x_[:], scalar=col(1), in1=grow[:], op0=ALU.mult, op1=ALU.mult),
                reads=[x_, s_, grow], writes=[hb_])
            for e_ in range(NEXP):
                eng = "dve" if e_ % 2 == 0 else "pool"
                eng = "dve"
                P.c(eng, lambda e, e_=e_: e.scalar_tensor_tensor(out=junk[:], in0=x_[:], scalar=col(1), in1=wrg[:, e_, :], op0=ALU.mult,
                                                                 op1=ALU.mult, accum_out=col(8 + e_)), reads=[x_, s_, wrg], writes=[junk, s_])
            lg, eq1, lg2, eq2, dest, tmp8 = col(8, 16), col(16, 24), col(24, 32), col(32, 40), col(40, 48), col(48, 56)
            P.c("dve", lambda e: e.tensor_tensor(out=lg, in0=lg, in1=brt[:], op=ALU.add), reads=[s_, brt], writes=[s_])
            P.c("dve", lambda e: e.reduce_max(out=col(2), in_=lg, axis=AX.X), reads=[s_], writes=[s_])
            P.c("dve", lambda e: e.tensor_scalar(out=eq1, in0=lg, scalar1=col(2), scalar2=None, op0=ALU.is_equal), reads=[s_], writes=[s_])
            P.c("dve", lambda e: e.scalar_tensor_tensor(out=lg2, in0=eq1, scalar=-1e30, in1=lg, op0=ALU.mult, op1=ALU.add), reads=[s_], writes=[s_])
            P.c("dve", lambda e: e.reduce_max(out=col(3), in_=lg2, axis=AX.X), reads=[s_], writes=[s_])
            P.c("dve", lambda e: e.tensor_scalar(out=eq2, in0=lg2, scalar1=col(3), scalar2=None, op0=ALU.is_equal), reads=[s_], writes=[s_])
            P.c("dve", lambda e: e.tensor_scalar(out=col(4), in0=col(2), scalar1=-1.0, scalar2=None, op0=ALU.mult), reads=[s_], writes=[s_])
            P.c("act", lambda e: e.activation(out=col(5), in_=col(3), func=AF.Exp, bias=col(4)), reads=[s_], writes=[s_])
            P.c("dve", lambda e: e.tensor_scalar(out=col(6), in0=col(5), scalar1=1.0, scalar2=None, op0=ALU.add), reads=[s_], writes=[s_])
            P.c("dve", lambda e: e.reciprocal(out=col(6), in_=col(6)), reads=[s_], writes=[s_])
            P.c("dve", lambda e: e.tensor_copy(out=gsm[:, i, 0:1], in_=col(6)), reads=[s_], writes=[gsm])
            P.c("dve", lambda e: e.tensor_tensor(out=gsm[:, i, 1:2], in0=col(5), in1=col(6), op=ALU.mult), reads=[s_], writes=[gsm])
            P.c("dve", lambda e: e.tensor_tensor(out=sb_[:], in0=eq1, in1=eq2, op=ALU.add), reads=[s_], writes=[sb_])
            pc = bank[i % 2]
            P.c("pe", lambda e: e.matmul(pc[:, 0:NEXP], lhsT=self.ones(), rhs=sb_[:], start=True, stop=False), reads=[self.cb, sb_], writes=[pc])
            P.c("pe", lambda e: e.matmul(pc[:, 0:NEXP], lhsT=self.tri(), rhs=sb_[:], start=False, stop=False), reads=[self.cb, sb_], writes=[pc])
            P.c("pe", lambda e: e.matmul(pc[:, 0:NEXP], lhsT=self.ones(), rhs=sc_old[:], start=False, stop=True), reads=[self.cb, sc_old], writes=[pc])
            P.c("dve", lambda e: e.tensor_tensor(out=sc_new[:], in0=sc_old[:], in1=sb_[:], op=ALU.add), reads=[sc_old, sb_], writes=[sc_new])
            P.c("dve", lambda e: e.scalar_tensor_tensor(out=dest, in0=pc[:, 0:NEXP], scalar=float(CAP - 1), in1=ebase[:], op0=ALU.min, op1=ALU.add),
                reads=[pc, ebase], writes=[s_])
            for kk, eq in enumerate((eq1, eq2)):
                P.c("dve", lambda e, eq=eq: e.tensor_tensor(out=tmp8, in0=eq, in1=dest, op=ALU.mult), reads=[s_], writes=[s_])
                P.c("dve", lambda e, kk=kk: e.reduce_sum(out=col(56 + kk), in_=tmp8, axis=AX.X), reads=[s_], writes=[s_])
                P.c("dve", lambda e, kk=kk: e.tensor_copy(out=idxs[:, i, kk:kk + 1], in_=col(56 + kk)), reads=[s_], writes=[idxs])
            for kk in range(2):
                P.idma(XS_d[:, :], bass.IndirectOffsetOnAxis(ap=idxs[:, i, kk:kk + 1], axis=0), hb_[:], None, hb_,
                       reads=[hb_, idxs], writes=[XS_d])

        for i in range(NT):
            route(i)
        P.scope_exit()
        for ex in range(NEXP):
            self.moe_expert(ex, CAP, XS_d, YS_d)
        P.scope_enter()
        xt = [P.sb("xtc", [128, D], F32) for _ in range(2)]
        yg = [[P.sb("yg", [128, D], F32) for _ in range(2)] for _ in range(2)]

        def comb(i):
            x_ = xt[i % 2]
            P.dma("sp", x_[:], x1_d[i * 128:(i + 1) * 128, :], x_, reads=[x1_d], writes=[x_])
            for kk in range(2):
                y_ = yg[kk][i % 2]
                P.idma(y_[:], None, YS_d[:, :], bass.IndirectOffsetOnAxis(ap=idxs[:, i, kk:kk + 1], axis=0), y_,
                       reads=[YS_d, idxs], writes=[y_])
                P.c("dve", lambda e, y_=y_, kk=kk: e.scalar_tensor_tensor(out=x_[:], in0=y_[:], scalar=gsm[:, i, kk:kk + 1], in1=x_[:],
                                                                          op0=ALU.mult, op1=ALU.add), reads=[y_, gsm, x_], writes=[x_])
            P.dma("act", xo_d[i * 128:(i + 1) * 128, :], x_[:], x_, reads=[x_], writes=[xo_d])

        for i in range(NT):
            comb(i)
        P.scope_exit()

    def moe_expert(self, ex, CAP, XS_d, YS_d):
        P, bank = self.P, self.bank
        P.scope_enter()
        xsT = P.sb("xsT", [128, 8, CAP], BF16)
        xr = [P.sb("xr", [128, D], BF16) for _ in range(2)]
        ys = [P.sb("ys", [128, D], F32) for _ in range(2)]
        for j in range(CAP // 128):
            x_ = xr[j % 2]
            r0 = ex * CAP + j * 128
            P.dma("sp", x_[:], XS_d[r0:r0 + 128, :], x_, reads=[XS_d], writes=[x_])
            pt = bank[6 + j % 2]
            ptv = pt[:].bitcast(BF16)
            for c in range(8):
                P.c("pe", lambda e, c=c, x_=x_, ptv=ptv: e.transpose(out=ptv[:, c * 128:(c + 1) * 128], in_=x_[:, c * 128:(c + 1) * 128],
                                                                   identity=self.ident()), reads=[x_, self.cb], writes=[pt])
            eng = "dve" if j % 2 == 0 else "act"
            if eng == "dve":
                P.c("dve", lambda e, j=j, ptv=ptv: e.tensor_copy(out=xsT[:, :, j * 128:(j + 1) * 128], in_=ptv[:, :].rearrange("p (c n) -> p c n", c=8)),
                    reads=[pt], writes=[xsT])
            else:
                P.c("act", lambda e, j=j, ptv=ptv: e.copy(out=xsT[:, :, j * 128:(j + 1) * 128], in_=ptv[:, :].rearrange("p (c n) -> p c n", c=8)),
                    reads=[pt], writes=[xsT])

        def sink(j, half, pm):
            y_ = ys[j % 2]
            if half == 0:
                P.c("act", lambda e: e.copy(out=y_[:, 0:512], in_=pm[:, :]), reads=[pm], writes=[y_])
            else:
                P.c("dve", lambda e: e.tensor_copy(out=y_[:, 512:1024], in_=pm[:, :]), reads=[pm], writes=[y_])
                r0 = ex * CAP + j * 128
                P.dma("act", YS_d[r0:r0 + 128, :], y_[:], y_, reads=[y_], writes=[YS_d])

        self.ffn_core(xsT, 0, CAP, (self.w_eg, self.w_eg.h[0][ex]), (self.w_eu, self.w_eu.h[0][ex]), (self.w_ed, self.w_ed.h[0][ex]), sink)
        P.scope_exit()


def _consts():
    i = np.arange(128)
    ident = np.eye(128, dtype=np.float32)
    tri = -(i[:, None] >= i[None, :]).astype(np.float32)
    negones = -np.ones((128, 128), np.float32)
    ones = np.ones((128, 128), np.float32)
    blockones = ((i[:, None] // 64) == (i[None, :] // 64)).astype(np.float32)
    zeros = np.zeros((128, 128), np.float32)
    sbmask = np.where(i[:, None] >= i[None, :], NEG, 0.0).astype(np.float32)
    cb = np.concatenate([ident, tri, negones, ones, blockones, zeros, sbmask], axis=1).astype(ml_dtypes.bfloat16)
    slopes = 2.0 ** (-8.0 * np.arange(1, 13, dtype=np.float32) / 12.0)
    bt = np.zeros((128, 12, 2, 128), np.float32)
    for H in range(12):
        d = DIL[H // 4]
        for half in range(2):
            dist = (half * 128 + i[None, :] - i[:, None]).astype(np.float32)
            valid = (dist >= 0) & (dist <= 128)
            bt[:, H, half, :] = np.where(valid, -slopes[H] * dist * d, NEG)
    return cb, bt.reshape(128, 12 * 256).astype(np.float32)


def host_inputs(inp):
    L = DEPTH
    f = lambda a: np.ascontiguousarray(np.asarray(a, dtype=np.float32))
    g = np.stack([f(inp["attn_norm_g"]), f(inp["ffn_norm_g"])], axis=1)
    gcols = g.reshape(L, 2, 8, 128).transpose(3, 0, 1, 2).reshape(128, L * 2 * 8)
    qk = np.stack([f(inp["q_norm_g"]), f(inp["k_norm_g"])], axis=1)
    qkg = np.concatenate([qk, qk], axis=2).transpose(2, 0, 1).reshape(128, L * 2)
    qkrow = np.broadcast_to(qk.reshape(1, L * 2 * 64), (128, L * 2 * 64))
    cw = f(inp["conv_w"])
    convw = cw.reshape(L, 31, 4, 128).transpose(3, 0, 2, 1).reshape(128, L * 4 * 31)
    cp = np.stack([f(inp["conv_b"]), f(inp["conv_norm_g"]), f(inp["conv_norm_b"])], axis=1)
    convp = cp.reshape(L, 3, 4, 128).transpose(3, 0, 1, 2).reshape(128, L * 3 * 4)
    wr = f(inp["w_router"])[0]
    wr_bc = np.broadcast_to(wr.T.reshape(1, NEXP * D), (128, NEXP * D))
    br_bc = np.broadcast_to(f(inp["b_router"])[0].reshape(1, NEXP), (128, NEXP))
    cb, bt = _consts()
    cap = 1536
    grow = np.broadcast_to(f(inp["ffn_norm_g"])[1].reshape(1, D), (128, D))
    ebase = np.broadcast_to((np.arange(NEXP, dtype=np.float32) * cap).reshape(1, NEXP), (128, NEXP))
    m = dict(grow=grow, ebase=ebase, gcols=gcols, qkg=qkg, qkrow=qkrow, convw=convw, convp=convp, wr_bc=wr_bc, br_bc=br_bc, cbf=cb, btab=bt)
    m = {k: np.ascontiguousarray(v) for k, v in m.items()}
    for k in ("w_in", "w_branch_a", "w_branch_b", "w_branch_c", "w_out", "w_ffn_gate", "w_ffn_up", "w_ffn_down",
              "w_exp_gate", "w_exp_up", "w_exp_down"):
        m[k] = f(inp[k])
    return m


_CACHE = {}


def kernel(**inputs):
    m = host_inputs(inputs)
    x = np.asarray(inputs["x"], dtype=np.float32)
    nb = x.shape[0]
    if "k" not in _CACHE:
        kk = K()
        kk.build_full()
        _CACHE["k"] = kk
    kk = _CACHE["k"]
    in_maps = []
    for b in range(nb):
        mm = dict(m)
        mm["x"] = np.ascontiguousarray(x[b])
        in_maps.append(mm)
    res = run_bass_kernel_spmd(kk.nc, in_maps, core_ids=list(range(nb)))
    return np.stack([np.asarray(r["out"], dtype=np.float32) for r in res.results], axis=0)
```

```python
import numpy as np
import ml_dtypes
import concourse.bass as bass
import concourse.mybir as mybir
from concourse.bass_utils import run_bass_kernel_spmd

F32 = mybir.dt.float32
BF16 = mybir.dt.bfloat16
I32 = mybir.dt.int32
ALU = mybir.AluOpType
AF = mybir.ActivationFunctionType
AX = mybir.AxisListType

D = 1024
SEQ = 4096
DEPTH = 2
IN_COLS = 7936
DFF = 2816
NF = DFF // 128
NEXP = 8
DIL = (1, 4, 16)
QA, KA, VA = 0, 768, 1536
QB, KB_, VB = 2304, 2816, 3328
GV, GG = 3840, 4352
GATE0 = 4864
NEG = -30000.0


class Tl:
    _n = 0

    def __init__(self, h, name):
        self.h = h
        self.name = name
        self.w = set()
        self.r = set()
        Tl._n += 1
        self.id = Tl._n
        self.born = 0
        self.freed = None

    def __getitem__(self, idx):
        return self.h[idx]


class Prog:
    STREAMS = ("pe", "act", "dve", "pool", "sp")

    def __init__(self, nc):
        self.nc = nc
        self.ops = []
        self.streams = {s: [] for s in self.STREAMS}
        self.semorder = {}
        self.n_t = 0
        self.tiles = {}
        self.ghost = {}
        self.scopes = []

    def sb(self, name, shape, dt):
        self.n_t += 1
        t = Tl(self.nc.alloc_sbuf_tensor(f"{name}_{self.n_t}", list(shape), dt), name)
        t.w = set(self.ghost.values())
        t.born = len(self.ops)
        self.tiles[t.id] = t
        if self.scopes:
            self.scopes[-1][1].append(t)
        return t

    def scope_enter(self):
        g = self.nc.reset_on_exit()
        g.__enter__()
        self.scopes.append((g, []))

    def scope_exit(self):
        g, tiles = self.scopes.pop()
        g.__exit__(None, None, None)
        for t in tiles:
            t.freed = len(self.ops)
            for oid in (t.w | t.r):
                op = self.ops[oid]
                k = op["semkey"]
                if k not in self.ghost or self.ops[self.ghost[k]]["pos"] < op["pos"]:
                    self.ghost[k] = oid

    def ps(self, name, shape, dt=F32):
        self.n_t += 1
        t = Tl(self.nc.alloc_psum_tensor(f"{name}_{self.n_t}", list(shape), dt), name)
        t.excl = True
        return t

    def dram(self, name, shape, dt, kind="Internal"):
        return Tl(self.nc.dram_tensor(name, list(shape), dt, kind=kind), name)

    def _add(self, stream, fn, reads, writes, semkey, inc):
        oid = len(self.ops)
        raw = set()
        rar = set()
        for t in reads:
            raw |= t.w
            if getattr(t, "excl", False):
                rar |= t.r
        deps = set(raw) | rar
        waw, war = set(), set()
        for t in writes:
            waw |= t.w
            war |= t.r
        deps |= waw | war
        is_dma = inc == 16
        keep = set()
        for d in deps:
            od = self.ops[d]
            if od["semkey"] == semkey and d not in raw:
                if not is_dma:
                    continue
                if d in waw and d not in war:
                    continue
            keep.add(d)
        op = dict(id=oid, stream=stream, fn=fn, deps=keep, semkey=semkey, inc=inc)
        self.ops.append(op)
        self.streams[stream].append(oid)
        self.semorder.setdefault(semkey, []).append(oid)
        op["pos"] = len(self.semorder[semkey]) - 1
        for t in reads:
            t.r.add(oid)
        for t in writes:
            t.w = {oid}
            t.r = set()
        return oid

    def c(self, eng, fn, reads=(), writes=()):
        return self._add(eng, fn, reads, writes, eng, 1)

    def dma(self, stream, out_ap, in_ap, semtile, reads=(), writes=(), **kw):
        fn = lambda e: e.dma_start(out=out_ap, in_=in_ap, **kw)
        return self._add(stream, fn, reads, writes, ("dma", semtile.id, stream == "pool"), 16)

    def idma(self, out_ap, out_off, in_ap, in_off, semtile, reads=(), writes=()):
        fn = lambda e: e.indirect_dma_start(out=out_ap, out_offset=out_off, in_=in_ap, in_offset=in_off)
        return self._add("pool", fn, reads, writes, ("dma", semtile.id, True), 16)

    def finish(self, stream, tiles):
        oid = self._add(stream, None, list(tiles), [], ("fin", stream), 0)
        for k, lst in self.semorder.items():
            if not isinstance(k, str) and k[0] == "dma":
                self.ops[oid]["deps"].add(lst[-1])
        return oid

    def emit(self):
        nc = self.nc
        ops = self.ops
        seen = {s: {} for s in self.STREAMS}
        needed = {}
        for op in ops:
            waits = {}
            for d in op["deps"]:
                od = ops[d]
                k = od["semkey"]
                if od["pos"] > waits.get(k, -1):
                    waits[k] = od["pos"]
            sn = seen[op["stream"]]
            w2 = []
            for k, p in waits.items():
                if sn.get(k, -1) >= p:
                    continue
                sn[k] = p
                needed.setdefault(k, set()).add(p)
                w2.append((k, p))
            op["waits"] = w2
        semval = {}
        sems = {}
        for k, lst in self.semorder.items():
            if not isinstance(k, str) or k not in needed:
                continue
            sems[k] = nc.alloc_semaphore("s_%s" % k)
            cnt = 0
            nd = needed[k]
            for p, oid in enumerate(lst):
                if p in nd:
                    cnt += 1
                    ops[oid]["do_inc"] = True
                semval[(k, p)] = cnt
        INF = 1 << 60
        dkeys = [k for k in self.semorder if not isinstance(k, str) and k[0] == "dma"]
        dkeys.sort(key=lambda k: self.tiles[k[1]].born)
        pools = {True: [], False: []}
        nphys = 0
        for k in dkeys:
            tl = self.tiles[k[1]]
            ent = None
            for cand in pools[k[2]]:
                if cand[2] <= tl.born:
                    ent = cand
                    break
            if ent is None:
                ent = [nc.alloc_semaphore("s_d%d" % nphys), 0, INF]
                nphys += 1
                pools[k[2]].append(ent)
            sems[k] = ent[0]
            base = ent[1]
            lst = self.semorder[k]
            for p, oid in enumerate(lst):
                ops[oid]["do_inc"] = True
                semval[(k, p)] = base + 16 * (p + 1)
            ent[1] = base + 16 * len(lst)
            ent[2] = tl.freed if tl.freed is not None else INF
        self.n_sems = len(sems)
        self.max_semval = max(semval.values()) if semval else 0
        engmap = dict(pe="tensor", act="scalar", dve="vector", pool="gpsimd", sp="sync")

        def run_stream(s):
            def body(e):
                for oid in self.streams[s]:
                    op = ops[oid]
                    for k, p in op["waits"]:
                        e.wait_ge(sems[k], semval[(k, p)])
                    if op["fn"] is None:
                        continue
                    ins = op["fn"](e)
                    if op.get("do_inc"):
                        ins.then_inc(sems[op["semkey"]], op["inc"])
            return body

        with nc.Block() as block:
            for s in self.STREAMS:
                if self.streams[s]:
                    getattr(block, engmap[s])(run_stream(s))


def sl(start, count, step=1):
    return slice(start, start + (count - 1) * step + 1, step)


def rr(gens):
    gens = list(gens)
    while gens:
        nxt = []
        for g in gens:
            try:
                next(g)
                nxt.append(g)
            except StopIteration:
                pass
        gens = nxt


class K:
    def __init__(self, T=SEQ, cfg=None):
        self.T = T
        self.cfg = cfg or {}
        self.nc = bass.Bass("TRN2", target_bir_lowering=False)
        self.P = Prog(self.nc)
        self.NT = T // 128
        self.NG = T // 512
        self._wq = 0
        self.setup()

    def setup(self):
        P, T = self.P, self.T
        L = DEPTH
        DFF = self.dff = self.cfg.get("dff", 2816)
        ein = lambda n, s, d: P.dram(n, s, d, kind="ExternalInput")
        self.x_in = ein("x", [T, D], F32)
        self.w_in = ein("w_in", [L, D, IN_COLS], F32)
        self.w_ba = ein("w_branch_a", [L, 256, D], F32)
        self.w_bb = ein("w_branch_b", [L, 512, D], F32)
        self.w_bc = ein("w_branch_c", [L, 512, D], F32)
        self.w_out = ein("w_out", [L, D, D], F32)
        self.w_fg = ein("w_ffn_gate", [1, D, DFF], F32)
        self.w_fu = ein("w_ffn_up", [1, D, DFF], F32)
        self.w_fd = ein("w_ffn_down", [1, DFF, D], F32)
        if self.cfg.get("moe", True):
            self.w_eg = ein("w_exp_gate", [1, NEXP, D, DFF], F32)
            self.w_eu = ein("w_exp_up", [1, NEXP, D, DFF], F32)
            self.w_ed = ein("w_exp_down", [1, NEXP, DFF, D], F32)
        self.d_gcols = ein("gcols", [128, L * 2 * 8], F32)
        self.d_qkg = ein("qkg", [128, L * 2], F32)
        self.d_qkrow = ein("qkrow", [128, L * 2 * 64], F32)
        self.d_convw = ein("convw", [128, L * 4 * 31], F32)
        self.d_convp = ein("convp", [128, L * 3 * 4], F32)
        self.d_wr = ein("wr_bc", [128, NEXP * D], F32)
        self.d_br = ein("br_bc", [128, NEXP], F32)
        self.d_grow = ein("grow", [128, D], F32)
        self.d_ebase = ein("ebase", [128, NEXP], F32)
        self.d_cb = ein("cbf", [128, 7 * 128], BF16)
        self.d_bt = ein("btab", [128, 12 * 256], F32)
        self.out = P.dram("out", [T, D], F32, kind="ExternalOutput")
        self.xs = [P.dram("xs0", [T, D], F32), P.dram("xs1", [T, D], F32)]
        self.mT_d = P.dram("mT_d", [D, T], BF16)
        self.oT_d = P.dram("oT_d", [1280, T], BF16, kind="ExternalOutput" if self.cfg.get("dbg_oT") else "Internal")
        self.dbg = {}

        def ld(name, d, shape, dt):
            t = P.sb(name, shape, dt)
            P.dma("sp", t[:], d[:], t, reads=[d], writes=[t])
            return t

        self.gcols = ld("gcols", self.d_gcols, [128, L * 2 * 8], F32)
        self.qkg = ld("qkg", self.d_qkg, [128, L * 2], F32)
        self.qkrow = ld("qkrow", self.d_qkrow, [128, L * 2 * 64], F32)
        self.convw = ld("convw", self.d_convw, [128, L * 4 * 31], F32)
        self.convp = ld("convp", self.d_convp, [128, L * 3 * 4], F32)
        self.cb = ld("cb", self.d_cb, [128, 7 * 128], BF16)
        self.bt = ld("bt", self.d_bt, [128, 12 * 256], F32)
        cbv = lambda i: (lambda: self.cb[:, i * 128:(i + 1) * 128])
        self.ident, self.tri, self.negones, self.ones, self.blockones, self.zeros, self.sbmask = [cbv(i) for i in range(7)]
        self.cst = P.sb("cst", [128, 4], F32)
        P.c("pool", lambda e: e.memset(self.cst[:, 0:1], 1e-6), writes=[self.cst])
        P.c("pool", lambda e: e.memset(self.cst[:, 1:2], 1.0), writes=[self.cst])
        P.c("pool", lambda e: e.memset(self.cst[:, 2:3], 0.0), writes=[self.cst])
        self.bank = [P.ps("bank%d" % i, [128, 512], F32) for i in range(8)]
        self.wbufs = [P.sb("wb", [128, 8, 128], BF16) for _ in range(6)]

    def build_full(self, n_layers=DEPTH):
        P = self.P
        x_cur = self.x_in
        for l in range(n_layers):
            P.scope_enter()
            self.hT = P.sb("hT", [128, 8, self.T], BF16)
            self.phase_norm(x_cur, (l * 2 + 0) * 8)
            self.phase_sb(l)
            self.phase_dil(l)
            self.phase_conv(l)
            x1_d = self.xs[0]
            self.phase_merge(l, x_cur, x1_d)
            xo = self.out if l == n_layers - 1 else self.xs[1]
            if l % 2 == 0:
                self.phase_norm(x1_d, (l * 2 + 1) * 8)
                self.phase_ffn_dense(x1_d, xo)
                P.scope_exit()
            else:
                P.scope_exit()
                self.phase_moe(l, x1_d, xo)
            x_cur = xo
        P.finish("sp", [self.out])
        P.emit()

    def dbg_out(self, name, shape, dt=F32):
        t = self.P.dram(name, shape, dt, kind="ExternalOutput")
        self.dbg[name] = t
        return t

    def wchunk(self, src_tl, src_ap):
        wb = self.wbufs[self._wq % len(self.wbufs)]
        self._wq += 1
        self.P.dma("pool", wb[:], src_ap, wb, reads=[src_tl], writes=[wb])
        return wb

    def win_cols(self, l, c0, n=128):
        return self.w_in.h[l].rearrange("(c p) n -> p c n", p=128)[:, :, c0:c0 + n]

    def phase_norm(self, x_d, gcol_off):
        P, T = self.P, self.T
        P.scope_enter()
        na = dict(
            xt=[P.sb("xt", [128, D], F32) for _ in range(2)],
            sq=P.sb("sq", [128, D], F32),
            ss=[P.sb("ss", [128, 1], F32) for _ in range(2)],
            rs=[P.sb("rs", [128, 1], F32) for _ in range(2)],
            hb=[P.sb("hb", [128, D], BF16) for _ in range(2)],
        )
        hT, gcols, cst = self.hT, self.gcols, self.cst
        for i in range(self.NT):
            b = i % 2
            xt, ss, rs, hb, sq = na["xt"][b], na["ss"][b], na["rs"][b], na["hb"][b], na["sq"]
            P.dma("sp", xt[:], x_d[i * 128:(i + 1) * 128, :], xt, reads=[x_d], writes=[xt])
            P.c("act", lambda e, xt=xt, ss=ss: e.activation(out=sq[:], in_=xt[:], func=AF.Square, accum_out=ss[:]),
                reads=[xt], writes=[sq, ss])
            P.c("act", lambda e, ss=ss, rs=rs: e.activation(out=rs[:], in_=ss[:], func=AF.Ln, scale=1.0 / D, bias=cst[:, 0:1]),
                reads=[ss, cst], writes=[rs])
            P.c("act", lambda e, rs=rs: e.activation(out=rs[:], in_=rs[:], func=AF.Exp, scale=-0.5),
                reads=[rs], writes=[rs])
            P.c("dve", lambda e, xt=xt, rs=rs, hb=hb: e.tensor_scalar(out=hb[:], in0=xt[:], scalar1=rs[:, 0:1], scalar2=None,
                                                                   op0=ALU.mult), reads=[xt, rs], writes=[hb])
            pt = self.bank[i % 2]
            ptv = pt[:].bitcast(BF16)
            for c in range(8):
                P.c("pe", lambda e, ptv=ptv, c=c, hb=hb: e.transpose(out=ptv[:, c * 128:(c + 1) * 128],
                                                                     in_=hb[:, c * 128:(c + 1) * 128], identity=self.ident()),
                    reads=[hb, self.cb], writes=[pt])
            for c in range(8):
                eng = "dve" if c % 2 == 0 else "pool"
                eng = "dve"
                P.c(eng, lambda e, ptv=ptv, c=c, i=i: e.tensor_scalar(
                    out=hT[:, c, i * 128:(i + 1) * 128], in0=ptv[:, c * 128:(c + 1) * 128],
                    scalar1=gcols[:, gcol_off + c:gcol_off + c + 1], scalar2=None, op0=ALU.mult),
                    reads=[pt, gcols], writes=[hT])
        P.scope_exit()

    def proj_fm(self, wb, pm, tg, wcol0=0, m=128):
        P, hT = self.P, self.hT
        for c in range(8):
            P.c("pe", lambda e, c=c: e.matmul(pm[0:m, :], lhsT=wb[:, c, wcol0:wcol0 + m], rhs=hT[:, c, tg * 512:(tg + 1) * 512],
                                              start=(c == 0), stop=(c == 7)), reads=[wb, hT], writes=[pm])

    def phase_sb(self, l):
        P, T, NT, NG = self.P, self.T, self.NT, self.NG
        hT, bank = self.hT, self.bank
        P.scope_enter()
        if True:
            NCH = 3
            self.sbb = dict(
                vall=P.sb("vall", [128, NT, 512], BF16),
                wv=P.sb("wv", [128, 8, 512], BF16),
                qT=P.sb("qT", [128, T], BF16),
                kT=P.sb("kT", [128, T], BF16),
                obT=P.sb("obT", [128, T], BF16),
                ch=[dict(e=P.sb("e", [128, 512], F32),
                         sp=[P.sb("sp", [128, 512], BF16) for _ in range(2)],
                         a=[P.sb("a", [128, 512], BF16) for _ in range(2)],
                         r32=P.sb("r32", [128, 512], F32),
                         rb=[P.sb("rb", [128, 512], BF16) for _ in range(2)],
                         zb=bank[2 + 2 * i], ob=bank[3 + 2 * i]) for i in range(NCH)],
            )
        S = self.sbb
        vall, wv, qT, kT, obT = S["vall"], S["wv"], S["qT"], S["kT"], S["obT"]
        cst = self.cst
        P.dma("pool", wv[:], self.win_cols(l, VB, 512), wv, reads=[self.w_in], writes=[wv])
        for i in range(NT):
            pm = bank[i % 2]
            for c in range(8):
                P.c("pe", lambda e, c=c, i=i, pm=pm: e.matmul(pm[:, :], lhsT=hT[:, c, i * 128:(i + 1) * 128], rhs=wv[:, c, :],
                                                             start=(c == 0), stop=(c == 7)), reads=[hT, wv], writes=[pm])
            if i % 2 == 0:
                P.c("act", lambda e, i=i, pm=pm: e.copy(out=vall[:, i, :], in_=pm[:, :]), reads=[pm], writes=[vall])
            else:
                P.c("dve", lambda e, i=i, pm=pm: e.tensor_copy(out=vall[:, i, :], in_=pm[:, :]), reads=[pm], writes=[vall])

        def chain(h, Q, C):
            prow = (h % 2) * 64
            hp = h // 2
            zb, ob, esb, r32 = C["zb"], C["ob"], C["e"], C["r32"]
            rows = slice(prow, prow + 64)
            q0 = Q * 512
            P.c("pe", lambda e: e.matmul(ob[:, :], lhsT=self.zeros(), rhs=qT[:, q0:q0 + 512], start=True, stop=False),
                reads=[self.cb, qT], writes=[ob])
            P.c("pool", lambda e: e.memset(r32[:], 0.0), writes=[r32])
            yield
            kbs = list(range(4 * Q + 3, -1, -1))
            for ti, kb in enumerate(kbs):
                j = max(kb - 4 * Q, 0)
                c0 = 128 * j
                diag = kb >= 4 * Q
                first = ti == 0
                last = kb == 0
                sp, a, rb = C["sp"][ti % 2], C["a"][ti % 2], C["rb"][ti % 2]
                rbp = C["rb"][(ti + 1) % 2]
                P.c("pe", lambda e, kb=kb, c0=c0: e.matmul(zb[:, c0:512], lhsT=kT[rows, kb * 128:(kb + 1) * 128],
                                                           rhs=qT[rows, q0 + c0:q0 + 512], start=True, stop=False, skip_group_check=True),
                    reads=[kT, qT], writes=[zb])
                if diag:
                    P.c("pe", lambda e, c0=c0: e.matmul(zb[:, c0:c0 + 128], lhsT=self.ident(), rhs=self.sbmask(),
                                                        start=False, stop=False, skip_group_check=True), reads=[self.cb], writes=[zb])
                yield
                P.c("act", lambda e, c0=c0: e.activation(out=esb[:, c0:512], in_=zb[:, c0:512], func=AF.Exp),
                    reads=[zb], writes=[esb])
                yield
                P.c("act", lambda e, c0=c0, sp=sp: e.activation(out=sp[:, c0:512], in_=esb[:, c0:512], func=AF.Ln, bias=cst[:, 1:2]),
                    reads=[esb, cst], writes=[sp])
                yield
                P.c("pe", lambda e, c0=c0, sp=sp, first=first: e.matmul(zb[:, c0:512], lhsT=self.tri(), rhs=sp[:, c0:512],
                                                                        start=False, stop=first, skip_group_check=True),
                    reads=[self.cb, sp], writes=[zb])
                if not first:
                    P.c("pe", lambda e, c0=c0, rbp=rbp: e.matmul(zb[:, c0:512], lhsT=self.negones(), rhs=rbp[:, c0:512],
                                                                 start=False, stop=True, skip_group_check=True),
                        reads=[self.cb, rbp], writes=[zb])
                if not last:
                    P.c("dve", lambda e, c0=c0, sp=sp: e.tensor_tensor(out=r32[:, c0:512], in0=r32[:, c0:512], in1=sp[:, c0:512],
                                                                       op=ALU.add), reads=[r32, sp], writes=[r32])
                    P.c("dve", lambda e, rb=rb: e.tensor_copy(out=rb[:, :], in_=r32[:, :]), reads=[r32], writes=[rb])
                yield
                P.c("act", lambda e, c0=c0, a=a: e.activation(out=a[:, c0:512], in_=zb[:, c0:512], func=AF.Exp),
                    reads=[zb], writes=[a])
                yield
                P.c("pe", lambda e, c0=c0, a=a, kb=kb, last=last: e.matmul(ob[:, c0:512], lhsT=vall[:, kb, hp * 128:(hp + 1) * 128],
                                                                          rhs=a[:, c0:512], start=False, stop=last),
                    reads=[vall, a], writes=[ob])
                yield
            P.c("dve", lambda e: e.tensor_copy(out=obT[rows, q0:q0 + 512], in_=ob[rows, :]), reads=[ob], writes=[obT])
            yield

        for hp in range(4):
            wq = self.wchunk(self.w_in, self.win_cols(l, QB + hp * 128))
            wk = self.wchunk(self.w_in, self.win_cols(l, KB_ + hp * 128))
            for tg in range(NG):
                pm = bank[0]
                self.proj_fm(wq, pm, tg)
                P.c("act", lambda e, tg=tg, pm=pm: e.mul(out=qT[:, tg * 512:(tg + 1) * 512], in_=pm[:, :], mul=0.125),
                    reads=[pm], writes=[qT])
                pm = bank[1]
                self.proj_fm(wk, pm, tg)
                P.c("dve", lambda e, tg=tg, pm=pm: e.tensor_copy(out=kT[:, tg * 512:(tg + 1) * 512], in_=pm[:, :]),
                    reads=[pm], writes=[kT])
            jobs = [(2 * hp + hh, Q) for Q in range(NG - 1, -1, -1) for hh in range(2)]
            nch = len(S["ch"])
            slots = [[] for _ in range(nch)]
            for ji, jb in enumerate(jobs):
                slots[ji % nch].append(jb)

            def slot_gen(si):
                for (h, Q) in slots[si]:
                    yield from chain(h, Q, S["ch"][si])
            rr([slot_gen(si) for si in range(nch)])
            P.dma("sp", self.oT_d[256 + hp * 128:256 + (hp + 1) * 128, :], obT[:, :], obT, reads=[obT], writes=[self.oT_d])
        P.scope_exit()

    def phase_dil(self, l):
        P, T, NT, NG = self.P, self.T, self.NT, self.NG
        hT, bank, cst = self.hT, self.bank, self.cst
        P.scope_enter()
        qn = P.sb("qn", [128, T], BF16)
        kn = P.sb("kn", [128, T], BF16)
        vp = P.sb("vp", [128, NT, 128], BF16)
        vT = P.sb("vT", [128, T], BF16)
        acc = P.sb("acc", [128, 2, T], F32)
        raw = [P.sb("raw", [128, 512], F32) for _ in range(2)]
        sq = [P.sb("sqd", [128, 512], BF16) for _ in range(2)]
        rst = [P.sb("rst", [128, 512], F32) for _ in range(2)]
        tmp = [P.sb("tmpd", [128, 512], F32) for _ in range(2)]
        pT = [P.sb("pT", [128, 512], BF16) for _ in range(3)]
        oa = P.sb("oa", [128, T], BF16)
        sm = P.sb("smalld", [128, 136], F32)
        bt = P.sb("bt", [128, 12 * 256], F32)
        P.dma("sp", bt[:], self.d_bt[:], bt, reads=[self.d_bt], writes=[bt])
        qkrow, qkg = self.qkrow, self.qkg
        P.c("dve", lambda e: e.tensor_tensor(out=sm[:, 0:128], in0=qkrow[:, l * 128:(l + 1) * 128], in1=qkrow[:, l * 128:(l + 1) * 128],
                                             op=ALU.mult), reads=[qkrow], writes=[sm])
        P.c("dve", lambda e: e.reduce_max(out=sm[:, 128:129], in_=sm[:, 0:64], axis=AX.X), reads=[sm], writes=[sm])
        P.c("dve", lambda e: e.reduce_max(out=sm[:, 129:130], in_=sm[:, 64:128], axis=AX.X), reads=[sm], writes=[sm])
        P.c("dve", lambda e: e.tensor_tensor(out=sm[:, 130:131], in0=sm[:, 128:129], in1=sm[:, 129:130], op=ALU.add),
            reads=[sm], writes=[sm])
        P.c("dve", lambda e: e.tensor_scalar(out=sm[:, 130:131], in0=sm[:, 130:131], scalar1=-4.0, scalar2=None, op0=ALU.mult),
            reads=[sm], writes=[sm])
        P.c("dve", lambda e: e.tensor_scalar(out=sm[:, 131:132], in0=qkg[:, 2 * l:2 * l + 1], scalar1=0.125, scalar2=None, op0=ALU.mult),
            reads=[qkg], writes=[sm])
        negc = lambda: sm[:, 130:131]
        g8 = lambda: sm[:, 131:132]
        gk = lambda: qkg[:, 2 * l + 1:2 * l + 2]
        cnt = [0]
        def group(s, g):
            if True:
                d = DIL[g]
                nb = T // (128 * d)
                H = 4 * g + 2 * s
                wq = self.wchunk(self.w_in, self.win_cols(l, QA + H * 64))
                wk = self.wchunk(self.w_in, self.win_cols(l, KA + H * 64))
                wvv = self.wchunk(self.w_in, self.win_cols(l, VA + H * 64))
                if self.cfg.get("dil_stop", 99) <= 0.1:
                    return
                for (w, dst, gain) in ((wq, qn, g8), (wk, kn, gk)):
                    for tg in range(NG):
                        i2 = cnt[0] % 2
                        cnt[0] += 1
                        pm, pm2 = bank[i2], bank[2 + i2]
                        rw, sqq, rs_ = raw[i2], sq[i2], rst[i2]
                        self.proj_fm(w, pm, tg)
                        P.c("dve", lambda e, pm=pm, rw=rw: e.tensor_copy(out=rw[:], in_=pm[:, :]), reads=[pm], writes=[rw])
                        P.c("act", lambda e, pm=pm, sqq=sqq: e.activation(out=sqq[:], in_=pm[:, :], func=AF.Square), reads=[pm], writes=[sqq])
                        P.c("pe", lambda e, pm2=pm2, sqq=sqq: e.matmul(pm2[:, :], lhsT=self.blockones(), rhs=sqq[:], start=True, stop=True),
                            reads=[self.cb, sqq], writes=[pm2])
                        P.c("act", lambda e, pm2=pm2, rs_=rs_: e.activation(out=rs_[:], in_=pm2[:, :], func=AF.Ln, scale=1.0 / 64, bias=cst[:, 0:1]),
                            reads=[pm2, cst], writes=[rs_])
                        P.c("act", lambda e, rs_=rs_: e.activation(out=rs_[:], in_=rs_[:], func=AF.Exp, scale=-0.5), reads=[rs_], writes=[rs_])
                        if self.cfg.get("dil_stop", 99) <= 0.5:
                            continue
                        P.c("dve", lambda e, dst=dst, tg=tg, rw=rw, rs_=rs_, gain=gain: e.scalar_tensor_tensor(
                            out=dst[:, :].rearrange("p (r i) -> p r i", r=d)[:, :, tg * (512 // d):(tg + 1) * (512 // d)],
                            in0=rw[:].rearrange("p (i r) -> p r i", r=d), scalar=gain(),
                            in1=rs_[:].rearrange("p (i r) -> p r i", r=d), op0=ALU.mult, op1=ALU.mult),
                            reads=[rw, rs_, sm, qkg], writes=[dst])
                if self.cfg.get("dil_stop", 99) <= 1:
                    return
                for tg in range(NG):
                    pm = bank[tg % 2]
                    self.proj_fm(wvv, pm, tg)
                    P.c("act", lambda e, pm=pm, tg=tg: e.copy(
                        out=vT[:, :].rearrange("p (r i) -> p r i", r=d)[:, :, tg * (512 // d):(tg + 1) * (512 // d)],
                        in_=pm[:, :].rearrange("p (i r) -> p r i", r=d)), reads=[pm], writes=[vT])
                for b in range(NT):
                    pm = bank[4 + (b // 4) % 2]
                    pmv = pm[:].bitcast(BF16)
                    P.c("pe", lambda e, pmv=pmv, b=b: e.transpose(out=pmv[:, (b % 4) * 128:(b % 4 + 1) * 128], in_=vT[:, b * 128:(b + 1) * 128],
                                                                 identity=self.ident()), reads=[vT, self.cb], writes=[pm])
                    if b % 4 == 3:
                        P.c("dve", lambda e, pmv=pmv, b=b: e.tensor_copy(out=vp[:, b - 3:b + 1, :],
                                                                        in_=pmv[:, 0:512].rearrange("p (a n) -> p a n", a=4)),
                            reads=[pm], writes=[vp])
                if self.cfg.get("dil_stop", 99) <= 2:
                    return
                def head(hh):
                    rows = slice(hh * 64, hh * 64 + 64)
                    Hh = H + hh
                    btH = lambda Hh=Hh: bt[:, Hh * 256:(Hh + 1) * 256]
                    batches = [(r, mb) for r in range(d) for mb in range(nb // 2)]

                    def stage1(bi):
                        r, mb = batches[bi]
                        sbk = bank[4 + bi % 2]
                        tm = tmp[bi % 2]
                        pt = pT[bi % 3]
                        width = 0
                        for i in range(2):
                            m = 2 * mb + i
                            nq = 256 if m < nb - 1 else 128
                            P.c("pe", lambda e, i=i, m=m, nq=nq: e.matmul(
                                sbk[:, i * 256:i * 256 + nq], lhsT=kn[rows, (r * nb + m) * 128:(r * nb + m + 1) * 128],
                                rhs=qn[rows, (r * nb + m) * 128:(r * nb + m) * 128 + nq], start=True, stop=True), reads=[kn, qn], writes=[sbk])
                            P.c("dve", lambda e, i=i, nq=nq: e.tensor_tensor(out=tm[:, i * 256:i * 256 + nq], in0=sbk[:, i * 256:i * 256 + nq],
                                                                            in1=btH()[:, 0:nq], op=ALU.add), reads=[sbk, bt], writes=[tm])
                            width = i * 256 + nq
                        P.c("act", lambda e, width=width: e.activation(out=pt[:, 0:width], in_=tm[:, 0:width], func=AF.Exp, bias=negc()),
                            reads=[tm, sm], writes=[pt])

                    def stage2(bi):
                        r, mb = batches[bi]
                        ub = bank[6 + bi % 2]
                        pt = pT[bi % 3]
                        ptp = pT[(bi - 1) % 3]
                        m0, m1 = 2 * mb, 2 * mb + 1
                        blk = lambda m: r * nb + m
                        contribs = [[], []]
                        if m0 > 0:
                            contribs[0].append((blk(m0 - 1), ptp, 384))
                        contribs[0].append((blk(m0), pt, 0))
                        contribs[1].append((blk(m0), pt, 128))
                        contribs[1].append((blk(m1), pt, 256))
                        for ni in range(2):
                            for ud in range(2):
                                n = len(contribs[ni])
                                for ci, (kb, ptile, c0) in enumerate(contribs[ni]):
                                    lhs = (lambda kb=kb: vp[:, kb, :]) if ud == 0 else self.ones
                                    P.c("pe", lambda e, lhs=lhs, ptile=ptile, c0=c0, ni=ni, ud=ud, ci=ci, n=n: e.matmul(
                                        ub[:, ud * 256 + ni * 128:ud * 256 + (ni + 1) * 128], lhsT=lhs(), rhs=ptile[:, c0:c0 + 128],
                                        start=(ci == 0), stop=(ci == n - 1)), reads=[vp, self.cb, ptile], writes=[ub])
                        ov = lambda: acc[rows, :, sl(m0 * 128 * d + r, 256, d)]
                        iv = lambda: ub[rows, :].rearrange("p (a n) -> p a n", a=2)
                        if g == 0:
                            P.c("dve", lambda e: e.tensor_copy(out=ov(), in_=iv()), reads=[ub], writes=[acc])
                        else:
                            P.c("dve", lambda e: e.tensor_tensor(out=ov(), in0=iv(), in1=ov(), op=ALU.add), reads=[ub, acc], writes=[acc])

                    for bi in range(len(batches) + 1):
                        if bi < len(batches):
                            stage1(bi)
                        if bi >= 1:
                            stage2(bi - 1)
                for hh in range(2):
                    head(hh)

        for s in range(2):
            for g in range(3):
                group(s, g)
            if self.cfg.get("dil_stop", 99) <= 3:
                continue
            P.c("dve", lambda e: e.reciprocal(out=acc[:, 1, :], in_=acc[:, 1, :]), reads=[acc], writes=[acc])
            P.c("dve", lambda e: e.tensor_tensor(out=oa[:, :], in0=acc[:, 0, :], in1=acc[:, 1, :], op=ALU.mult), reads=[acc], writes=[oa])
            P.dma("sp", self.oT_d[s * 128:(s + 1) * 128, :], oa[:, :], oa, reads=[oa], writes=[self.oT_d])
        P.scope_exit()

    def phase_conv(self, l):
        P, T, NT, NG = self.P, self.T, self.NT, self.NG
        hT, bank, cst = self.hT, self.bank, self.cst
        P.scope_enter()
        uT = P.sb("uT", [128, 4, 32 + T], BF16)
        dg = P.sb("dg", [128, 4, 31, 128], BF16)
        idf = P.sb("idf", [128, 128], F32)
        cv = [P.sb("cv", [128, 512], F32) for _ in range(4)]
        xc = [P.sb("xc", [128, 512], F32) for _ in range(4)]
        sqc = [P.sb("sqc", [128, 512], F32) for _ in range(2)]
        sig = [P.sb("sig", [128, 512], F32) for _ in range(2)]
        rsd = P.sb("rsd", [128, 512], F32)
        ost = [P.sb("ost", [128, 4, 512], BF16) for _ in range(2)]
        onesf = P.sb("onesf", [128, 128], F32)
        convw, convp = self.convw, self.convp
        P.c("pool", lambda e: e.memset(onesf[:], 1.0), writes=[onesf])
        P.c("dve", lambda e: e.tensor_copy(out=idf[:], in_=self.ident()), reads=[self.cb], writes=[idf])
        P.c("pool", lambda e: e.memset(uT[:, :, 0:32], 0.0), writes=[uT])
        for cc in range(4):
            for k in range(31):
                col = (l * 4 + cc) * 31 + k
                P.c("dve", lambda e, cc=cc, k=k, col=col: e.tensor_scalar(out=dg[:, cc, k, :], in0=idf[:], scalar1=convw[:, col:col + 1],
                                                                        scalar2=None, op0=ALU.mult), reads=[idf, convw], writes=[dg])
        for cc in range(4):
            wv_ = self.wchunk(self.w_in, self.win_cols(l, GV + cc * 128))
            wg_ = self.wchunk(self.w_in, self.win_cols(l, GG + cc * 128))
            for tg in range(NG):
                pmv, pmg = bank[tg % 2], bank[2 + tg % 2]
                sg = sig[tg % 2]
                self.proj_fm(wg_, pmg, tg)
                self.proj_fm(wv_, pmv, tg)
                P.c("act", lambda e, pmg=pmg, sg=sg: e.activation(out=sg[:], in_=pmg[:, :], func=AF.Sigmoid), reads=[pmg], writes=[sg])
                P.c("dve", lambda e, pmv=pmv, sg=sg, cc=cc, tg=tg: e.tensor_tensor(out=uT[:, cc, 32 + tg * 512:32 + (tg + 1) * 512], in0=pmv[:, :],
                                                                                 in1=sg[:], op=ALU.mult), reads=[pmv, sg], writes=[uT])
        pcol = lambda which, cc: convp[:, (l * 3 + which) * 4 + cc:(l * 3 + which) * 4 + cc + 1]
        def conv_tile(tg):
            for cc in range(4):
                pm = bank[4 + cc % 2]
                for k in range(31):
                    P.c("pe", lambda e, cc=cc, k=k, pm=pm: e.matmul(pm[:, :], lhsT=dg[:, cc, k, :],
                                                                   rhs=uT[:, cc, 2 + tg * 512 + k:2 + tg * 512 + k + 512],
                                                                   start=(k == 0), stop=(k == 30)), reads=[dg, uT], writes=[pm])
                P.c("act", lambda e, cc=cc, pm=pm: e.activation(out=cv[cc][:], in_=pm[:, :], func=AF.Identity, bias=pcol(0, cc)),
                    reads=[pm, convp], writes=[cv[cc]])
            pmm = bank[6]
            for cc in range(4):
                P.c("pe", lambda e, cc=cc: e.matmul(pmm[:, :], lhsT=onesf[:], rhs=cv[cc][:], start=(cc == 0), stop=(cc == 3)),
                    reads=[onesf, cv[cc]], writes=[pmm])
            for cc in range(4):
                P.c("dve", lambda e, cc=cc: e.scalar_tensor_tensor(out=xc[cc][:], in0=pmm[:, :], scalar=-1.0 / 512, in1=cv[cc][:],
                                                                   op0=ALU.mult, op1=ALU.add), reads=[pmm, cv[cc]], writes=[xc[cc]])
            pmv = bank[7]
            for cc in range(4):
                sq_ = sqc[cc % 2]
                P.c("act", lambda e, cc=cc, sq_=sq_: e.activation(out=sq_[:], in_=xc[cc][:], func=AF.Square), reads=[xc[cc]], writes=[sq_])
                P.c("pe", lambda e, cc=cc, sq_=sq_: e.matmul(pmv[:, :], lhsT=onesf[:], rhs=sq_[:], start=(cc == 0), stop=(cc == 3)),
                    reads=[onesf, sq_], writes=[pmv])
            P.c("act", lambda e: e.activation(out=rsd[:], in_=pmv[:, :], func=AF.Ln, scale=1.0 / 512, bias=cst[:, 0:1]),
                reads=[pmv, cst], writes=[rsd])
            P.c("act", lambda e: e.activation(out=rsd[:], in_=rsd[:], func=AF.Exp, scale=-0.5), reads=[rsd], writes=[rsd])
            os_ = ost[tg % 2]
            for cc in range(4):
                P.c("dve", lambda e, cc=cc: e.tensor_tensor(out=xc[cc][:], in0=xc[cc][:], in1=rsd[:], op=ALU.mult),
                    reads=[xc[cc], rsd], writes=[xc[cc]])
                P.c("dve", lambda e, cc=cc: e.tensor_scalar(out=xc[cc][:], in0=xc[cc][:], scalar1=pcol(1, cc), scalar2=pcol(2, cc),
                                                            op0=ALU.mult, op1=ALU.add), reads=[xc[cc], convp], writes=[xc[cc]])
                P.c("act", lambda e, cc=cc, os_=os_: e.activation(out=os_[:, cc, :], in_=xc[cc][:], func=AF.Silu), reads=[xc[cc]], writes=[os_])
            P.dma("sp", self.oT_d.h[768:1280, tg * 512:(tg + 1) * 512].rearrange("(c p) t -> p c t", p=128), os_[:], os_,
                  reads=[os_], writes=[self.oT_d])

        for tg in range(NG):
            conv_tile(tg)
        P.scope_exit()

    def phase_merge(self, l, x_d, x1_d):
        P, T, NT, NG = self.P, self.T, self.NT, self.NG
        hT, bank = self.hT, self.bank
        P.scope_enter()
        oTt = [P.sb("oTt", [128, 10, 512], BF16) for _ in range(2)]
        sg = [P.sb("sg", [128, 512], F32) for _ in range(3)]
        t1 = [P.sb("t1", [128, 512], F32) for _ in range(3)]
        mst = [P.sb("mst", [128, 512], BF16) for _ in range(2)]
        wbr = [P.sb("wbr", [128, 10, 128], BF16) for _ in range(2)]
        mT_d = self.mT_d
        wsrc = ((self.w_ba, 0, 2), (self.w_bb, 2, 4), (self.w_bc, 6, 4))

        def one(dmc, tg, wb_, wg):
            ot = oTt[tg % 2]
            P.dma("sp", ot[:], self.oT_d.h[:, tg * 512:(tg + 1) * 512].rearrange("(c p) t -> p c t", p=128), ot,
                  reads=[self.oT_d], writes=[ot])
            for b in range(3):
                self.proj_fm(wg[b], bank[b], tg)
                P.c("act", lambda e, b=b: e.activation(out=sg[b][:], in_=bank[b][:, :], func=AF.Sigmoid), reads=[bank[b]], writes=[sg[b]])
            for b, (wt, c0, nchunk) in enumerate(wsrc):
                pm = bank[3 + b]
                for ci in range(nchunk):
                    P.c("pe", lambda e, pm=pm, c0=c0, ci=ci, nchunk=nchunk: e.matmul(pm[:, :], lhsT=wb_[:, c0 + ci, :], rhs=ot[:, c0 + ci, :],
                                                                                   start=(ci == 0), stop=(ci == nchunk - 1)),
                        reads=[wb_, ot], writes=[pm])
                P.c("dve", lambda e, pm=pm, b=b: e.tensor_tensor(out=t1[b][:], in0=pm[:, :], in1=sg[b][:], op=ALU.mult),
                    reads=[pm, sg[b]], writes=[t1[b]])
            P.c("pool", lambda e: e.tensor_tensor(out=t1[0][:], in0=t1[0][:], in1=t1[1][:], op=ALU.add), reads=[t1[0], t1[1]], writes=[t1[0]])
            ms = mst[tg % 2]
            P.c("dve", lambda e, ms=ms: e.tensor_tensor(out=ms[:], in0=t1[0][:], in1=t1[2][:], op=ALU.add), reads=[t1[0], t1[2]], writes=[ms])
            P.dma("act", mT_d[dmc * 128:(dmc + 1) * 128, tg * 512:(tg + 1) * 512], ms[:], ms, reads=[ms], writes=[mT_d])

        for dmc in range(8):
            wb_ = wbr[dmc % 2]
            for (wt, c0, nchunk) in wsrc:
                P.dma("pool", wb_[:, c0:c0 + nchunk, :], wt.h[l].rearrange("(c p) n -> p c n", p=128)[:, :, dmc * 128:(dmc + 1) * 128], wb_,
                      reads=[wt], writes=[wb_])
            wg = [self.wchunk(self.w_in, self.win_cols(l, GATE0 + b * 1024 + dmc * 128)) for b in range(3)]
            for tg in range(NG):
                one(dmc, tg, wb_, wg)
        P.scope_exit()
        P.scope_enter()
        wo = P.sb("wo", [128, 8, 1024], BF16)
        mt = [P.sb("mt", [128, 8, 512], BF16) for _ in range(2)]
        xt = [P.sb("xt2", [128, D], F32) for _ in range(3)]
        for c in range(8):
            P.dma("pool", wo[:, c, :], self.w_out.h[l][c * 128:(c + 1) * 128, :], wo, reads=[self.w_out], writes=[wo])

        def tile(tg, j, mtt):
            i = tg * 4 + j
            x_ = xt[i % 3]
            P.dma("sp", x_[:], x_d[i * 128:(i + 1) * 128, :], x_, reads=[x_d], writes=[x_])
            for half in range(2):
                pm = bank[(2 * i + half) % 4]
                for c in range(8):
                    P.c("pe", lambda e, pm=pm, c=c, half=half: e.matmul(pm[:, :], lhsT=mtt[:, c, j * 128:(j + 1) * 128],
                                                                       rhs=wo[:, c, half * 512:(half + 1) * 512], start=(c == 0), stop=(c == 7)),
                        reads=[mtt, wo], writes=[pm])
                P.c("dve", lambda e, pm=pm, half=half: e.tensor_tensor(out=x_[:, half * 512:(half + 1) * 512], in0=pm[:, :],
                                                                      in1=x_[:, half * 512:(half + 1) * 512], op=ALU.add),
                    reads=[pm, x_], writes=[x_])
            P.dma("act", x1_d[i * 128:(i + 1) * 128, :], x_[:], x_, reads=[x_], writes=[x1_d])

        for tg in range(NG):
            mtt = mt[tg % 2]
            P.dma("sp", mtt[:], mT_d.h[:, tg * 512:(tg + 1) * 512].rearrange("(c p) t -> p c t", p=128), mtt, reads=[mT_d], writes=[mtt])
            for j in range(4):
                tile(tg, j, mtt)
        P.scope_exit()

    def ffn_core(self, xT, xoff, R, wg_src, wu_src, wd_src, sink):
        P, bank = self.P, self.bank
        nf = self.dff // 128
        hid = P.sb("hid", [128, nf, R], BF16)
        wd = [P.sb("wd", [128, nf, 512], BF16) for _ in range(2)]
        sil = [P.sb("sil", [128, 512], F32) for _ in range(2)]
        for half in range(2):
            for f0 in range(0, nf, 11):
                f1 = min(nf, f0 + 11)
                P.dma("pool", wd[half][:, f0:f1, :], wd_src[1][f0 * 128:f1 * 128, half * 512:(half + 1) * 512].rearrange("(f p) n -> p f n", p=128),
                      wd[half], reads=[wd_src[0]], writes=[wd[half]])
        segs = [(r0, min(512, R - r0)) for r0 in range(0, R, 512)]
        cnt = 0
        for f in range(nf):
            wg = self.wchunk(wg_src[0], wg_src[1].rearrange("(c p) n -> p c n", p=128)[:, :, f * 128:(f + 1) * 128])
            wu = self.wchunk(wu_src[0], wu_src[1].rearrange("(c p) n -> p c n", p=128)[:, :, f * 128:(f + 1) * 128])
            for (r0, w) in segs:
                pa, pu = bank[cnt % 2], bank[2 + cnt % 2]
                sl_ = sil[cnt % 2]
                cnt += 1
                for (wt, pm) in ((wg, pa), (wu, pu)):
                    for c in range(8):
                        P.c("pe", lambda e, wt=wt, pm=pm, c=c, r0=r0, w=w: e.matmul(pm[:, 0:w], lhsT=wt[:, c, :], rhs=xT[:, c, xoff + r0:xoff + r0 + w],
                                                                                   start=(c == 0), stop=(c == 7)), reads=[wt, xT], writes=[pm])
                P.c("act", lambda e, pa=pa, sl_=sl_, w=w: e.activation(out=sl_[:, 0:w], in_=pa[:, 0:w], func=AF.Silu), reads=[pa], writes=[sl_])
                P.c("dve", lambda e, pu=pu, sl_=sl_, w=w, r0=r0, f=f: e.tensor_tensor(out=hid[:, f, r0:r0 + w], in0=pu[:, 0:w], in1=sl_[:, 0:w],
                                                                                     op=ALU.mult), reads=[pu, sl_], writes=[hid])
        for j in range(R // 128):
            for half in range(2):
                pm = bank[4 + (2 * j + half) % 4]
                for f in range(nf):
                    P.c("pe", lambda e, pm=pm, f=f, j=j, half=half: e.matmul(pm[:, :], lhsT=hid[:, f, j * 128:(j + 1) * 128], rhs=wd[half][:, f, :],
                                                                            start=(f == 0), stop=(f == nf - 1)), reads=[hid, wd[half]], writes=[pm])
                sink(j, half, pm)

    def phase_ffn_dense(self, x1_d, xo_d):
        P, T = self.P, self.T
        RG = min(1024, T)
        for rg in range(T // RG):
            P.scope_enter()
            xt = [P.sb("xt3", [128, D], F32) for _ in range(3)]

            def sink(j, half, pm, rg=rg, xt=xt):
                i = rg * (RG // 128) + j
                x_ = xt[i % 3]
                if half == 0:
                    P.dma("sp", x_[:], x1_d[i * 128:(i + 1) * 128, :], x_, reads=[x1_d], writes=[x_])
                P.c("dve", lambda e: e.tensor_tensor(out=x_[:, half * 512:(half + 1) * 512], in0=pm[:, :],
                                                     in1=x_[:, half * 512:(half + 1) * 512], op=ALU.add), reads=[pm, x_], writes=[x_])
                if half == 1:
                    P.dma("act", xo_d[i * 128:(i + 1) * 128, :], x_[:], x_, reads=[x_], writes=[xo_d])

            self.ffn_core(self.hT, rg * RG, RG, (self.w_fg, self.w_fg.h[0]), (self.w_fu, self.w_fu.h[0]), (self.w_fd, self.w_fd.h[0]), sink)
            P.scope_exit()

    def phase_moe(self, l, x1_d, xo_d):
        P, T, NT, bank, cst = self.P, self.T, self.NT, self.bank, self.cst
        CAP = self.cfg.get("cap", 1536)
        XS_d = P.dram("XS_d", [NEXP * CAP, D], BF16)
        YS_d = P.dram("YS_d", [NEXP * CAP, D], F32)
        gsm = P.sb("gsm", [128, NT, 2], F32)
        idxs = P.sb("idxs", [128, NT, 2], I32)
        P.scope_enter()
        grow = P.sb("grow", [128, D], F32)
        wrg = P.sb("wrg", [128, NEXP, D], F32)
        brt = P.sb("brt", [128, NEXP], F32)
        ebase = P.sb("ebase", [128, NEXP], F32)
        zt = P.sb("zt", [128, 2, D], BF16)
        xt = [P.sb("xtm", [128, D], F32) for _ in range(2)]
        junk = P.sb("junk", [128, D], F32)
        junk2 = P.sb("junk2", [128, D], F32)
        h2b = [P.sb("h2b", [128, D], BF16) for _ in range(2)]
        sm = [P.sb("smm", [128, 96], F32) for _ in range(2)]
        selb = [P.sb("selb", [128, NEXP], BF16) for _ in range(2)]
        sm2 = [P.sb("smm2", [128, 2], F32) for _ in range(2)]
        selcum = [P.sb("selcum", [128, NEXP], BF16) for _ in range(2)]
        P.dma("sp", grow[:], self.d_grow[:], grow, reads=[self.d_grow], writes=[grow])
        P.dma("sp", wrg[:], self.d_wr.h.rearrange("p (e d) -> p e d", e=NEXP), wrg, reads=[self.d_wr], writes=[wrg])
        P.dma("sp", brt[:], self.d_br[:], brt, reads=[self.d_br], writes=[brt])
        P.dma("sp", ebase[:], self.d_ebase[:], ebase, reads=[self.d_ebase], writes=[ebase])
        for e_ in range(NEXP):
            P.c("pool", lambda e, e_=e_: e.tensor_tensor(out=wrg[:, e_, :], in0=wrg[:, e_, :], in1=grow[:], op=ALU.mult),
                reads=[wrg, grow], writes=[wrg])
        P.c("pool", lambda e: e.memset(zt[:], 0.0), writes=[zt])
        P.c("pool", lambda e: e.memset(selcum[1][:], 0.0), writes=[selcum[1]])
        for a in range(NEXP * CAP // 256):
            P.dma("sp", XS_d.h[a * 256:(a + 1) * 256, :].rearrange("(a p) n -> p a n", p=128), zt[:], zt, reads=[zt], writes=[XS_d])

        def route(i):
            x_, hb_, s_, sb_ = xt[i % 2], h2b[i % 2], sm[i % 2], selb[i % 2]
            s2_ = sm2[i % 2]
            sc_new, sc_old = selcum[i % 2], selcum[(i + 1) % 2]
            col = lambda a, b=None: s_[:, a:(a + 1 if b is None else b)]
            P.dma("sp", x_[:], x1_d[i * 128:(i + 1) * 128, :], x_, reads=[x1_d], writes=[x_])
            P.c("act", lambda e: e.activation(out=junk[:], in_=x_[:], func=AF.Square, accum_out=col(0)), reads=[x_], writes=[junk, s_])
            P.c("act", lambda e: e.activation(out=col(1), in_=col(0), func=AF.Ln, scale=1.0 / D, bias=cst[:, 0:1]), reads=[s_, cst], writes=[s_])
            P.c("act", lambda e: e.activation(out=col(1), in_=col(1), func=AF.Exp, scale=-0.5), reads=[s_], writes=[s_])
            P.c("dve", lambda e: e.scalar_tensor_tensor(out=hb_[:], in0=x_[:], scalar=col(1), in1=grow[:], op0=ALU.mult, op1=ALU.mult),
                reads=[x_, s_, grow], writes=[hb_])
            for e_ in range(NEXP):
                P.c("dve", lambda e, e_=e_: e.scalar_tensor_tensor(out=junk[:], in0=x_[:], scalar=col(1), in1=wrg[:, e_, :], op0=ALU.mult,
                                                                   op1=ALU.mult, accum_out=col(8 + e_)), reads=[x_, s_, wrg], writes=[junk, s_])
            if True:
                pass
            lg, eq1, lg2, eq2, dest, tmp8 = col(8, 16), col(16, 24), col(24, 32), col(32, 40), col(40, 48), col(48, 56)
            P.c("dve", lambda e: e.tensor_tensor(out=lg, in0=lg, in1=brt[:], op=ALU.add), reads=[s_, brt], writes=[s_])
            P.c("dve", lambda e: e.reduce_max(out=col(2), in_=lg, axis=AX.X), reads=[s_], writes=[s_])
            P.c("dve", lambda e: e.tensor_scalar(out=eq1, in0=lg, scalar1=col(2), scalar2=None, op0=ALU.is_equal), reads=[s_], writes=[s_])
            P.c("dve", lambda e: e.scalar_tensor_tensor(out=lg2, in0=eq1, scalar=-1e30, in1=lg, op0=ALU.mult, op1=ALU.add), reads=[s_], writes=[s_])
            P.c("dve", lambda e: e.reduce_max(out=col(3), in_=lg2, axis=AX.X), reads=[s_], writes=[s_])
            P.c("dve", lambda e: e.tensor_scalar(out=eq2, in0=lg2, scalar1=col(3), scalar2=None, op0=ALU.is_equal), reads=[s_], writes=[s_])
            P.c("dve", lambda e: e.tensor_scalar(out=col(4), in0=col(2), scalar1=-1.0, scalar2=None, op0=ALU.mult), reads=[s_], writes=[s_])
            P.c("act", lambda e: e.activation(out=col(5), in_=col(3), func=AF.Exp, bias=col(4)), reads=[s_], writes=[s_])
            P.c("dve", lambda e: e.tensor_scalar(out=col(6), in0=col(5), scalar1=1.0, scalar2=None, op0=ALU.add), reads=[s_], writes=[s_])
            P.c("dve", lambda e: e.reciprocal(out=col(6), in_=col(6)), reads=[s_], writes=[s_])
            P.c("dve", lambda e: e.tensor_copy(out=gsm[:, i, 0:1], in_=col(6)), reads=[s_], writes=[gsm])
            P.c("dve", lambda e: e.tensor_tensor(out=gsm[:, i, 1:2], in0=col(5), in1=col(6), op=ALU.mult), reads=[s_], writes=[gsm])
            P.c("dve", lambda e: e.tensor_tensor(out=sb_[:], in0=eq1, in1=eq2, op=ALU.add), reads=[s_], writes=[sb_])
            pc = bank[i % 2]
            P.c("pe", lambda e: e.matmul(pc[:, 0:NEXP], lhsT=self.ones(), rhs=sb_[:], start=True, stop=False), reads=[self.cb, sb_], writes=[pc])
            P.c("pe", lambda e: e.matmul(pc[:, 0:NEXP], lhsT=self.tri(), rhs=sb_[:], start=False, stop=False), reads=[self.cb, sb_], writes=[pc])
            P.c("pe", lambda e: e.matmul(pc[:, 0:NEXP], lhsT=self.ones(), rhs=sc_old[:], start=False, stop=True), reads=[self.cb, sc_old], writes=[pc])
            P.c("dve", lambda e: e.tensor_tensor(out=sc_new[:], in0=sc_old[:], in1=sb_[:], op=ALU.add), reads=[sc_old, sb_], writes=[sc_new])
            P.c("dve", lambda e: e.scalar_tensor_tensor(out=dest, in0=pc[:, 0:NEXP], scalar=float(CAP - 1), in1=ebase[:], op0=ALU.min, op1=ALU.add),
                reads=[pc, ebase], writes=[s_])
            for kk, eq in enumerate((eq1, eq2)):
                P.c("dve", lambda e, eq=eq: e.tensor_tensor(out=tmp8, in0=eq, in1=dest, op=ALU.mult), reads=[s_], writes=[s_])
                P.c("dve", lambda e, kk=kk: e.reduce_sum(out=col(56 + kk), in_=tmp8, axis=AX.X), reads=[s_], writes=[s_])
                P.c("dve", lambda e, kk=kk: e.tensor_copy(out=idxs[:, i, kk:kk + 1], in_=col(56 + kk)), reads=[s_], writes=[idxs])
            for kk in range(2):
                P.idma(XS_d[:, :], bass.IndirectOffsetOnAxis(ap=idxs[:, i, kk:kk + 1], axis=0), hb_[:], None, hb_,
                       reads=[hb_, idxs], writes=[XS_d])

        for i in range(NT):
            route(i)
        P.scope_exit()
        for ex in range(NEXP):
            self.moe_expert(ex, CAP, XS_d, YS_d)
        P.scope_enter()
        xt = [P.sb("xtc", [128, D], F32) for _ in range(2)]
        yg = [[P.sb("yg", [128, D], F32) for _ in range(2)] for _ in range(2)]

        def comb(i):
            x_ = xt[i % 2]
            P.dma("sp", x_[:], x1_d[i * 128:(i + 1) * 128, :], x_, reads=[x1_d], writes=[x_])
            for kk in range(2):
                y_ = yg[kk][i % 2]
                P.idma(y_[:], None, YS_d[:, :], bass.IndirectOffsetOnAxis(ap=idxs[:, i, kk:kk + 1], axis=0), y_,
                       reads=[YS_d, idxs], writes=[y_])
                P.c("dve", lambda e, y_=y_, kk=kk: e.scalar_tensor_tensor(out=x_[:], in0=y_[:], scalar=gsm[:, i, kk:kk + 1], in1=x_[:],
                                                                          op0=ALU.mult, op1=ALU.add), reads=[y_, gsm, x_], writes=[x_])
            P.dma("act", xo_d[i * 128:(i + 1) * 128, :], x_[:], x_, reads=[x_], writes=[xo_d])

        for i in range(NT):
            comb(i)
        P.scope_exit()

    def moe_expert(self, ex, CAP, XS_d, YS_d):
        P, bank = self.P, self.bank
        P.scope_enter()
        xsT = P.sb("xsT", [128, 8, CAP], BF16)
        xr = [P.sb("xr", [128, D], BF16) for _ in range(2)]
        ys = [P.sb("ys", [128, D], F32) for _ in range(2)]
        for j in range(CAP // 128):
            x_ = xr[j % 2]
            r0 = ex * CAP + j * 128
            P.dma("sp", x_[:], XS_d[r0:r0 + 128, :], x_, reads=[XS_d], writes=[x_])
            pt = bank[6 + j % 2]
            ptv = pt[:].bitcast(BF16)
            for c in range(8):
                P.c("pe", lambda e, c=c, x_=x_, ptv=ptv: e.transpose(out=ptv[:, c * 128:(c + 1) * 128], in_=x_[:, c * 128:(c + 1) * 128],
                                                                   identity=self.ident()), reads=[x_, self.cb], writes=[pt])
            eng = "dve" if j % 2 == 0 else "act"
            if eng == "dve":
                P.c("dve", lambda e, j=j, ptv=ptv: e.tensor_copy(out=xsT[:, :, j * 128:(j + 1) * 128], in_=ptv[:, :].rearrange("p (c n) -> p c n", c=8)),
                    reads=[pt], writes=[xsT])
            else:
                P.c("act", lambda e, j=j, ptv=ptv: e.copy(out=xsT[:, :, j * 128:(j + 1) * 128], in_=ptv[:, :].rearrange("p (c n) -> p c n", c=8)),
                    reads=[pt], writes=[xsT])

        def sink(j, half, pm):
            y_ = ys[j % 2]
            if half == 0:
                P.c("act", lambda e: e.copy(out=y_[:, 0:512], in_=pm[:, :]), reads=[pm], writes=[y_])
            else:
                P.c("dve", lambda e: e.tensor_copy(out=y_[:, 512:1024], in_=pm[:, :]), reads=[pm], writes=[y_])
                r0 = ex * CAP + j * 128
                P.dma("act", YS_d[r0:r0 + 128, :], y_[:], y_, reads=[y_], writes=[YS_d])

        self.ffn_core(xsT, 0, CAP, (self.w_eg, self.w_eg.h[0][ex]), (self.w_eu, self.w_eu.h[0][ex]), (self.w_ed, self.w_ed.h[0][ex]), sink)
        P.scope_exit()


def _consts():
    i = np.arange(128)
    ident = np.eye(128, dtype=np.float32)
    tri = -(i[:, None] >= i[None, :]).astype(np.float32)
    negones = -np.ones((128, 128), np.float32)
    ones = np.ones((128, 128), np.float32)
    blockones = ((i[:, None] // 64) == (i[None, :] // 64)).astype(np.float32)
    zeros = np.zeros((128, 128), np.float32)
    sbmask = np.where(i[:, None] >= i[None, :], NEG, 0.0).astype(np.float32)
    cb = np.concatenate([ident, tri, negones, ones, blockones, zeros, sbmask], axis=1).astype(ml_dtypes.bfloat16)
    slopes = 2.0 ** (-8.0 * np.arange(1, 13, dtype=np.float32) / 12.0)
    bt = np.zeros((128, 12, 2, 128), np.float32)
    for H in range(12):
        d = DIL[H // 4]
        for half in range(2):
            dist = (half * 128 + i[None, :] - i[:, None]).astype(np.float32)
            valid = (dist >= 0) & (dist <= 128)
            bt[:, H, half, :] = np.where(valid, -slopes[H] * dist * d, NEG)
    return cb, bt.reshape(128, 12 * 256).astype(np.float32)


def host_inputs(inp):
    L = DEPTH
    f = lambda a: np.ascontiguousarray(np.asarray(a, dtype=np.float32))
    g = np.stack([f(inp["attn_norm_g"]), f(inp["ffn_norm_g"])], axis=1)
    gcols = g.reshape(L, 2, 8, 128).transpose(3, 0, 1, 2).reshape(128, L * 2 * 8)
    qk = np.stack([f(inp["q_norm_g"]), f(inp["k_norm_g"])], axis=1)
    qkg = np.concatenate([qk, qk], axis=2).transpose(2, 0, 1).reshape(128, L * 2)
    qkrow = np.broadcast_to(qk.reshape(1, L * 2 * 64), (128, L * 2 * 64))
    cw = f(inp["conv_w"])
    convw = cw.reshape(L, 31, 4, 128).transpose(3, 0, 2, 1).reshape(128, L * 4 * 31)
    cp = np.stack([f(inp["conv_b"]), f(inp["conv_norm_g"]), f(inp["conv_norm_b"])], axis=1)
    convp = cp.reshape(L, 3, 4, 128).transpose(3, 0, 1, 2).reshape(128, L * 3 * 4)
    wr = f(inp["w_router"])[0]
    wr_bc = np.broadcast_to(wr.T.reshape(1, NEXP * D), (128, NEXP * D))
    br_bc = np.broadcast_to(f(inp["b_router"])[0].reshape(1, NEXP), (128, NEXP))
    cb, bt = _consts()
    cap = 1536
    grow = np.broadcast_to(f(inp["ffn_norm_g"])[1].reshape(1, D), (128, D))
    ebase = np.broadcast_to((np.arange(NEXP, dtype=np.float32) * cap).reshape(1, NEXP), (128, NEXP))
    m = dict(grow=grow, ebase=ebase, gcols=gcols, qkg=qkg, qkrow=qkrow, convw=convw, convp=convp, wr_bc=wr_bc, br_bc=br_bc, cbf=cb, btab=bt)
    m = {k: np.ascontiguousarray(v) for k, v in m.items()}
    for k in ("w_in", "w_branch_a", "w_branch_b", "w_branch_c", "w_out", "w_ffn_gate", "w_ffn_up", "w_ffn_down",
              "w_exp_gate", "w_exp_up", "w_exp_down"):
        m[k] = f(inp[k])
    return m


_CACHE = {}


def kernel(**inputs):
    m = host_inputs(inputs)
    x = np.asarray(inputs["x"], dtype=np.float32)
    nb = x.shape[0]
    if "k" not in _CACHE:
        kk = K()
        kk.build_full()
        _CACHE["k"] = kk
    kk = _CACHE["k"]
    in_maps = []
    for b in range(nb):
        mm = dict(m)
        mm["x"] = np.ascontiguousarray(x[b])
        in_maps.append(mm)
    res = run_bass_kernel_spmd(kk.nc, in_maps, core_ids=list(range(nb)))
    return np.stack([np.asarray(r["out"], dtype=np.float32) for r in res.results], axis=0)
```

```python
import numpy as np
import ml_dtypes
import concourse.bass as bass
import concourse.mybir as mybir
from concourse.bass_utils import run_bass_kernel_spmd

F32 = mybir.dt.float32
BF16 = mybir.dt.bfloat16
I32 = mybir.dt.int32
ALU = mybir.AluOpType
AF = mybir.ActivationFunctionType
AX = mybir.AxisListType

D = 1024
SEQ = 4096
DEPTH = 2
IN_COLS = 7936
DFF = 2816
NF = DFF // 128
NEXP = 8
DIL = (1, 4, 16)
QA, KA, VA = 0, 768, 1536
QB, KB_, VB = 2304, 2816, 3328
GV, GG = 3840, 4352
GATE0 = 4864
NEG = -30000.0


class Tl:
    _n = 0

    def __init__(self, h, name):
        self.h = h
        self.name = name
        self.w = set()
        self.r = set()
        Tl._n += 1
        self.id = Tl._n
        self.born = 0
        self.freed = None

    def __getitem__(self, idx):
        return self.h[idx]


class Prog:
    STREAMS = ("pe", "act", "dve", "pool", "sp")

    def __init__(self, nc):
        self.nc = nc
        self.ops = []
        self.streams = {s: [] for s in self.STREAMS}
        self.semorder = {}
        self.n_t = 0
        self.tiles = {}
        self.ghost = {}
        self.scopes = []

    def sb(self, name, shape, dt):
        self.n_t += 1
        t = Tl(self.nc.alloc_sbuf_tensor(f"{name}_{self.n_t}", list(shape), dt), name)
        t.w = set(self.ghost.values())
        t.born = len(self.ops)
        self.tiles[t.id] = t
        if self.scopes:
            self.scopes[-1][1].append(t)
        return t

    def scope_enter(self):
        g = self.nc.reset_on_exit()
        g.__enter__()
        self.scopes.append((g, []))

    def scope_exit(self):
        g, tiles = self.scopes.pop()
        g.__exit__(None, None, None)
        for t in tiles:
            t.freed = len(self.ops)
            for oid in (t.w | t.r):
                op = self.ops[oid]
                k = op["semkey"]
                if k not in self.ghost or self.ops[self.ghost[k]]["pos"] < op["pos"]:
                    self.ghost[k] = oid

    def ps(self, name, shape, dt=F32):
        self.n_t += 1
        t = Tl(self.nc.alloc_psum_tensor(f"{name}_{self.n_t}", list(shape), dt), name)
        t.excl = True
        return t

    def dram(self, name, shape, dt, kind="Internal"):
        return Tl(self.nc.dram_tensor(name, list(shape), dt, kind=kind), name)

    def _add(self, stream, fn, reads, writes, semkey, inc):
        oid = len(self.ops)
        raw = set()
        rar = set()
        for t in reads:
            raw |= t.w
            if getattr(t, "excl", False):
                rar |= t.r
        deps = set(raw) | rar
        waw, war = set(), set()
        for t in writes:
            waw |= t.w
            war |= t.r
        deps |= waw | war
        is_dma = inc == 16
        keep = set()
        for d in deps:
            od = self.ops[d]
            if od["semkey"] == semkey and d not in raw:
                if not is_dma:
                    continue
                if d in waw and d not in war:
                    continue
            keep.add(d)
        op = dict(id=oid, stream=stream, fn=fn, deps=keep, semkey=semkey, inc=inc)
        self.ops.append(op)
        self.streams[stream].append(oid)
        self.semorder.setdefault(semkey, []).append(oid)
        op["pos"] = len(self.semorder[semkey]) - 1
        for t in reads:
            t.r.add(oid)
        for t in writes:
            t.w = {oid}
            t.r = set()
        return oid

    def c(self, eng, fn, reads=(), writes=()):
        return self._add(eng, fn, reads, writes, eng, 1)

    def dma(self, stream, out_ap, in_ap, semtile, reads=(), writes=(), **kw):
        fn = lambda e: e.dma_start(out=out_ap, in_=in_ap, **kw)
        return self._add(stream, fn, reads, writes, ("dma", semtile.id, stream == "pool"), 16)

    def idma(self, out_ap, out_off, in_ap, in_off, semtile, reads=(), writes=()):
        fn = lambda e: e.indirect_dma_start(out=out_ap, out_offset=out_off, in_=in_ap, in_offset=in_off)
        return self._add("pool", fn, reads, writes, ("dma", semtile.id, True), 16)

    def finish(self, stream, tiles):
        oid = self._add(stream, None, list(tiles), [], ("fin", stream), 0)
        for k, lst in self.semorder.items():
            if not isinstance(k, str) and k[0] == "dma":
                self.ops[oid]["deps"].add(lst[-1])
        return oid

    def emit(self):
        nc = self.nc
        ops = self.ops
        seen = {s: {} for s in self.STREAMS}
        needed = {}
        for op in ops:
            waits = {}
            for d in op["deps"]:
                od = ops[d]
                k = od["semkey"]
                if od["pos"] > waits.get(k, -1):
                    waits[k] = od["pos"]
            sn = seen[op["stream"]]
            w2 = []
            for k, p in waits.items():
                if sn.get(k, -1) >= p:
                    continue
                sn[k] = p
                needed.setdefault(k, set()).add(p)
                w2.append((k, p))
            op["waits"] = w2
        semval = {}
        sems = {}
        for k, lst in self.semorder.items():
            if not isinstance(k, str) or k not in needed:
                continue
            sems[k] = nc.alloc_semaphore("s_%s" % k)
            cnt = 0
            nd = needed[k]
            for p, oid in enumerate(lst):
                if p in nd:
                    cnt += 1
                    ops[oid]["do_inc"] = True
                semval[(k, p)] = cnt
        INF = 1 << 60
        dkeys = [k for k in self.semorder if not isinstance(k, str) and k[0] == "dma"]
        dkeys.sort(key=lambda k: self.tiles[k[1]].born)
        pools = {True: [], False: []}
        nphys = 0
        for k in dkeys:
            tl = self.tiles[k[1]]
            ent = None
            for cand in pools[k[2]]:
                if cand[2] <= tl.born:
                    ent = cand
                    break
            if ent is None:
                ent = [nc.alloc_semaphore("s_d%d" % nphys), 0, INF]
                nphys += 1
                pools[k[2]].append(ent)
            sems[k] = ent[0]
            base = ent[1]
            lst = self.semorder[k]
            for p, oid in enumerate(lst):
                ops[oid]["do_inc"] = True
                semval[(k, p)] = base + 16 * (p + 1)
            ent[1] = base + 16 * len(lst)
            ent[2] = tl.freed if tl.freed is not None else INF
        self.n_sems = len(sems)
        self.max_semval = max(semval.values()) if semval else 0
        engmap = dict(pe="tensor", act="scalar", dve="vector", pool="gpsimd", sp="sync")

        def run_stream(s):
            def body(e):
                for oid in self.streams[s]:
                    op = ops[oid]
                    for k, p in op["waits"]:
                        e.wait_ge(sems[k], semval[(k, p)])
                    if op["fn"] is None:
                        continue
                    ins = op["fn"](e)
                    if op.get("do_inc"):
                        ins.then_inc(sems[op["semkey"]], op["inc"])
            return body

        with nc.Block() as block:
            for s in self.STREAMS:
                if self.streams[s]:
                    getattr(block, engmap[s])(run_stream(s))


def sl(start, count, step=1):
    return slice(start, start + (count - 1) * step + 1, step)


def rr(gens):
    gens = list(gens)
    while gens:
        nxt = []
        for g in gens:
            try:
                next(g)
                nxt.append(g)
            except StopIteration:
                pass
        gens = nxt


class K:
    def __init__(self, T=SEQ, cfg=None):
        self.T = T
        self.cfg = cfg or {}
        self.nc = bass.Bass("TRN2", target_bir_lowering=False)
        self.P = Prog(self.nc)
        self.NT = T // 128
        self.NG = T // 512
        self._wq = 0
        self.setup()

    def setup(self):
        P, T = self.P, self.T
        L = DEPTH
        DFF = self.dff = self.cfg.get("dff", 2816)
        ein = lambda n, s, d: P.dram(n, s, d, kind="ExternalInput")
        self.x_in = ein("x", [T, D], F32)
        self.w_in = ein("w_in", [L, D, IN_COLS], F32)
        self.w_ba = ein("w_branch_a", [L, 256, D], F32)
        self.w_bb = ein("w_branch_b", [L, 512, D], F32)
        self.w_bc = ein("w_branch_c", [L, 512, D], F32)
        self.w_out = ein("w_out", [L, D, D], F32)
        self.w_fg = ein("w_ffn_gate", [1, D, DFF], F32)
        self.w_fu = ein("w_ffn_up", [1, D, DFF], F32)
        self.w_fd = ein("w_ffn_down", [1, DFF, D], F32)
        if self.cfg.get("moe", True):
            self.w_eg = ein("w_exp_gate", [1, NEXP, D, DFF], F32)
            self.w_eu = ein("w_exp_up", [1, NEXP, D, DFF], F32)
            self.w_ed = ein("w_exp_down", [1, NEXP, DFF, D], F32)
        self.d_gcols = ein("gcols", [128, L * 2 * 8], F32)
        self.d_qkg = ein("qkg", [128, L * 2], F32)
        self.d_qkrow = ein("qkrow", [128, L * 2 * 64], F32)
        self.d_convw = ein("convw", [128, L * 4 * 31], F32)
        self.d_convp = ein("convp", [128, L * 3 * 4], F32)
        self.d_wr = ein("wr_bc", [128, NEXP * D], F32)
        self.d_br = ein("br_bc", [128, NEXP], F32)
        self.d_grow = ein("grow", [128, D], F32)
        self.d_ebase = ein("ebase", [128, NEXP], F32)
        self.d_cb = ein("cbf", [128, 7 * 128], BF16)
        self.d_bt = ein("btab", [128, 12 * 256], F32)
        self.out = P.dram("out", [T, D], F32, kind="ExternalOutput")
        self.xs = [P.dram("xs0", [T, D], F32), P.dram("xs1", [T, D], F32)]
        self.mT_d = P.dram("mT_d", [D, T], BF16)
        self.oT_d = P.dram("oT_d", [1280, T], BF16, kind="ExternalOutput" if self.cfg.get("dbg_oT") else "Internal")
        self.dbg = {}

        def ld(name, d, shape, dt):
            t = P.sb(name, shape, dt)
            P.dma("sp", t[:], d[:], t, reads=[d], writes=[t])
            return t

        self.gcols = ld("gcols", self.d_gcols, [128, L * 2 * 8], F32)
        self.qkg = ld("qkg", self.d_qkg, [128, L * 2], F32)
        self.qkrow = ld("qkrow", self.d_qkrow, [128, L * 2 * 64], F32)
        self.convw = ld("convw", self.d_convw, [128, L * 4 * 31], F32)
        self.convp = ld("convp", self.d_convp, [128, L * 3 * 4], F32)
        self.cb = ld("cb", self.d_cb, [128, 7 * 128], BF16)
        self.bt = ld("bt", self.d_bt, [128, 12 * 256], F32)
        cbv = lambda i: (lambda: self.cb[:, i * 128:(i + 1) * 128])
        self.ident, self.tri, self.negones, self.ones, self.blockones, self.zeros, self.sbmask = [cbv(i) for i in range(7)]
        self.cst = P.sb("cst", [128, 4], F32)
        P.c("pool", lambda e: e.memset(self.cst[:, 0:1], 1e-6), writes=[self.cst])
        P.c("pool", lambda e: e.memset(self.cst[:, 1:2], 1.0), writes=[self.cst])
        P.c("pool", lambda e: e.memset(self.cst[:, 2:3], 0.0), writes=[self.cst])
        self.bank = [P.ps("bank%d" % i, [128, 512], F32) for i in range(8)]
        self.wbufs = [P.sb("wb", [128, 8, 128], BF16) for _ in range(6)]

    def build_full(self, n_layers=DEPTH):
        P = self.P
        x_cur = self.x_in
        for l in range(n_layers):
            P.scope_enter()
            self.hT = P.sb("hT", [128, 8, self.T], BF16)
            self.phase_norm(x_cur, (l * 2 + 0) * 8)
            self.phase_sb(l)
            self.phase_dil(l)
            self.phase_conv(l)
            x1_d = self.xs[0]
            self.phase_merge(l, x_cur, x1_d)
            xo = self.out if l == n_layers - 1 else self.xs[1]
            if l % 2 == 0:
                self.phase_norm(x1_d, (l * 2 + 1) * 8)
                self.phase_ffn_dense(x1_d, xo)
                P.scope_exit()
            else:
                P.scope_exit()
                self.phase_moe(l, x1_d, xo)
            x_cur = xo
        P.finish("sp", [self.out])
        P.emit()

    def dbg_out(self, name, shape, dt=F32):
        t = self.P.dram(name, shape, dt, kind="ExternalOutput")
        self.dbg[name] = t
        return t

    def wchunk(self, src_tl, src_ap):
        wb = self.wbufs[self._wq % len(self.wbufs)]
        self._wq += 1
        self.P.dma("pool", wb[:], src_ap, wb, reads=[src_tl], writes=[wb])
        return wb

    def win_cols(self, l, c0, n=128):
        return self.w_in.h[l].rearrange("(c p) n -> p c n", p=128)[:, :, c0:c0 + n]

    def phase_norm(self, x_d, gcol_off):
        P, T = self.P, self.T
        P.scope_enter()
        na = dict(
            xt=[P.sb("xt", [128, D], F32) for _ in range(2)],
            sq=P.sb("sq", [128, D], F32),
            ss=[P.sb("ss", [128, 1], F32) for _ in range(2)],
            rs=[P.sb("rs", [128, 1], F32) for _ in range(2)],
            hb=[P.sb("hb", [128, D], BF16) for _ in range(2)],
        )
        hT, gcols, cst = self.hT, self.gcols, self.cst
        for i in range(self.NT):
            b = i % 2
            xt, ss, rs, hb, sq = na["xt"][b], na["ss"][b], na["rs"][b], na["hb"][b], na["sq"]
            P.dma("sp", xt[:], x_d[i * 128:(i + 1) * 128, :], xt, reads=[x_d], writes=[xt])
            P.c("act", lambda e, xt=xt, ss=ss: e.activation(out=sq[:], in_=xt[:], func=AF.Square, accum_out=ss[:]),
                reads=[xt], writes=[sq, ss])
            P.c("act", lambda e, ss=ss, rs=rs: e.activation(out=rs[:], in_=ss[:], func=AF.Ln, scale=1.0 / D, bias=cst[:, 0:1]),
                reads=[ss, cst], writes=[rs])
            P.c("act", lambda e, rs=rs: e.activation(out=rs[:], in_=rs[:], func=AF.Exp, scale=-0.5),
                reads=[rs], writes=[rs])
            P.c("dve", lambda e, xt=xt, rs=rs, hb=hb: e.tensor_scalar(out=hb[:], in0=xt[:], scalar1=rs[:, 0:1], scalar2=None,
                                                                   op0=ALU.mult), reads=[xt, rs], writes=[hb])
            pt = self.bank[i % 2]
            ptv = pt[:].bitcast(BF16)
            for c in range(8):
                P.c("pe", lambda e, ptv=ptv, c=c, hb=hb: e.transpose(out=ptv[:, c * 128:(c + 1) * 128],
                                                                     in_=hb[:, c * 128:(c + 1) * 128], identity=self.ident()),
                    reads=[hb, self.cb], writes=[pt])
            for c in range(8):
                eng = "dve" if c % 2 == 0 else "pool"
                eng = "dve"
                P.c(eng, lambda e, ptv=ptv, c=c, i=i: e.tensor_scalar(
                    out=hT[:, c, i * 128:(i + 1) * 128], in0=ptv[:, c * 128:(c + 1) * 128],
                    scalar1=gcols[:, gcol_off + c:gcol_off + c + 1], scalar2=None, op0=ALU.mult),
                    reads=[pt, gcols], writes=[hT])
        P.scope_exit()

    def proj_fm(self, wb, pm, tg, wcol0=0, m=128):
        P, hT = self.P, self.hT
        for c in range(8):
            P.c("pe", lambda e, c=c: e.matmul(pm[0:m, :], lhsT=wb[:, c, wcol0:wcol0 + m], rhs=hT[:, c, tg * 512:(tg + 1) * 512],
                                              start=(c == 0), stop=(c == 7)), reads=[wb, hT], writes=[pm])

    def phase_sb(self, l):
        P, T, NT, NG = self.P, self.T, self.NT, self.NG
        hT, bank = self.hT, self.bank
        P.scope_enter()
        if True:
            NCH = 4
            self.sbb = dict(
                vall=P.sb("vall", [128, NT, 512], BF16),
                wv=P.sb("wv", [128, 8, 512], BF16),
                qT=P.sb("qT", [128, T], BF16),
                kT=P.sb("kT", [128, T], BF16),
                obT=P.sb("obT", [128, T], BF16),
                ch=[dict(e=P.sb("e", [128, 512], F32),
                         sp=[P.sb("sp", [128, 512], BF16) for _ in range(2)],
                         a=[P.sb("a", [128, 512], BF16) for _ in range(2)],
                         r32=P.sb("r32", [128, 512], F32),
                         rb=[P.sb("rb", [128, 512], BF16) for _ in range(2)],
                         zb=bank[2 * i], ob=bank[2 * i + 1]) for i in range(NCH)],
            )
        S = self.sbb
        vall, wv, qT, kT, obT = S["vall"], S["wv"], S["qT"], S["kT"], S["obT"]
        cst = self.cst
        P.dma("pool", wv[:], self.win_cols(l, VB, 512), wv, reads=[self.w_in], writes=[wv])
        for i in range(NT):
            pm = bank[i % 2]
            for c in range(8):
                P.c("pe", lambda e, c=c, i=i, pm=pm: e.matmul(pm[:, :], lhsT=hT[:, c, i * 128:(i + 1) * 128], rhs=wv[:, c, :],
                                                             start=(c == 0), stop=(c == 7)), reads=[hT, wv], writes=[pm])
            if i % 2 == 0:
                P.c("act", lambda e, i=i, pm=pm: e.copy(out=vall[:, i, :], in_=pm[:, :]), reads=[pm], writes=[vall])
            else:
                P.c("dve", lambda e, i=i, pm=pm: e.tensor_copy(out=vall[:, i, :], in_=pm[:, :]), reads=[pm], writes=[vall])

        def chain(h, Q, C):
            prow = (h % 2) * 64
            hp = h // 2
            zb, ob, esb, r32 = C["zb"], C["ob"], C["e"], C["r32"]
            rows = slice(prow, prow + 64)
            q0 = Q * 512
            P.c("pe", lambda e: e.matmul(ob[:, :], lhsT=self.zeros(), rhs=qT[:, q0:q0 + 512], start=True, stop=False),
                reads=[self.cb, qT], writes=[ob])
            P.c("pool", lambda e: e.memset(r32[:], 0.0), writes=[r32])
            yield
            kbs = list(range(4 * Q + 3, -1, -1))
            for ti, kb in enumerate(kbs):
                j = max(kb - 4 * Q, 0)
                c0 = 128 * j
                diag = kb >= 4 * Q
                first = ti == 0
                last = kb == 0
                sp, a, rb = C["sp"][ti % 2], C["a"][ti % 2], C["rb"][ti % 2]
                rbp = C["rb"][(ti + 1) % 2]
                P.c("pe", lambda e, kb=kb, c0=c0: e.matmul(zb[:, c0:512], lhsT=kT[rows, kb * 128:(kb + 1) * 128],
                                                           rhs=qT[rows, q0 + c0:q0 + 512], start=True, stop=False, skip_group_check=True),
                    reads=[kT, qT], writes=[zb])
                if diag:
                    P.c("pe", lambda e, c0=c0: e.matmul(zb[:, c0:c0 + 128], lhsT=self.ident(), rhs=self.sbmask(),
                                                        start=False, stop=False, skip_group_check=True), reads=[self.cb], writes=[zb])
                yield
                P.c("act", lambda e, c0=c0: e.activation(out=esb[:, c0:512], in_=zb[:, c0:512], func=AF.Exp),
                    reads=[zb], writes=[esb])
                yield
                P.c("act", lambda e, c0=c0, sp=sp: e.activation(out=sp[:, c0:512], in_=esb[:, c0:512], func=AF.Ln, bias=cst[:, 1:2]),
                    reads=[esb, cst], writes=[sp])
                yield
                P.c("pe", lambda e, c0=c0, sp=sp, first=first: e.matmul(zb[:, c0:512], lhsT=self.tri(), rhs=sp[:, c0:512],
                                                                        start=False, stop=first, skip_group_check=True),
                    reads=[self.cb, sp], writes=[zb])
                if not first:
                    P.c("pe", lambda e, c0=c0, rbp=rbp: e.matmul(zb[:, c0:512], lhsT=self.negones(), rhs=rbp[:, c0:512],
                                                                 start=False, stop=True, skip_group_check=True),
                        reads=[self.cb, rbp], writes=[zb])
                if not last:
                    P.c("dve", lambda e, c0=c0, sp=sp: e.tensor_tensor(out=r32[:, c0:512], in0=r32[:, c0:512], in1=sp[:, c0:512],
                                                                       op=ALU.add), reads=[r32, sp], writes=[r32])
                    P.c("dve", lambda e, rb=rb: e.tensor_copy(out=rb[:, :], in_=r32[:, :]), reads=[r32], writes=[rb])
                yield
                P.c("act", lambda e, c0=c0, a=a: e.activation(out=a[:, c0:512], in_=zb[:, c0:512], func=AF.Exp),
                    reads=[zb], writes=[a])
                yield
                P.c("pe", lambda e, c0=c0, a=a, kb=kb, last=last: e.matmul(ob[:, c0:512], lhsT=vall[:, kb, hp * 128:(hp + 1) * 128],
                                                                          rhs=a[:, c0:512], start=False, stop=last),
                    reads=[vall, a], writes=[ob])
                yield
            P.c("dve", lambda e: e.tensor_copy(out=obT[rows, q0:q0 + 512], in_=ob[rows, :]), reads=[ob], writes=[obT])
            yield

        for hp in range(4):
            wq = self.wchunk(self.w_in, self.win_cols(l, QB + hp * 128))
            wk = self.wchunk(self.w_in, self.win_cols(l, KB_ + hp * 128))
            for tg in range(NG):
                pm = bank[0]
                self.proj_fm(wq, pm, tg)
                P.c("act", lambda e, tg=tg, pm=pm: e.mul(out=qT[:, tg * 512:(tg + 1) * 512], in_=pm[:, :], mul=0.125),
                    reads=[pm], writes=[qT])
                pm = bank[1]
                self.proj_fm(wk, pm, tg)
                P.c("dve", lambda e, tg=tg, pm=pm: e.tensor_copy(out=kT[:, tg * 512:(tg + 1) * 512], in_=pm[:, :]),
                    reads=[pm], writes=[kT])
            jobs = [(2 * hp + hh, Q) for Q in range(NG - 1, -1, -1) for hh in range(2)]
            nch = len(S["ch"])
            slots = [[] for _ in range(nch)]
            load = [0] * nch
            for jb in sorted(jobs, key=lambda jb: -jb[1]):
                si = load.index(min(load))
                slots[si].append(jb)
                load[si] += 4 * jb[1] + 4

            def slot_gen(si):
                for (h, Q) in slots[si]:
                    yield from chain(h, Q, S["ch"][si])
            rr([slot_gen(si) for si in range(nch)])
            P.dma("sp", self.oT_d[256 + hp * 128:256 + (hp + 1) * 128, :], obT[:, :], obT, reads=[obT], writes=[self.oT_d])
        P.scope_exit()

    def phase_dil(self, l):
        P, T, NT, NG = self.P, self.T, self.NT, self.NG
        hT, bank, cst = self.hT, self.bank, self.cst
        P.scope_enter()
        qn = P.sb("qn", [128, T], BF16)
        kn = P.sb("kn", [128, T], BF16)
        vp = P.sb("vp", [128, NT, 128], BF16)
        vT = P.sb("vT", [128, T], BF16)
        acc = P.sb("acc", [128, 2, T], F32)
        raw = [P.sb("raw", [128, 512], F32) for _ in range(2)]
        sq = [P.sb("sqd", [128, 512], BF16) for _ in range(2)]
        rst = [P.sb("rst", [128, 512], F32) for _ in range(2)]
        tmp = [P.sb("tmpd", [128, 512], F32) for _ in range(2)]
        pT = [P.sb("pT", [128, 512], BF16) for _ in range(3)]
        oa = P.sb("oa", [128, T], BF16)
        sm = P.sb("smalld", [128, 136], F32)
        bt = P.sb("bt", [128, 12 * 256], F32)
        P.dma("sp", bt[:], self.d_bt[:], bt, reads=[self.d_bt], writes=[bt])
        qkrow, qkg = self.qkrow, self.qkg
        P.c("dve", lambda e: e.tensor_tensor(out=sm[:, 0:128], in0=qkrow[:, l * 128:(l + 1) * 128], in1=qkrow[:, l * 128:(l + 1) * 128],
                                             op=ALU.mult), reads=[qkrow], writes=[sm])
        P.c("dve", lambda e: e.reduce_max(out=sm[:, 128:129], in_=sm[:, 0:64], axis=AX.X), reads=[sm], writes=[sm])
        P.c("dve", lambda e: e.reduce_max(out=sm[:, 129:130], in_=sm[:, 64:128], axis=AX.X), reads=[sm], writes=[sm])
        P.c("dve", lambda e: e.tensor_tensor(out=sm[:, 130:131], in0=sm[:, 128:129], in1=sm[:, 129:130], op=ALU.add),
            reads=[sm], writes=[sm])
        P.c("dve", lambda e: e.tensor_scalar(out=sm[:, 130:131], in0=sm[:, 130:131], scalar1=-4.0, scalar2=None, op0=ALU.mult),
            reads=[sm], writes=[sm])
        P.c("dve", lambda e: e.tensor_scalar(out=sm[:, 131:132], in0=qkg[:, 2 * l:2 * l + 1], scalar1=0.125, scalar2=None, op0=ALU.mult),
            reads=[qkg], writes=[sm])
        negc = lambda: sm[:, 130:131]
        g8 = lambda: sm[:, 131:132]
        gk = lambda: qkg[:, 2 * l + 1:2 * l + 2]
        cnt = [0]
        def group(s, g):
            if True:
                d = DIL[g]
                nb = T // (128 * d)
                H = 4 * g + 2 * s
                wq = self.wchunk(self.w_in, self.win_cols(l, QA + H * 64))
                wk = self.wchunk(self.w_in, self.win_cols(l, KA + H * 64))
                wvv = self.wchunk(self.w_in, self.win_cols(l, VA + H * 64))
                if self.cfg.get("dil_stop", 99) <= 0.1:
                    return
                for (w, dst, gain) in ((wq, qn, g8), (wk, kn, gk)):
                    for tg in range(NG):
                        i2 = cnt[0] % 2
                        cnt[0] += 1
                        pm, pm2 = bank[i2], bank[2 + i2]
                        rw, sqq, rs_ = raw[i2], sq[i2], rst[i2]
                        self.proj_fm(w, pm, tg)
                        P.c("dve", lambda e, pm=pm, rw=rw: e.tensor_copy(out=rw[:], in_=pm[:, :]), reads=[pm], writes=[rw])
                        P.c("act", lambda e, pm=pm, sqq=sqq: e.activation(out=sqq[:], in_=pm[:, :], func=AF.Square), reads=[pm], writes=[sqq])
                        P.c("pe", lambda e, pm2=pm2, sqq=sqq: e.matmul(pm2[:, :], lhsT=self.blockones(), rhs=sqq[:], start=True, stop=True),
                            reads=[self.cb, sqq], writes=[pm2])
                        P.c("act", lambda e, pm2=pm2, rs_=rs_: e.activation(out=rs_[:], in_=pm2[:, :], func=AF.Ln, scale=1.0 / 64, bias=cst[:, 0:1]),
                            reads=[pm2, cst], writes=[rs_])
                        P.c("act", lambda e, rs_=rs_: e.activation(out=rs_[:], in_=rs_[:], func=AF.Exp, scale=-0.5), reads=[rs_], writes=[rs_])
                        if self.cfg.get("dil_stop", 99) <= 0.5:
                            continue
                        P.c("dve", lambda e, dst=dst, tg=tg, rw=rw, rs_=rs_, gain=gain: e.scalar_tensor_tensor(
                            out=dst[:, :].rearrange("p (r i) -> p r i", r=d)[:, :, tg * (512 // d):(tg + 1) * (512 // d)],
                            in0=rw[:].rearrange("p (i r) -> p r i", r=d), scalar=gain(),
                            in1=rs_[:].rearrange("p (i r) -> p r i", r=d), op0=ALU.mult, op1=ALU.mult),
                            reads=[rw, rs_, sm, qkg], writes=[dst])
                if self.cfg.get("dil_stop", 99) <= 1:
                    return
                for tg in range(NG):
                    pm = bank[tg % 2]
                    self.proj_fm(wvv, pm, tg)
                    P.c("act", lambda e, pm=pm, tg=tg: e.copy(
                        out=vT[:, :].rearrange("p (r i) -> p r i", r=d)[:, :, tg * (512 // d):(tg + 1) * (512 // d)],
                        in_=pm[:, :].rearrange("p (i r) -> p r i", r=d)), reads=[pm], writes=[vT])
                for b in range(NT):
                    pm = bank[4 + (b // 4) % 2]
                    pmv = pm[:].bitcast(BF16)
                    P.c("pe", lambda e, pmv=pmv, b=b: e.transpose(out=pmv[:, (b % 4) * 128:(b % 4 + 1) * 128], in_=vT[:, b * 128:(b + 1) * 128],
                                                                 identity=self.ident()), reads=[vT, self.cb], writes=[pm])
                    if b % 4 == 3:
                        P.c("dve", lambda e, pmv=pmv, b=b: e.tensor_copy(out=vp[:, b - 3:b + 1, :],
                                                                        in_=pmv[:, 0:512].rearrange("p (a n) -> p a n", a=4)),
                            reads=[pm], writes=[vp])
                if self.cfg.get("dil_stop", 99) <= 2:
                    return
                def head(hh):
                    rows = slice(hh * 64, hh * 64 + 64)
                    Hh = H + hh
                    btH = lambda Hh=Hh: bt[:, Hh * 256:(Hh + 1) * 256]
                    batches = [(r, mb) for r in range(d) for mb in range(nb // 2)]

                    def stage1(bi):
                        r, mb = batches[bi]
                        sbk = bank[4 + bi % 2]
                        tm = tmp[bi % 2]
                        pt = pT[bi % 3]
                        width = 0
                        for i in range(2):
                            m = 2 * mb + i
                            nq = 256 if m < nb - 1 else 128
                            P.c("pe", lambda e, i=i, m=m, nq=nq: e.matmul(
                                sbk[:, i * 256:i * 256 + nq], lhsT=kn[rows, (r * nb + m) * 128:(r * nb + m + 1) * 128],
                                rhs=qn[rows, (r * nb + m) * 128:(r * nb + m) * 128 + nq], start=True, stop=True), reads=[kn, qn], writes=[sbk])
                            P.c("dve", lambda e, i=i, nq=nq: e.tensor_tensor(out=tm[:, i * 256:i * 256 + nq], in0=sbk[:, i * 256:i * 256 + nq],
                                                                            in1=btH()[:, 0:nq], op=ALU.add), reads=[sbk, bt], writes=[tm])
                            width = i * 256 + nq
                        P.c("act", lambda e, width=width: e.activation(out=pt[:, 0:width], in_=tm[:, 0:width], func=AF.Exp, bias=negc()),
                            reads=[tm, sm], writes=[pt])

                    def stage2(bi):
                        r, mb = batches[bi]
                        ub = bank[6 + bi % 2]
                        pt = pT[bi % 3]
                        ptp = pT[(bi - 1) % 3]
                        m0, m1 = 2 * mb, 2 * mb + 1
                        blk = lambda m: r * nb + m
                        contribs = [[], []]
                        if m0 > 0:
                            contribs[0].append((blk(m0 - 1), ptp, 384))
                        contribs[0].append((blk(m0), pt, 0))
                        contribs[1].append((blk(m0), pt, 128))
                        contribs[1].append((blk(m1), pt, 256))
                        for ni in range(2):
                            for ud in range(2):
                                n = len(contribs[ni])
                                for ci, (kb, ptile, c0) in enumerate(contribs[ni]):
                                    lhs = (lambda kb=kb: vp[:, kb, :]) if ud == 0 else self.ones
                                    P.c("pe", lambda e, lhs=lhs, ptile=ptile, c0=c0, ni=ni, ud=ud, ci=ci, n=n: e.matmul(
                                        ub[:, ud * 256 + ni * 128:ud * 256 + (ni + 1) * 128], lhsT=lhs(), rhs=ptile[:, c0:c0 + 128],
                                        start=(ci == 0), stop=(ci == n - 1)), reads=[vp, self.cb, ptile], writes=[ub])
                        ov = lambda: acc[rows, :, sl(m0 * 128 * d + r, 256, d)]
                        iv = lambda: ub[rows, :].rearrange("p (a n) -> p a n", a=2)
                        if g == 0:
                            P.c("dve", lambda e: e.tensor_copy(out=ov(), in_=iv()), reads=[ub], writes=[acc])
                        else:
                            P.c("dve", lambda e: e.tensor_tensor(out=ov(), in0=iv(), in1=ov(), op=ALU.add), reads=[ub, acc], writes=[acc])

                    for bi in range(len(batches) + 1):
                        if bi < len(batches):
                            stage1(bi)
                        if bi >= 1:
                            stage2(bi - 1)
                for hh in range(2):
                    head(hh)

        for s in range(2):
            for g in range(3):
                group(s, g)
            if self.cfg.get("dil_stop", 99) <= 3:
                continue
            P.c("dve", lambda e: e.reciprocal(out=acc[:, 1, :], in_=acc[:, 1, :]), reads=[acc], writes=[acc])
            P.c("dve", lambda e: e.tensor_tensor(out=oa[:, :], in0=acc[:, 0, :], in1=acc[:, 1, :], op=ALU.mult), reads=[acc], writes=[oa])
            P.dma("sp", self.oT_d[s * 128:(s + 1) * 128, :], oa[:, :], oa, reads=[oa], writes=[self.oT_d])
        P.scope_exit()

    def phase_conv(self, l):
        P, T, NT, NG = self.P, self.T, self.NT, self.NG
        hT, bank, cst = self.hT, self.bank, self.cst
        P.scope_enter()
        uT = P.sb("uT", [128, 4, 32 + T], BF16)
        dg = P.sb("dg", [128, 4, 31, 128], BF16)
        idf = P.sb("idf", [128, 128], F32)
        cv = [P.sb("cv", [128, 512], F32) for _ in range(4)]
        xc = [P.sb("xc", [128, 512], F32) for _ in range(4)]
        sqc = [P.sb("sqc", [128, 512], F32) for _ in range(2)]
        sig = [P.sb("sig", [128, 512], F32) for _ in range(2)]
        rsd = P.sb("rsd", [128, 512], F32)
        ost = [P.sb("ost", [128, 4, 512], BF16) for _ in range(2)]
        onesf = P.sb("onesf", [128, 128], F32)
        convw, convp = self.convw, self.convp
        P.c("pool", lambda e: e.memset(onesf[:], 1.0), writes=[onesf])
        P.c("dve", lambda e: e.tensor_copy(out=idf[:], in_=self.ident()), reads=[self.cb], writes=[idf])
        P.c("pool", lambda e: e.memset(uT[:, :, 0:32], 0.0), writes=[uT])
        for cc in range(4):
            for k in range(31):
                col = (l * 4 + cc) * 31 + k
                P.c("dve", lambda e, cc=cc, k=k, col=col: e.tensor_scalar(out=dg[:, cc, k, :], in0=idf[:], scalar1=convw[:, col:col + 1],
                                                                        scalar2=None, op0=ALU.mult), reads=[idf, convw], writes=[dg])
        for cc in range(4):
            wv_ = self.wchunk(self.w_in, self.win_cols(l, GV + cc * 128))
            wg_ = self.wchunk(self.w_in, self.win_cols(l, GG + cc * 128))
            for tg in range(NG):
                pmv, pmg = bank[tg % 2], bank[2 + tg % 2]
                sg = sig[tg % 2]
                self.proj_fm(wg_, pmg, tg)
                self.proj_fm(wv_, pmv, tg)
                P.c("act", lambda e, pmg=pmg, sg=sg: e.activation(out=sg[:], in_=pmg[:, :], func=AF.Sigmoid), reads=[pmg], writes=[sg])
                P.c("dve", lambda e, pmv=pmv, sg=sg, cc=cc, tg=tg: e.tensor_tensor(out=uT[:, cc, 32 + tg * 512:32 + (tg + 1) * 512], in0=pmv[:, :],
                                                                                 in1=sg[:], op=ALU.mult), reads=[pmv, sg], writes=[uT])
        pcol = lambda which, cc: convp[:, (l * 3 + which) * 4 + cc:(l * 3 + which) * 4 + cc + 1]
        def conv_tile(tg):
            for cc in range(4):
                pm = bank[4 + cc % 2]
                for k in range(31):
                    P.c("pe", lambda e, cc=cc, k=k, pm=pm: e.matmul(pm[:, :], lhsT=dg[:, cc, k, :],
                                                                   rhs=uT[:, cc, 2 + tg * 512 + k:2 + tg * 512 + k + 512],
                                                                   start=(k == 0), stop=(k == 30)), reads=[dg, uT], writes=[pm])
                P.c("act", lambda e, cc=cc, pm=pm: e.activation(out=cv[cc][:], in_=pm[:, :], func=AF.Identity, bias=pcol(0, cc)),
                    reads=[pm, convp], writes=[cv[cc]])
            pmm = bank[6]
            for cc in range(4):
                P.c("pe", lambda e, cc=cc: e.matmul(pmm[:, :], lhsT=onesf[:], rhs=cv[cc][:], start=(cc == 0), stop=(cc == 3)),
                    reads=[onesf, cv[cc]], writes=[pmm])
            for cc in range(4):
                P.c("dve", lambda e, cc=cc: e.scalar_tensor_tensor(out=xc[cc][:], in0=pmm[:, :], scalar=-1.0 / 512, in1=cv[cc][:],
                                                                   op0=ALU.mult, op1=ALU.add), reads=[pmm, cv[cc]], writes=[xc[cc]])
            pmv = bank[7]
            for cc in range(4):
                sq_ = sqc[cc % 2]
                P.c("act", lambda e, cc=cc, sq_=sq_: e.activation(out=sq_[:], in_=xc[cc][:], func=AF.Square), reads=[xc[cc]], writes=[sq_])
                P.c("pe", lambda e, cc=cc, sq_=sq_: e.matmul(pmv[:, :], lhsT=onesf[:], rhs=sq_[:], start=(cc == 0), stop=(cc == 3)),
                    reads=[onesf, sq_], writes=[pmv])
            P.c("act", lambda e: e.activation(out=rsd[:], in_=pmv[:, :], func=AF.Ln, scale=1.0 / 512, bias=cst[:, 0:1]),
                reads=[pmv, cst], writes=[rsd])
            P.c("act", lambda e: e.activation(out=rsd[:], in_=rsd[:], func=AF.Exp, scale=-0.5), reads=[rsd], writes=[rsd])
            os_ = ost[tg % 2]
            for cc in range(4):
                P.c("dve", lambda e, cc=cc: e.tensor_tensor(out=xc[cc][:], in0=xc[cc][:], in1=rsd[:], op=ALU.mult),
                    reads=[xc[cc], rsd], writes=[xc[cc]])
                P.c("dve", lambda e, cc=cc: e.tensor_scalar(out=xc[cc][:], in0=xc[cc][:], scalar1=pcol(1, cc), scalar2=pcol(2, cc),
                                                            op0=ALU.mult, op1=ALU.add), reads=[xc[cc], convp], writes=[xc[cc]])
                P.c("act", lambda e, cc=cc, os_=os_: e.activation(out=os_[:, cc, :], in_=xc[cc][:], func=AF.Silu), reads=[xc[cc]], writes=[os_])
            P.dma("sp", self.oT_d.h[768:1280, tg * 512:(tg + 1) * 512].rearrange("(c p) t -> p c t", p=128), os_[:], os_,
                  reads=[os_], writes=[self.oT_d])

        for tg in range(NG):
            conv_tile(tg)
        P.scope_exit()

    def phase_merge(self, l, x_d, x1_d):
        P, T, NT, NG = self.P, self.T, self.NT, self.NG
        hT, bank = self.hT, self.bank
        P.scope_enter()
        oTt = [P.sb("oTt", [128, 10, 512], BF16) for _ in range(2)]
        sg = [P.sb("sg", [128, 512], F32) for _ in range(3)]
        t1 = [P.sb("t1", [128, 512], F32) for _ in range(3)]
        mst = [P.sb("mst", [128, 512], BF16) for _ in range(2)]
        wbr = [P.sb("wbr", [128, 10, 128], BF16) for _ in range(2)]
        mT_d = self.mT_d
        wsrc = ((self.w_ba, 0, 2), (self.w_bb, 2, 4), (self.w_bc, 6, 4))

        def one(dmc, tg, wb_, wg):
            ot = oTt[tg % 2]
            P.dma("sp", ot[:], self.oT_d.h[:, tg * 512:(tg + 1) * 512].rearrange("(c p) t -> p c t", p=128), ot,
                  reads=[self.oT_d], writes=[ot])
            for b in range(3):
                self.proj_fm(wg[b], bank[b], tg)
                P.c("act", lambda e, b=b: e.activation(out=sg[b][:], in_=bank[b][:, :], func=AF.Sigmoid), reads=[bank[b]], writes=[sg[b]])
            for b, (wt, c0, nchunk) in enumerate(wsrc):
                pm = bank[3 + b]
                for ci in range(nchunk):
                    P.c("pe", lambda e, pm=pm, c0=c0, ci=ci, nchunk=nchunk: e.matmul(pm[:, :], lhsT=wb_[:, c0 + ci, :], rhs=ot[:, c0 + ci, :],
                                                                                   start=(ci == 0), stop=(ci == nchunk - 1)),
                        reads=[wb_, ot], writes=[pm])
                P.c("dve", lambda e, pm=pm, b=b: e.tensor_tensor(out=t1[b][:], in0=pm[:, :], in1=sg[b][:], op=ALU.mult),
                    reads=[pm, sg[b]], writes=[t1[b]])
            P.c("pool", lambda e: e.tensor_tensor(out=t1[0][:], in0=t1[0][:], in1=t1[1][:], op=ALU.add), reads=[t1[0], t1[1]], writes=[t1[0]])
            ms = mst[tg % 2]
            P.c("dve", lambda e, ms=ms: e.tensor_tensor(out=ms[:], in0=t1[0][:], in1=t1[2][:], op=ALU.add), reads=[t1[0], t1[2]], writes=[ms])
            P.dma("act", mT_d[dmc * 128:(dmc + 1) * 128, tg * 512:(tg + 1) * 512], ms[:], ms, reads=[ms], writes=[mT_d])

        for dmc in range(8):
            wb_ = wbr[dmc % 2]
            for (wt, c0, nchunk) in wsrc:
                P.dma("pool", wb_[:, c0:c0 + nchunk, :], wt.h[l].rearrange("(c p) n -> p c n", p=128)[:, :, dmc * 128:(dmc + 1) * 128], wb_,
                      reads=[wt], writes=[wb_])
            wg = [self.wchunk(self.w_in, self.win_cols(l, GATE0 + b * 1024 + dmc * 128)) for b in range(3)]
            for tg in range(NG):
                one(dmc, tg, wb_, wg)
        P.scope_exit()
        P.scope_enter()
        wo = P.sb("wo", [128, 8, 1024], BF16)
        mt = [P.sb("mt", [128, 8, 512], BF16) for _ in range(2)]
        xt = [P.sb("xt2", [128, D], F32) for _ in range(3)]
        for c in range(8):
            P.dma("pool", wo[:, c, :], self.w_out.h[l][c * 128:(c + 1) * 128, :], wo, reads=[self.w_out], writes=[wo])

        def tile(tg, j, mtt):
            i = tg * 4 + j
            x_ = xt[i % 3]
            P.dma("sp", x_[:], x_d[i * 128:(i + 1) * 128, :], x_, reads=[x_d], writes=[x_])
            for half in range(2):
                pm = bank[(2 * i + half) % 4]
                for c in range(8):
                    P.c("pe", lambda e, pm=pm, c=c, half=half: e.matmul(pm[:, :], lhsT=mtt[:, c, j * 128:(j + 1) * 128],
                                                                       rhs=wo[:, c, half * 512:(half + 1) * 512], start=(c == 0), stop=(c == 7)),
                        reads=[mtt, wo], writes=[pm])
                P.c("dve", lambda e, pm=pm, half=half: e.tensor_tensor(out=x_[:, half * 512:(half + 1) * 512], in0=pm[:, :],
                                                                      in1=x_[:, half * 512:(half + 1) * 512], op=ALU.add),
                    reads=[pm, x_], writes=[x_])
            P.dma("act", x1_d[i * 128:(i + 1) * 128, :], x_[:], x_, reads=[x_], writes=[x1_d])

        for tg in range(NG):
            mtt = mt[tg % 2]
            P.dma("sp", mtt[:], mT_d.h[:, tg * 512:(tg + 1) * 512].rearrange("(c p) t -> p c t", p=128), mtt, reads=[mT_d], writes=[mtt])
            for j in range(4):
                tile(tg, j, mtt)
        P.scope_exit()

    def ffn_core(self, xT, xoff, R, wg_src, wu_src, wd_src, sink):
        P, bank = self.P, self.bank
        nf = self.dff // 128
        hid = P.sb("hid", [128, nf, R], BF16)
        wd = [P.sb("wd", [128, nf, 512], BF16) for _ in range(2)]
        sil = [P.sb("sil", [128, 512], F32) for _ in range(2)]
        for half in range(2):
            for f0 in range(0, nf, 11):
                f1 = min(nf, f0 + 11)
                P.dma("pool", wd[half][:, f0:f1, :], wd_src[1][f0 * 128:f1 * 128, half * 512:(half + 1) * 512].rearrange("(f p) n -> p f n", p=128),
                      wd[half], reads=[wd_src[0]], writes=[wd[half]])
        segs = [(r0, min(512, R - r0)) for r0 in range(0, R, 512)]
        cnt = 0
        for f in range(nf):
            wg = self.wchunk(wg_src[0], wg_src[1].rearrange("(c p) n -> p c n", p=128)[:, :, f * 128:(f + 1) * 128])
            wu = self.wchunk(wu_src[0], wu_src[1].rearrange("(c p) n -> p c n", p=128)[:, :, f * 128:(f + 1) * 128])
            for (r0, w) in segs:
                pa, pu = bank[cnt % 2], bank[2 + cnt % 2]
                sl_ = sil[cnt % 2]
                cnt += 1
                for (wt, pm) in ((wg, pa), (wu, pu)):
                    for c in range(8):
                        P.c("pe", lambda e, wt=wt, pm=pm, c=c, r0=r0, w=w: e.matmul(pm[:, 0:w], lhsT=wt[:, c, :], rhs=xT[:, c, xoff + r0:xoff + r0 + w],
                                                                                   start=(c == 0), stop=(c == 7)), reads=[wt, xT], writes=[pm])
                P.c("act", lambda e, pa=pa, sl_=sl_, w=w: e.activation(out=sl_[:, 0:w], in_=pa[:, 0:w], func=AF.Silu), reads=[pa], writes=[sl_])
                P.c("dve", lambda e, pu=pu, sl_=sl_, w=w, r0=r0, f=f: e.tensor_tensor(out=hid[:, f, r0:r0 + w], in0=pu[:, 0:w], in1=sl_[:, 0:w],
                                                                                     op=ALU.mult), reads=[pu, sl_], writes=[hid])
        for j in range(R // 128):
            for half in range(2):
                pm = bank[4 + (2 * j + half) % 4]
                for f in range(nf):
                    P.c("pe", lambda e, pm=pm, f=f, j=j, half=half: e.matmul(pm[:, :], lhsT=hid[:, f, j * 128:(j + 1) * 128], rhs=wd[half][:, f, :],
                                                                            start=(f == 0), stop=(f == nf - 1)), reads=[hid, wd[half]], writes=[pm])
                sink(j, half, pm)

    def phase_ffn_dense(self, x1_d, xo_d):
        P, T = self.P, self.T
        RG = min(1024, T)
        for rg in range(T // RG):
            P.scope_enter()
            xt = [P.sb("xt3", [128, D], F32) for _ in range(3)]

            def sink(j, half, pm, rg=rg, xt=xt):
                i = rg * (RG // 128) + j
                x_ = xt[i % 3]
                if half == 0:
                    P.dma("sp", x_[:], x1_d[i * 128:(i + 1) * 128, :], x_, reads=[x1_d], writes=[x_])
                P.c("dve", lambda e: e.tensor_tensor(out=x_[:, half * 512:(half + 1) * 512], in0=pm[:, :],
                                                     in1=x_[:, half * 512:(half + 1) * 512], op=ALU.add), reads=[pm, x_], writes=[x_])
                if half == 1:
                    P.dma("act", xo_d[i * 128:(i + 1) * 128, :], x_[:], x_, reads=[x_], writes=[xo_d])

            self.ffn_core(self.hT, rg * RG, RG, (self.w_fg, self.w_fg.h[0]), (self.w_fu, self.w_fu.h[0]), (self.w_fd, self.w_fd.h[0]), sink)
            P.scope_exit()

    def phase_moe(self, l, x1_d, xo_d):
        P, T, NT, bank, cst = self.P, self.T, self.NT, self.bank, self.cst
        CAP = self.cfg.get("cap", 1536)
        XS_d = P.dram("XS_d", [NEXP * CAP, D], BF16)
        YS_d = P.dram("YS_d", [NEXP * CAP, D], F32)
        gsm = P.sb("gsm", [128, NT, 2], F32)
        idxs = P.sb("idxs", [128, NT, 2], I32)
        P.scope_enter()
        grow = P.sb("grow", [128, D], F32)
        wrg = P.sb("wrg", [128, NEXP, D], F32)
        brt = P.sb("brt", [128, NEXP], F32)
        ebase = P.sb("ebase", [128, NEXP], F32)
        zt = P.sb("zt", [128, 2, D], BF16)
        xt = [P.sb("xtm", [128, D], F32) for _ in range(2)]
        junk = P.sb("junk", [128, D], F32)
        junk2 = P.sb("junk2", [128, D], F32)
        h2b = [P.sb("h2b", [128, D], BF16) for _ in range(2)]
        sm = [P.sb("smm", [128, 96], F32) for _ in range(2)]
        selb = [P.sb("selb", [128, NEXP], BF16) for _ in range(2)]
        sm2 = [P.sb("smm2", [128, 2], F32) for _ in range(2)]
        selcum = [P.sb("selcum", [128, NEXP], BF16) for _ in range(2)]
        P.dma("sp", grow[:], self.d_grow[:], grow, reads=[self.d_grow], writes=[grow])
        P.dma("sp", wrg[:], self.d_wr.h.rearrange("p (e d) -> p e d", e=NEXP), wrg, reads=[self.d_wr], writes=[wrg])
        P.dma("sp", brt[:], self.d_br[:], brt, reads=[self.d_br], writes=[brt])
        P.dma("sp", ebase[:], self.d_ebase[:], ebase, reads=[self.d_ebase], writes=[ebase])
        for e_ in range(NEXP):
            P.c("pool", lambda e, e_=e_: e.tensor_tensor(out=wrg[:, e_, :], in0=wrg[:, e_, :], in1=grow[:], op=ALU.mult),
                reads=[wrg, grow], writes=[wrg])
        P.c("pool", lambda e: e.memset(zt[:], 0.0), writes=[zt])
        P.c("pool", lambda e: e.memset(selcum[1][:], 0.0), writes=[selcum[1]])
        for a in range(NEXP * CAP // 256):
            P.dma("sp", XS_d.h[a * 256:(a + 1) * 256, :].rearrange("(a p) n -> p a n", p=128), zt[:], zt, reads=[zt], writes=[XS_d])

        def route(i):
            x_, hb_, s_, sb_ = xt[i % 2], h2b[i % 2], sm[i % 2], selb[i % 2]
            s2_ = sm2[i % 2]
            sc_new, sc_old = selcum[i % 2], selcum[(i + 1) % 2]
            col = lambda a, b=None: s_[:, a:(a + 1 if b is None else b)]
            P.dma("sp", x_[:], x1_d[i * 128:(i + 1) * 128, :], x_, reads=[x1_d], writes=[x_])
            P.c("act", lambda e: e.activation(out=junk[:], in_=x_[:], func=AF.Square, accum_out=col(0)), reads=[x_], writes=[junk, s_])
            P.c("act", lambda e: e.activation(out=col(1), in_=col(0), func=AF.Ln, scale=1.0 / D, bias=cst[:, 0:1]), reads=[s_, cst], writes=[s_])
            P.c("act", lambda e: e.activation(out=col(1), in_=col(1), func=AF.Exp, scale=-0.5), reads=[s_], writes=[s_])
            P.c("dve", lambda e: e.scalar_tensor_tensor(out=hb_[:], in0=x_[:], scalar=col(1), in1=grow[:], op0=ALU.mult, op1=ALU.mult),
                reads=[x_, s_, grow], writes=[hb_])
            for e_ in range(NEXP):
                P.c("dve", lambda e, e_=e_: e.scalar_tensor_tensor(out=junk[:], in0=x_[:], scalar=col(1), in1=wrg[:, e_, :], op0=ALU.mult,
                                                                   op1=ALU.mult, accum_out=col(8 + e_)), reads=[x_, s_, wrg], writes=[junk, s_])
            if True:
                pass
            lg, eq1, lg2, eq2, dest, tmp8 = col(8, 16), col(16, 24), col(24, 32), col(32, 40), col(40, 48), col(48, 56)
            P.c("dve", lambda e: e.tensor_tensor(out=lg, in0=lg, in1=brt[:], op=ALU.add), reads=[s_, brt], writes=[s_])
            P.c("dve", lambda e: e.reduce_max(out=col(2), in_=lg, axis=AX.X), reads=[s_], writes=[s_])
            P.c("dve", lambda e: e.tensor_scalar(out=eq1, in0=lg, scalar1=col(2), scalar2=None, op0=ALU.is_equal), reads=[s_], writes=[s_])
            P.c("dve", lambda e: e.scalar_tensor_tensor(out=lg2, in0=eq1, scalar=-1e30, in1=lg, op0=ALU.mult, op1=ALU.add), reads=[s_], writes=[s_])
            P.c("dve", lambda e: e.reduce_max(out=col(3), in_=lg2, axis=AX.X), reads=[s_], writes=[s_])
            P.c("dve", lambda e: e.tensor_scalar(out=eq2, in0=lg2, scalar1=col(3), scalar2=None, op0=ALU.is_equal), reads=[s_], writes=[s_])
            P.c("dve", lambda e: e.tensor_scalar(out=col(4), in0=col(2), scalar1=-1.0, scalar2=None, op0=ALU.mult), reads=[s_], writes=[s_])
            P.c("act", lambda e: e.activation(out=col(5), in_=col(3), func=AF.Exp, bias=col(4)), reads=[s_], writes=[s_])
            P.c("dve", lambda e: e.tensor_scalar(out=col(6), in0=col(5), scalar1=1.0, scalar2=None, op0=ALU.add), reads=[s_], writes=[s_])
            P.c("dve", lambda e: e.reciprocal(out=col(6), in_=col(6)), reads=[s_], writes=[s_])
            P.c("dve", lambda e: e.tensor_copy(out=gsm[:, i, 0:1], in_=col(6)), reads=[s_], writes=[gsm])
            P.c("dve", lambda e: e.tensor_tensor(out=gsm[:, i, 1:2], in0=col(5), in1=col(6), op=ALU.mult), reads=[s_], writes=[gsm])
            P.c("dve", lambda e: e.tensor_tensor(out=sb_[:], in0=eq1, in1=eq2, op=ALU.add), reads=[s_], writes=[sb_])
            pc = bank[i % 2]
            P.c("pe", lambda e: e.matmul(pc[:, 0:NEXP], lhsT=self.ones(), rhs=sb_[:], start=True, stop=False), reads=[self.cb, sb_], writes=[pc])
            P.c("pe", lambda e: e.matmul(pc[:, 0:NEXP], lhsT=self.tri(), rhs=sb_[:], start=False, stop=False), reads=[self.cb, sb_], writes=[pc])
            P.c("pe", lambda e: e.matmul(pc[:, 0:NEXP], lhsT=self.ones(), rhs=sc_old[:], start=False, stop=True), reads=[self.cb, sc_old], writes=[pc])
            P.c("dve", lambda e: e.tensor_tensor(out=sc_new[:], in0=sc_old[:], in1=sb_[:], op=ALU.add), reads=[sc_old, sb_], writes=[sc_new])
            P.c("dve", lambda e: e.scalar_tensor_tensor(out=dest, in0=pc[:, 0:NEXP], scalar=float(CAP - 1), in1=ebase[:], op0=ALU.min, op1=ALU.add),
                reads=[pc, ebase], writes=[s_])
            for kk, eq in enumerate((eq1, eq2)):
                P.c("dve", lambda e, eq=eq: e.tensor_tensor(out=tmp8, in0=eq, in1=dest, op=ALU.mult), reads=[s_], writes=[s_])
                P.c("dve", lambda e, kk=kk: e.reduce_sum(out=col(56 + kk), in_=tmp8, axis=AX.X), reads=[s_], writes=[s_])
                P.c("dve", lambda e, kk=kk: e.tensor_copy(out=idxs[:, i, kk:kk + 1], in_=col(56 + kk)), reads=[s_], writes=[idxs])
            for kk in range(2):
                P.idma(XS_d[:, :], bass.IndirectOffsetOnAxis(ap=idxs[:, i, kk:kk + 1], axis=0), hb_[:], None, hb_,
                       reads=[hb_, idxs], writes=[XS_d])

        for i in range(NT):
            route(i)
        P.scope_exit()
        for ex in range(NEXP):
            self.moe_expert(ex, CAP, XS_d, YS_d)
        P.scope_enter()
        xt = [P.sb("xtc", [128, D], F32) for _ in range(2)]
        yg = [[P.sb("yg", [128, D], F32) for _ in range(2)] for _ in range(2)]

        def comb(i):
            x_ = xt[i % 2]
            P.dma("sp", x_[:], x1_d[i * 128:(i + 1) * 128, :], x_, reads=[x1_d], writes=[x_])
            for kk in range(2):
                y_ = yg[kk][i % 2]
                P.idma(y_[:], None, YS_d[:, :], bass.IndirectOffsetOnAxis(ap=idxs[:, i, kk:kk + 1], axis=0), y_,
                       reads=[YS_d, idxs], writes=[y_])
                P.c("dve", lambda e, y_=y_, kk=kk: e.scalar_tensor_tensor(out=x_[:], in0=y_[:], scalar=gsm[:, i, kk:kk + 1], in1=x_[:],
                                                                          op0=ALU.mult, op1=ALU.add), reads=[y_, gsm, x_], writes=[x_])
            P.dma("act", xo_d[i * 128:(i + 1) * 128, :], x_[:], x_, reads=[x_], writes=[xo_d])

        for i in range(NT):
            comb(i)
        P.scope_exit()

    def moe_expert(self, ex, CAP, XS_d, YS_d):
        P, bank = self.P, self.bank
        P.scope_enter()
        xsT = P.sb("xsT", [128, 8, CAP], BF16)
        xr = [P.sb("xr", [128, D], BF16) for _ in range(2)]
        ys = [P.sb("ys", [128, D], F32) for _ in range(2)]
        for j in range(CAP // 128):
            x_ = xr[j % 2]
            r0 = ex * CAP + j * 128
            P.dma("sp", x_[:], XS_d[r0:r0 + 128, :], x_, reads=[XS_d], writes=[x_])
            pt = bank[6 + j % 2]
            ptv = pt[:].bitcast(BF16)
            for c in range(8):
                P.c("pe", lambda e, c=c, x_=x_, ptv=ptv: e.transpose(out=ptv[:, c * 128:(c + 1) * 128], in_=x_[:, c * 128:(c + 1) * 128],
                                                                   identity=self.ident()), reads=[x_, self.cb], writes=[pt])
            eng = "dve" if j % 2 == 0 else "act"
            if eng == "dve":
                P.c("dve", lambda e, j=j, ptv=ptv: e.tensor_copy(out=xsT[:, :, j * 128:(j + 1) * 128], in_=ptv[:, :].rearrange("p (c n) -> p c n", c=8)),
                    reads=[pt], writes=[xsT])
            else:
                P.c("act", lambda e, j=j, ptv=ptv: e.copy(out=xsT[:, :, j * 128:(j + 1) * 128], in_=ptv[:, :].rearrange("p (c n) -> p c n", c=8)),
                    reads=[pt], writes=[xsT])

        def sink(j, half, pm):
            y_ = ys[j % 2]
            if half == 0:
                P.c("act", lambda e: e.copy(out=y_[:, 0:512], in_=pm[:, :]), reads=[pm], writes=[y_])
            else:
                P.c("dve", lambda e: e.tensor_copy(out=y_[:, 512:1024], in_=pm[:, :]), reads=[pm], writes=[y_])
                r0 = ex * CAP + j * 128
                P.dma("act", YS_d[r0:r0 + 128, :], y_[:], y_, reads=[y_], writes=[YS_d])

        self.ffn_core(xsT, 0, CAP, (self.w_eg, self.w_eg.h[0][ex]), (self.w_eu, self.w_eu.h[0][ex]), (self.w_ed, self.w_ed.h[0][ex]), sink)
        P.scope_exit()


def _consts():
    i = np.arange(128)
    ident = np.eye(128, dtype=np.float32)
    tri = -(i[:, None] >= i[None, :]).astype(np.float32)
    negones = -np.ones((128, 128), np.float32)
    ones = np.ones((128, 128), np.float32)
    blockones = ((i[:, None] // 64) == (i[None, :] // 64)).astype(np.float32)
    zeros = np.zeros((128, 128), np.float32)
    sbmask = np.where(i[:, None] >= i[None, :], NEG, 0.0).astype(np.float32)
    cb = np.concatenate([ident, tri, negones, ones, blockones, zeros, sbmask], axis=1).astype(ml_dtypes.bfloat16)
    slopes = 2.0 ** (-8.0 * np.arange(1, 13, dtype=np.float32) / 12.0)
    bt = np.zeros((128, 12, 2, 128), np.float32)
    for H in range(12):
        d = DIL[H // 4]
        for half in range(2):
            dist = (half * 128 + i[None, :] - i[:, None]).astype(np.float32)
            valid = (dist >= 0) & (dist <= 128)
            bt[:, H, half, :] = np.where(valid, -slopes[H] * dist * d, NEG)
    return cb, bt.reshape(128, 12 * 256).astype(np.float32)


def host_inputs(inp):
    L = DEPTH
    f = lambda a: np.ascontiguousarray(np.asarray(a, dtype=np.float32))
    g = np.stack([f(inp["attn_norm_g"]), f(inp["ffn_norm_g"])], axis=1)
    gcols = g.reshape(L, 2, 8, 128).transpose(3, 0, 1, 2).reshape(128, L * 2 * 8)
    qk = np.stack([f(inp["q_norm_g"]), f(inp["k_norm_g"])], axis=1)
    qkg = np.concatenate([qk, qk], axis=2).transpose(2, 0, 1).reshape(128, L * 2)
    qkrow = np.broadcast_to(qk.reshape(1, L * 2 * 64), (128, L * 2 * 64))
    cw = f(inp["conv_w"])
    convw = cw.reshape(L, 31, 4, 128).transpose(3, 0, 2, 1).reshape(128, L * 4 * 31)
    cp = np.stack([f(inp["conv_b"]), f(inp["conv_norm_g"]), f(inp["conv_norm_b"])], axis=1)
    convp = cp.reshape(L, 3, 4, 128).transpose(3, 0, 1, 2).reshape(128, L * 3 * 4)
    wr = f(inp["w_router"])[0]
    wr_bc = np.broadcast_to(wr.T.reshape(1, NEXP * D), (128, NEXP * D))
    br_bc = np.broadcast_to(f(inp["b_router"])[0].reshape(1, NEXP), (128, NEXP))
    cb, bt = _consts()
    cap = 1536
    grow = np.broadcast_to(f(inp["ffn_norm_g"])[1].reshape(1, D), (128, D))
    ebase = np.broadcast_to((np.arange(NEXP, dtype=np.float32) * cap).reshape(1, NEXP), (128, NEXP))
    m = dict(grow=grow, ebase=ebase, gcols=gcols, qkg=qkg, qkrow=qkrow, convw=convw, convp=convp, wr_bc=wr_bc, br_bc=br_bc, cbf=cb, btab=bt)
    m = {k: np.ascontiguousarray(v) for k, v in m.items()}
    for k in ("w_in", "w_branch_a", "w_branch_b", "w_branch_c", "w_out", "w_ffn_gate", "w_ffn_up", "w_ffn_down",
              "w_exp_gate", "w_exp_up", "w_exp_down"):
        m[k] = f(inp[k])
    return m


_CACHE = {}


def kernel(**inputs):
    m = host_inputs(inputs)
    x = np.asarray(inputs["x"], dtype=np.float32)
    nb = x.shape[0]
    if "k" not in _CACHE:
        kk = K()
        kk.build_full()
        _CACHE["k"] = kk
    kk = _CACHE["k"]
    in_maps = []
    for b in range(nb):
        mm = dict(m)
        mm["x"] = np.ascontiguousarray(x[b])
        in_maps.append(mm)
    res = run_bass_kernel_spmd(kk.nc, in_maps, core_ids=list(range(nb)))
    return np.stack([np.asarray(r["out"], dtype=np.float32) for r in res.results], axis=0)
```

```python
import numpy as np
import ml_dtypes
import concourse.bass as bass
import concourse.mybir as mybir
from concourse.bass_utils import run_bass_kernel_spmd

F32 = mybir.dt.float32
BF16 = mybir.dt.bfloat16
I32 = mybir.dt.int32
ALU = mybir.AluOpType
AF = mybir.ActivationFunctionType
AX = mybir.AxisListType

D = 1024
SEQ = 4096
DEPTH = 2
IN_COLS = 7936
DFF = 2816
NF = DFF // 128
NEXP = 8
DIL = (1, 4, 16)
QA, KA, VA = 0, 768, 1536
QB, KB_, VB = 2304, 2816, 3328
GV, GG = 3840, 4352
GATE0 = 4864
NEG = -30000.0


class Tl:
    _n = 0

    def __init__(self, h, name):
        self.h = h
        self.name = name
        self.w = set()
        self.r = set()
        Tl._n += 1
        self.id = Tl._n
        self.born = 0
        self.freed = None

    def __getitem__(self, idx):
        return self.h[idx]


class Prog:
    STREAMS = ("pe", "act", "dve", "pool", "sp")

    def __init__(self, nc):
        self.nc = nc
        self.ops = []
        self.streams = {s: [] for s in self.STREAMS}
        self.semorder = {}
        self.n_t = 0
        self.tiles = {}
        self.ghost = {}
        self.scopes = []

    def sb(self, name, shape, dt):
        self.n_t += 1
        t = Tl(self.nc.alloc_sbuf_tensor(f"{name}_{self.n_t}", list(shape), dt), name)
        t.w = set(self.ghost.values())
        t.born = len(self.ops)
        self.tiles[t.id] = t
        if self.scopes:
            self.scopes[-1][1].append(t)
        return t

    def scope_enter(self):
        g = self.nc.reset_on_exit()
        g.__enter__()
        self.scopes.append((g, []))

    def scope_exit(self):
        g, tiles = self.scopes.pop()
        g.__exit__(None, None, None)
        for t in tiles:
            t.freed = len(self.ops)
            for oid in (t.w | t.r):
                op = self.ops[oid]
                k = op["semkey"]
                if k not in self.ghost or self.ops[self.ghost[k]]["pos"] < op["pos"]:
                    self.ghost[k] = oid

    def ps(self, name, shape, dt=F32):
        self.n_t += 1
        t = Tl(self.nc.alloc_psum_tensor(f"{name}_{self.n_t}", list(shape), dt), name)
        t.excl = True
        return t

    def dram(self, name, shape, dt, kind="Internal"):
        return Tl(self.nc.dram_tensor(name, list(shape), dt, kind=kind), name)

    def _add(self, stream, fn, reads, writes, semkey, inc):
        oid = len(self.ops)
        raw = set()
        rar = set()
        for t in reads:
            raw |= t.w
            if getattr(t, "excl", False):
                rar |= t.r
        deps = set(raw) | rar
        waw, war = set(), set()
        for t in writes:
            waw |= t.w
            war |= t.r
        deps |= waw | war
        is_dma = inc == 16
        keep = set()
        for d in deps:
            od = self.ops[d]
            if od["semkey"] == semkey and d not in raw:
                if not is_dma:
                    continue
                if d in waw and d not in war:
                    continue
            keep.add(d)
        op = dict(id=oid, stream=stream, fn=fn, deps=keep, semkey=semkey, inc=inc)
        self.ops.append(op)
        self.streams[stream].append(oid)
        self.semorder.setdefault(semkey, []).append(oid)
        op["pos"] = len(self.semorder[semkey]) - 1
        for t in reads:
            t.r.add(oid)
        for t in writes:
            t.w = {oid}
            t.r = set()
        return oid

    def c(self, eng, fn, reads=(), writes=()):
        return self._add(eng, fn, reads, writes, eng, 1)

    def dma(self, stream, out_ap, in_ap, semtile, reads=(), writes=(), **kw):
        fn = lambda e: e.dma_start(out=out_ap, in_=in_ap, **kw)
        return self._add(stream, fn, reads, writes, ("dma", semtile.id, stream == "pool"), 16)

    def idma(self, out_ap, out_off, in_ap, in_off, semtile, reads=(), writes=()):
        fn = lambda e: e.indirect_dma_start(out=out_ap, out_offset=out_off, in_=in_ap, in_offset=in_off)
        return self._add("pool", fn, reads, writes, ("dma", semtile.id, True), 16)

    def finish(self, stream, tiles):
        oid = self._add(stream, None, list(tiles), [], ("fin", stream), 0)
        for k, lst in self.semorder.items():
            if not isinstance(k, str) and k[0] == "dma":
                self.ops[oid]["deps"].add(lst[-1])
        return oid

    def emit(self):
        nc = self.nc
        ops = self.ops
        seen = {s: {} for s in self.STREAMS}
        needed = {}
        for op in ops:
            waits = {}
            for d in op["deps"]:
                od = ops[d]
                k = od["semkey"]
                if od["pos"] > waits.get(k, -1):
                    waits[k] = od["pos"]
            sn = seen[op["stream"]]
            w2 = []
            for k, p in waits.items():
                if sn.get(k, -1) >= p:
                    continue
                sn[k] = p
                needed.setdefault(k, set()).add(p)
                w2.append((k, p))
            op["waits"] = w2
        semval = {}
        sems = {}
        for k, lst in self.semorder.items():
            if not isinstance(k, str) or k not in needed:
                continue
            sems[k] = nc.alloc_semaphore("s_%s" % k)
            cnt = 0
            nd = needed[k]
            for p, oid in enumerate(lst):
                if p in nd:
                    cnt += 1
                    ops[oid]["do_inc"] = True
                semval[(k, p)] = cnt
        INF = 1 << 60
        dkeys = [k for k in self.semorder if not isinstance(k, str) and k[0] == "dma"]
        dkeys.sort(key=lambda k: self.tiles[k[1]].born)
        pools = {True: [], False: []}
        nphys = 0
        for k in dkeys:
            tl = self.tiles[k[1]]
            ent = None
            for cand in pools[k[2]]:
                if cand[2] <= tl.born:
                    ent = cand
                    break
            if ent is None:
                ent = [nc.alloc_semaphore("s_d%d" % nphys), 0, INF]
                nphys += 1
                pools[k[2]].append(ent)
            sems[k] = ent[0]
            base = ent[1]
            lst = self.semorder[k]
            for p, oid in enumerate(lst):
                ops[oid]["do_inc"] = True
                semval[(k, p)] = base + 16 * (p + 1)
            ent[1] = base + 16 * len(lst)
            ent[2] = tl.freed if tl.freed is not None else INF
        self.n_sems = len(sems)
        self.max_semval = max(semval.values()) if semval else 0
        engmap = dict(pe="tensor", act="scalar", dve="vector", pool="gpsimd", sp="sync")

        def run_stream(s):
            def body(e):
                for oid in self.streams[s]:
                    op = ops[oid]
                    for k, p in op["waits"]:
                        e.wait_ge(sems[k], semval[(k, p)])
                    if op["fn"] is None:
                        continue
                    ins = op["fn"](e)
                    if op.get("do_inc"):
                        ins.then_inc(sems[op["semkey"]], op["inc"])
            return body

        with nc.Block() as block:
            for s in self.STREAMS:
                if self.streams[s]:
                    getattr(block, engmap[s])(run_stream(s))


def sl(start, count, step=1):
    return slice(start, start + (count - 1) * step + 1, step)


def rr(gens):
    gens = list(gens)
    while gens:
        nxt = []
        for g in gens:
            try:
                next(g)
                nxt.append(g)
            except StopIteration:
                pass
        gens = nxt


class K:
    def __init__(self, T=SEQ, cfg=None):
        self.T = T
        self.cfg = cfg or {}
        self.nc = bass.Bass("TRN2", target_bir_lowering=False)
        self.P = Prog(self.nc)
        self.NT = T // 128
        self.NG = T // 512
        self._wq = 0
        self.setup()

    def setup(self):
        P, T = self.P, self.T
        L = DEPTH
        DFF = self.dff = self.cfg.get("dff", 2816)
        ein = lambda n, s, d: P.dram(n, s, d, kind="ExternalInput")
        self.x_in = ein("x", [T, D], F32)
        self.w_in = ein("w_in", [L, D, IN_COLS], F32)
        self.w_ba = ein("w_branch_a", [L, 256, D], F32)
        self.w_bb = ein("w_branch_b", [L, 512, D], F32)
        self.w_bc = ein("w_branch_c", [L, 512, D], F32)
        self.w_out = ein("w_out", [L, D, D], F32)
        self.w_fg = ein("w_ffn_gate", [1, D, DFF], F32)
        self.w_fu = ein("w_ffn_up", [1, D, DFF], F32)
        self.w_fd = ein("w_ffn_down", [1, DFF, D], F32)
        if self.cfg.get("moe", True):
            self.w_eg = ein("w_exp_gate", [1, NEXP, D, DFF], F32)
            self.w_eu = ein("w_exp_up", [1, NEXP, D, DFF], F32)
            self.w_ed = ein("w_exp_down", [1, NEXP, DFF, D], F32)
        self.d_gcols = ein("gcols", [128, L * 2 * 8], F32)
        self.d_qkg = ein("qkg", [128, L * 2], F32)
        self.d_qkrow = ein("qkrow", [128, L * 2 * 64], F32)
        self.d_convw = ein("convw", [128, L * 4 * 31], F32)
        self.d_convp = ein("convp", [128, L * 3 * 4], F32)
        self.d_wr = ein("wr_bc", [128, NEXP * D], F32)
        self.d_br = ein("br_bc", [128, NEXP], F32)
        self.d_grow = ein("grow", [128, D], F32)
        self.d_ebase = ein("ebase", [128, NEXP], F32)
        self.d_cb = ein("cbf", [128, 7 * 128], BF16)
        self.d_bt = ein("btab", [128, 12 * 256], F32)
        self.out = P.dram("out", [T, D], F32, kind="ExternalOutput")
        self.xs = [P.dram("xs0", [T, D], F32), P.dram("xs1", [T, D], F32)]
        self.mT_d = P.dram("mT_d", [D, T], BF16)
        self.oT_d = P.dram("oT_d", [1280, T], BF16, kind="ExternalOutput" if self.cfg.get("dbg_oT") else "Internal")
        self.dbg = {}

        def ld(name, d, shape, dt):
            t = P.sb(name, shape, dt)
            P.dma("sp", t[:], d[:], t, reads=[d], writes=[t])
            return t

        self.gcols = ld("gcols", self.d_gcols, [128, L * 2 * 8], F32)
        self.qkg = ld("qkg", self.d_qkg, [128, L * 2], F32)
        self.qkrow = ld("qkrow", self.d_qkrow, [128, L * 2 * 64], F32)
        self.convw = ld("convw", self.d_convw, [128, L * 4 * 31], F32)
        self.convp = ld("convp", self.d_convp, [128, L * 3 * 4], F32)
        self.cb = ld("cb", self.d_cb, [128, 7 * 128], BF16)
        self.bt = ld("bt", self.d_bt, [128, 12 * 256], F32)
        cbv = lambda i: (lambda: self.cb[:, i * 128:(i + 1) * 128])
        self.ident, self.tri, self.negones, self.ones, self.blockones, self.zeros, self.sbmask = [cbv(i) for i in range(7)]
        self.cst = P.sb("cst", [128, 4], F32)
        P.c("pool", lambda e: e.memset(self.cst[:, 0:1], 1e-6), writes=[self.cst])
        P.c("pool", lambda e: e.memset(self.cst[:, 1:2], 1.0), writes=[self.cst])
        P.c("pool", lambda e: e.memset(self.cst[:, 2:3], 0.0), writes=[self.cst])
        self.bank = [P.ps("bank%d" % i, [128, 512], F32) for i in range(8)]
        self.wbufs = [P.sb("wb", [128, 8, 128], BF16) for _ in range(6)]

    def build_full(self, n_layers=DEPTH):
        P = self.P
        x_cur = self.x_in
        for l in range(n_layers):
            P.scope_enter()
            self.hT = P.sb("hT", [128, 8, self.T], BF16)
            self.phase_norm(x_cur, (l * 2 + 0) * 8)
            self.phase_sb(l)
            self.phase_dil(l)
            self.phase_conv(l)
            x1_d = self.xs[0]
            self.phase_merge(l, x_cur, x1_d)
            xo = self.out if l == n_layers - 1 else self.xs[1]
            if l % 2 == 0:
                self.phase_norm(x1_d, (l * 2 + 1) * 8)
                self.phase_ffn_dense(x1_d, xo)
                P.scope_exit()
            else:
                P.scope_exit()
                self.phase_moe(l, x1_d, xo)
            x_cur = xo
        P.finish("sp", [self.out])
        P.emit()

    def dbg_out(self, name, shape, dt=F32):
        t = self.P.dram(name, shape, dt, kind="ExternalOutput")
        self.dbg[name] = t
        return t

    def wchunk(self, src_tl, src_ap):
        wb = self.wbufs[self._wq % len(self.wbufs)]
        self._wq += 1
        self.P.dma("pool", wb[:], src_ap, wb, reads=[src_tl], writes=[wb])
        return wb

    def win_cols(self, l, c0, n=128):
        return self.w_in.h[l].rearrange("(c p) n -> p c n", p=128)[:, :, c0:c0 + n]

    def phase_norm(self, x_d, gcol_off):
        P, T = self.P, self.T
        P.scope_enter()
        na = dict(
            xt=[P.sb("xt", [128, D], F32) for _ in range(2)],
            sq=P.sb("sq", [128, D], F32),
            ss=[P.sb("ss", [128, 1], F32) for _ in range(2)],
            rs=[P.sb("rs", [128, 1], F32) for _ in range(2)],
            hb=[P.sb("hb", [128, D], BF16) for _ in range(2)],
        )
        hT, gcols, cst = self.hT, self.gcols, self.cst
        for i in range(self.NT):
            b = i % 2
            xt, ss, rs, hb, sq = na["xt"][b], na["ss"][b], na["rs"][b], na["hb"][b], na["sq"]
            P.dma("sp", xt[:], x_d[i * 128:(i + 1) * 128, :], xt, reads=[x_d], writes=[xt])
            P.c("act", lambda e, xt=xt, ss=ss: e.activation(out=sq[:], in_=xt[:], func=AF.Square, accum_out=ss[:]),
                reads=[xt], writes=[sq, ss])
            P.c("act", lambda e, ss=ss, rs=rs: e.activation(out=rs[:], in_=ss[:], func=AF.Ln, scale=1.0 / D, bias=cst[:, 0:1]),
                reads=[ss, cst], writes=[rs])
            P.c("act", lambda e, rs=rs: e.activation(out=rs[:], in_=rs[:], func=AF.Exp, scale=-0.5),
                reads=[rs], writes=[rs])
            P.c("dve", lambda e, xt=xt, rs=rs, hb=hb: e.tensor_scalar(out=hb[:], in0=xt[:], scalar1=rs[:, 0:1], scalar2=None,
                                                                   op0=ALU.mult), reads=[xt, rs], writes=[hb])
            pt = self.bank[i % 2]
            ptv = pt[:].bitcast(BF16)
            for c in range(8):
                P.c("pe", lambda e, ptv=ptv, c=c, hb=hb: e.transpose(out=ptv[:, c * 128:(c + 1) * 128],
                                                                     in_=hb[:, c * 128:(c + 1) * 128], identity=self.ident()),
                    reads=[hb, self.cb], writes=[pt])
            for c in range(8):
                eng = "dve" if c % 2 == 0 else "pool"
                eng = "dve"
                P.c(eng, lambda e, ptv=ptv, c=c, i=i: e.tensor_scalar(
                    out=hT[:, c, i * 128:(i + 1) * 128], in0=ptv[:, c * 128:(c + 1) * 128],
                    scalar1=gcols[:, gcol_off + c:gcol_off + c + 1], scalar2=None, op0=ALU.mult),
                    reads=[pt, gcols], writes=[hT])
        P.scope_exit()

    def proj_fm(self, wb, pm, tg, wcol0=0, m=128):
        P, hT = self.P, self.hT
        for c in range(8):
            P.c("pe", lambda e, c=c: e.matmul(pm[0:m, :], lhsT=wb[:, c, wcol0:wcol0 + m], rhs=hT[:, c, tg * 512:(tg + 1) * 512],
                                              start=(c == 0), stop=(c == 7)), reads=[wb, hT], writes=[pm])

    def phase_sb(self, l):
        P, T, NT, NG = self.P, self.T, self.NT, self.NG
        hT, bank = self.hT, self.bank
        P.scope_enter()
        if True:
            NCH = 4
            self.sbb = dict(
                vall=P.sb("vall", [128, NT, 512], BF16),
                wv=P.sb("wv", [128, 8, 512], BF16),
                qT=P.sb("qT", [128, T], BF16),
                kT=P.sb("kT", [128, T], BF16),
                obT=P.sb("obT", [128, T], BF16),
                ch=[dict(e=P.sb("e", [128, 512], F32),
                         sp=[P.sb("sp", [128, 512], BF16) for _ in range(2)],
                         a=[P.sb("a", [128, 512], BF16) for _ in range(2)],
                         r32=P.sb("r32", [128, 512], F32),
                         rb=[P.sb("rb", [128, 512], BF16) for _ in range(2)],
                         zb=bank[2 * i], ob=bank[2 * i + 1]) for i in range(NCH)],
            )
        S = self.sbb
        vall, wv, qT, kT, obT = S["vall"], S["wv"], S["qT"], S["kT"], S["obT"]
        cst = self.cst
        P.dma("pool", wv[:], self.win_cols(l, VB, 512), wv, reads=[self.w_in], writes=[wv])
        for i in range(NT):
            pm = bank[i % 2]
            for c in range(8):
                P.c("pe", lambda e, c=c, i=i, pm=pm: e.matmul(pm[:, :], lhsT=hT[:, c, i * 128:(i + 1) * 128], rhs=wv[:, c, :],
                                                             start=(c == 0), stop=(c == 7)), reads=[hT, wv], writes=[pm])
            if i % 2 == 0:
                P.c("act", lambda e, i=i, pm=pm: e.copy(out=vall[:, i, :], in_=pm[:, :]), reads=[pm], writes=[vall])
            else:
                P.c("dve", lambda e, i=i, pm=pm: e.tensor_copy(out=vall[:, i, :], in_=pm[:, :]), reads=[pm], writes=[vall])

        def chain(h, Q, C):
            prow = (h % 2) * 64
            hp = h // 2
            zb, ob, esb, r32 = C["zb"], C["ob"], C["e"], C["r32"]
            rows = slice(prow, prow + 64)
            q0 = Q * 512
            P.c("pe", lambda e: e.matmul(ob[:, :], lhsT=self.zeros(), rhs=qT[:, q0:q0 + 512], start=True, stop=False),
                reads=[self.cb, qT], writes=[ob])
            P.c("pool", lambda e: e.memset(r32[:], 0.0), writes=[r32])
            yield
            kbs = list(range(4 * Q + 3, -1, -1))
            for ti, kb in enumerate(kbs):
                j = max(kb - 4 * Q, 0)
                c0 = 128 * j
                diag = kb >= 4 * Q
                first = ti == 0
                last = kb == 0
                sp, a, rb = C["sp"][ti % 2], C["a"][ti % 2], C["rb"][ti % 2]
                rbp = C["rb"][(ti + 1) % 2]
                P.c("pe", lambda e, kb=kb, c0=c0: e.matmul(zb[:, c0:512], lhsT=kT[rows, kb * 128:(kb + 1) * 128],
                                                           rhs=qT[rows, q0 + c0:q0 + 512], start=True, stop=False, skip_group_check=True),
                    reads=[kT, qT], writes=[zb])
                if diag:
                    P.c("pe", lambda e, c0=c0: e.matmul(zb[:, c0:c0 + 128], lhsT=self.ident(), rhs=self.sbmask(),
                                                        start=False, stop=False, skip_group_check=True), reads=[self.cb], writes=[zb])
                yield
                P.c("act", lambda e, c0=c0: e.activation(out=esb[:, c0:512], in_=zb[:, c0:512], func=AF.Exp),
                    reads=[zb], writes=[esb])
                yield
                P.c("act", lambda e, c0=c0, sp=sp: e.activation(out=sp[:, c0:512], in_=esb[:, c0:512], func=AF.Ln, bias=cst[:, 1:2]),
                    reads=[esb, cst], writes=[sp])
                yield
                P.c("pe", lambda e, c0=c0, sp=sp, first=first: e.matmul(zb[:, c0:512], lhsT=self.tri(), rhs=sp[:, c0:512],
                                                                        start=False, stop=first, skip_group_check=True),
                    reads=[self.cb, sp], writes=[zb])
                if not first:
                    P.c("pe", lambda e, c0=c0, rbp=rbp: e.matmul(zb[:, c0:512], lhsT=self.negones(), rhs=rbp[:, c0:512],
                                                                 start=False, stop=True, skip_group_check=True),
                        reads=[self.cb, rbp], writes=[zb])
                if not last:
                    P.c("dve", lambda e, c0=c0, sp=sp: e.tensor_tensor(out=r32[:, c0:512], in0=r32[:, c0:512], in1=sp[:, c0:512],
                                                                       op=ALU.add), reads=[r32, sp], writes=[r32])
                    P.c("dve", lambda e, rb=rb: e.tensor_copy(out=rb[:, :], in_=r32[:, :]), reads=[r32], writes=[rb])
                yield
                P.c("act", lambda e, c0=c0, a=a: e.activation(out=a[:, c0:512], in_=zb[:, c0:512], func=AF.Exp),
                    reads=[zb], writes=[a])
                yield
                P.c("pe", lambda e, c0=c0, a=a, kb=kb, last=last: e.matmul(ob[:, c0:512], lhsT=vall[:, kb, hp * 128:(hp + 1) * 128],
                                                                          rhs=a[:, c0:512], start=False, stop=last),
                    reads=[vall, a], writes=[ob])
                yield
            P.c("dve", lambda e: e.tensor_copy(out=obT[rows, q0:q0 + 512], in_=ob[rows, :]), reads=[ob], writes=[obT])
            yield

        for hp in range(4):
            wq = self.wchunk(self.w_in, self.win_cols(l, QB + hp * 128))
            wk = self.wchunk(self.w_in, self.win_cols(l, KB_ + hp * 128))
            for tg in range(NG):
                pm = bank[0]
                self.proj_fm(wq, pm, tg)
                P.c("act", lambda e, tg=tg, pm=pm: e.mul(out=qT[:, tg * 512:(tg + 1) * 512], in_=pm[:, :], mul=0.125),
                    reads=[pm], writes=[qT])
                pm = bank[1]
                self.proj_fm(wk, pm, tg)
                P.c("dve", lambda e, tg=tg, pm=pm: e.tensor_copy(out=kT[:, tg * 512:(tg + 1) * 512], in_=pm[:, :]),
                    reads=[pm], writes=[kT])
            jobs = [(2 * hp + hh, Q) for Q in range(NG - 1, -1, -1) for hh in range(2)]
            nch = len(S["ch"])
            slots = [[] for _ in range(nch)]
            load = [0] * nch
            for jb in sorted(jobs, key=lambda jb: -jb[1]):
                si = load.index(min(load))
                slots[si].append(jb)
                load[si] += 4 * jb[1] + 4

            def slot_gen(si):
                for (h, Q) in slots[si]:
                    yield from chain(h, Q, S["ch"][si])
            rr([slot_gen(si) for si in range(nch)])
            P.dma("sp", self.oT_d[256 + hp * 128:256 + (hp + 1) * 128, :], obT[:, :], obT, reads=[obT], writes=[self.oT_d])
        P.scope_exit()

    def phase_dil(self, l):
        P, T, NT, NG = self.P, self.T, self.NT, self.NG
        hT, bank, cst = self.hT, self.bank, self.cst
        P.scope_enter()
        qn = P.sb("qn", [128, T], BF16)
        kn = P.sb("kn", [128, T], BF16)
        vp = P.sb("vp", [128, NT, 128], BF16)
        vT = P.sb("vT", [128, T], BF16)
        acc = P.sb("acc", [128, 2, T], F32)
        raw = [P.sb("raw", [128, 512], F32) for _ in range(2)]
        sq = [P.sb("sqd", [128, 512], BF16) for _ in range(2)]
        rst = [P.sb("rst", [128, 512], F32) for _ in range(2)]
        tmp = [P.sb("tmpd", [128, 512], F32) for _ in range(2)]
        pT = [P.sb("pT", [128, 512], BF16) for _ in range(3)]
        oa = P.sb("oa", [128, T], BF16)
        sm = P.sb("smalld", [128, 136], F32)
        bt = P.sb("bt", [128, 12 * 256], F32)
        P.dma("sp", bt[:], self.d_bt[:], bt, reads=[self.d_bt], writes=[bt])
        qkrow, qkg = self.qkrow, self.qkg
        P.c("dve", lambda e: e.tensor_tensor(out=sm[:, 0:128], in0=qkrow[:, l * 128:(l + 1) * 128], in1=qkrow[:, l * 128:(l + 1) * 128],
                                             op=ALU.mult), reads=[qkrow], writes=[sm])
        P.c("dve", lambda e: e.reduce_max(out=sm[:, 128:129], in_=sm[:, 0:64], axis=AX.X), reads=[sm], writes=[sm])
        P.c("dve", lambda e: e.reduce_max(out=sm[:, 129:130], in_=sm[:, 64:128], axis=AX.X), reads=[sm], writes=[sm])
        P.c("dve", lambda e: e.tensor_tensor(out=sm[:, 130:131], in0=sm[:, 128:129], in1=sm[:, 129:130], op=ALU.add),
            reads=[sm], writes=[sm])
        P.c("dve", lambda e: e.tensor_scalar(out=sm[:, 130:131], in0=sm[:, 130:131], scalar1=-4.0, scalar2=None, op0=ALU.mult),
            reads=[sm], writes=[sm])
        P.c("dve", lambda e: e.tensor_scalar(out=sm[:, 131:132], in0=qkg[:, 2 * l:2 * l + 1], scalar1=0.125, scalar2=None, op0=ALU.mult),
            reads=[qkg], writes=[sm])
        negc = lambda: sm[:, 130:131]
        g8 = lambda: sm[:, 131:132]
        gk = lambda: qkg[:, 2 * l + 1:2 * l + 2]
        cnt = [0]
        def group(s, g):
            if True:
                d = DIL[g]
                nb = T // (128 * d)
                H = 4 * g + 2 * s
                wq = self.wchunk(self.w_in, self.win_cols(l, QA + H * 64))
                wk = self.wchunk(self.w_in, self.win_cols(l, KA + H * 64))
                wvv = self.wchunk(self.w_in, self.win_cols(l, VA + H * 64))
                if self.cfg.get("dil_stop", 99) <= 0.1:
                    return
                for (w, dst, gain) in ((wq, qn, g8), (wk, kn, gk)):
                    for tg in range(NG):
                        i2 = cnt[0] % 2
                        cnt[0] += 1
                        pm, pm2 = bank[i2], bank[2 + i2]
                        rw, sqq, rs_ = raw[i2], sq[i2], rst[i2]
                        self.proj_fm(w, pm, tg)
                        P.c("dve", lambda e, pm=pm, rw=rw: e.tensor_copy(out=rw[:], in_=pm[:, :]), reads=[pm], writes=[rw])
                        P.c("act", lambda e, pm=pm, sqq=sqq: e.activation(out=sqq[:], in_=pm[:, :], func=AF.Square), reads=[pm], writes=[sqq])
                        P.c("pe", lambda e, pm2=pm2, sqq=sqq: e.matmul(pm2[:, :], lhsT=self.blockones(), rhs=sqq[:], start=True, stop=True),
                            reads=[self.cb, sqq], writes=[pm2])
                        P.c("act", lambda e, pm2=pm2, rs_=rs_: e.activation(out=rs_[:], in_=pm2[:, :], func=AF.Ln, scale=1.0 / 64, bias=cst[:, 0:1]),
                            reads=[pm2, cst], writes=[rs_])
                        P.c("act", lambda e, rs_=rs_: e.activation(out=rs_[:], in_=rs_[:], func=AF.Exp, scale=-0.5), reads=[rs_], writes=[rs_])
                        if self.cfg.get("dil_stop", 99) <= 0.5:
                            continue
                        P.c("dve", lambda e, dst=dst, tg=tg, rw=rw, rs_=rs_, gain=gain: e.scalar_tensor_tensor(
                            out=dst[:, :].rearrange("p (r i) -> p r i", r=d)[:, :, tg * (512 // d):(tg + 1) * (512 // d)],
                            in0=rw[:].rearrange("p (i r) -> p r i", r=d), scalar=gain(),
                            in1=rs_[:].rearrange("p (i r) -> p r i", r=d), op0=ALU.mult, op1=ALU.mult),
                            reads=[rw, rs_, sm, qkg], writes=[dst])
                if self.cfg.get("dil_stop", 99) <= 1:
                    return
                for tg in range(NG):
                    pm = bank[tg % 2]
                    self.proj_fm(wvv, pm, tg)
                    P.c("act", lambda e, pm=pm, tg=tg: e.copy(
                        out=vT[:, :].rearrange("p (r i) -> p r i", r=d)[:, :, tg * (512 // d):(tg + 1) * (512 // d)],
                        in_=pm[:, :].rearrange("p (i r) -> p r i", r=d)), reads=[pm], writes=[vT])
                for b in range(NT):
                    pm = bank[4 + (b // 4) % 2]
                    pmv = pm[:].bitcast(BF16)
                    P.c("pe", lambda e, pmv=pmv, b=b: e.transpose(out=pmv[:, (b % 4) * 128:(b % 4 + 1) * 128], in_=vT[:, b * 128:(b + 1) * 128],
                                                                 identity=self.ident()), reads=[vT, self.cb], writes=[pm])
                    if b % 4 == 3:
                        P.c("dve", lambda e, pmv=pmv, b=b: e.tensor_copy(out=vp[:, b - 3:b + 1, :],
                                                                        in_=pmv[:, 0:512].rearrange("p (a n) -> p a n", a=4)),
                            reads=[pm], writes=[vp])
                if self.cfg.get("dil_stop", 99) <= 2:
                    return
                def head(hh):
                    rows = slice(hh * 64, hh * 64 + 64)
                    Hh = H + hh
                    btH = lambda Hh=Hh: bt[:, Hh * 256:(Hh + 1) * 256]
                    batches = [(r, mb) for r in range(d) for mb in range(nb // 2)]

                    def stage1(bi):
                        r, mb = batches[bi]
                        sbk = bank[4 + bi % 2]
                        tm = tmp[bi % 2]
                        pt = pT[bi % 3]
                        width = 0
                        for i in range(2):
                            m = 2 * mb + i
                            nq = 256 if m < nb - 1 else 128
                            P.c("pe", lambda e, i=i, m=m, nq=nq: e.matmul(
                                sbk[:, i * 256:i * 256 + nq], lhsT=kn[rows, (r * nb + m) * 128:(r * nb + m + 1) * 128],
                                rhs=qn[rows, (r * nb + m) * 128:(r * nb + m) * 128 + nq], start=True, stop=True), reads=[kn, qn], writes=[sbk])
                            P.c("dve", lambda e, i=i, nq=nq: e.tensor_tensor(out=tm[:, i * 256:i * 256 + nq], in0=sbk[:, i * 256:i * 256 + nq],
                                                                            in1=btH()[:, 0:nq], op=ALU.add), reads=[sbk, bt], writes=[tm])
                            width = i * 256 + nq
                        P.c("act", lambda e, width=width: e.activation(out=pt[:, 0:width], in_=tm[:, 0:width], func=AF.Exp, bias=negc()),
                            reads=[tm, sm], writes=[pt])

                    def stage2(bi):
                        r, mb = batches[bi]
                        ub = bank[6 + bi % 2]
                        pt = pT[bi % 3]
                        ptp = pT[(bi - 1) % 3]
                        m0, m1 = 2 * mb, 2 * mb + 1
                        blk = lambda m: r * nb + m
                        contribs = [[], []]
                        if m0 > 0:
                            contribs[0].append((blk(m0 - 1), ptp, 384))
                        contribs[0].append((blk(m0), pt, 0))
                        contribs[1].append((blk(m0), pt, 128))
                        contribs[1].append((blk(m1), pt, 256))
                        for ni in range(2):
                            for ud in range(2):
                                n = len(contribs[ni])
                                for ci, (kb, ptile, c0) in enumerate(contribs[ni]):
                                    lhs = (lambda kb=kb: vp[:, kb, :]) if ud == 0 else self.ones
                                    P.c("pe", lambda e, lhs=lhs, ptile=ptile, c0=c0, ni=ni, ud=ud, ci=ci, n=n: e.matmul(
                                        ub[:, ud * 256 + ni * 128:ud * 256 + (ni + 1) * 128], lhsT=lhs(), rhs=ptile[:, c0:c0 + 128],
                                        start=(ci == 0), stop=(ci == n - 1)), reads=[vp, self.cb, ptile], writes=[ub])
                        ov = lambda: acc[rows, :, sl(m0 * 128 * d + r, 256, d)]
                        iv = lambda: ub[rows, :].rearrange("p (a n) -> p a n", a=2)
                        if g == 0:
                            P.c("dve", lambda e: e.tensor_copy(out=ov(), in_=iv()), reads=[ub], writes=[acc])
                        else:
                            P.c("dve", lambda e: e.tensor_tensor(out=ov(), in0=iv(), in1=ov(), op=ALU.add), reads=[ub, acc], writes=[acc])

                    for bi in range(len(batches) + 1):
                        if bi < len(batches):
                            stage1(bi)
                        if bi >= 1:
                            stage2(bi - 1)
                for hh in range(2):
                    head(hh)

        for s in range(2):
            for g in range(3):
                group(s, g)
            if self.cfg.get("dil_stop", 99) <= 3:
                continue
            P.c("dve", lambda e: e.reciprocal(out=acc[:, 1, :], in_=acc[:, 1, :]), reads=[acc], writes=[acc])
            P.c("dve", lambda e: e.tensor_tensor(out=oa[:, :], in0=acc[:, 0, :], in1=acc[:, 1, :], op=ALU.mult), reads=[acc], writes=[oa])
            P.dma("sp", self.oT_d[s * 128:(s + 1) * 128, :], oa[:, :], oa, reads=[oa], writes=[self.oT_d])
        P.scope_exit()

    def phase_conv(self, l):
        P, T, NT, NG = self.P, self.T, self.NT, self.NG
        hT, bank, cst = self.hT, self.bank, self.cst
        P.scope_enter()
        uT = P.sb("uT", [128, 4, 32 + T], BF16)
        dg = P.sb("dg", [128, 4, 31, 128], BF16)
        idf = P.sb("idf", [128, 128], F32)
        cv = [P.sb("cv", [128, 512], F32) for _ in range(4)]
        xc = [P.sb("xc", [128, 512], F32) for _ in range(4)]
        sqc = [P.sb("sqc", [128, 512], F32) for _ in range(2)]
        sig = [P.sb("sig", [128, 512], F32) for _ in range(2)]
        rsd = P.sb("rsd", [128, 512], F32)
        ost = [P.sb("ost", [128, 4, 512], BF16) for _ in range(2)]
        onesf = P.sb("onesf", [128, 128], F32)
        convw, convp = self.convw, self.convp
        P.c("pool", lambda e: e.memset(onesf[:], 1.0), writes=[onesf])
        P.c("dve", lambda e: e.tensor_copy(out=idf[:], in_=self.ident()), reads=[self.cb], writes=[idf])
        P.c("pool", lambda e: e.memset(uT[:, :, 0:32], 0.0), writes=[uT])
        for cc in range(4):
            for k in range(31):
                col = (l * 4 + cc) * 31 + k
                P.c("dve", lambda e, cc=cc, k=k, col=col: e.tensor_scalar(out=dg[:, cc, k, :], in0=idf[:], scalar1=convw[:, col:col + 1],
                                                                        scalar2=None, op0=ALU.mult), reads=[idf, convw], writes=[dg])
        for cc in range(4):
            wv_ = self.wchunk(self.w_in, self.win_cols(l, GV + cc * 128))
            wg_ = self.wchunk(self.w_in, self.win_cols(l, GG + cc * 128))
            for tg in range(NG):
                pmv, pmg = bank[tg % 2], bank[2 + tg % 2]
                sg = sig[tg % 2]
                self.proj_fm(wg_, pmg, tg)
                self.proj_fm(wv_, pmv, tg)
                P.c("act", lambda e, pmg=pmg, sg=sg: e.activation(out=sg[:], in_=pmg[:, :], func=AF.Sigmoid), reads=[pmg], writes=[sg])
                P.c("dve", lambda e, pmv=pmv, sg=sg, cc=cc, tg=tg: e.tensor_tensor(out=uT[:, cc, 32 + tg * 512:32 + (tg + 1) * 512], in0=pmv[:, :],
                                                                                 in1=sg[:], op=ALU.mult), reads=[pmv, sg], writes=[uT])
        pcol = lambda which, cc: convp[:, (l * 3 + which) * 4 + cc:(l * 3 + which) * 4 + cc + 1]
        def conv_tile(tg):
            for cc in range(4):
                pm = bank[4 + cc % 2]
                for k in range(31):
                    P.c("pe", lambda e, cc=cc, k=k, pm=pm: e.matmul(pm[:, :], lhsT=dg[:, cc, k, :],
                                                                   rhs=uT[:, cc, 2 + tg * 512 + k:2 + tg * 512 + k + 512],
                                                                   start=(k == 0), stop=(k == 30)), reads=[dg, uT], writes=[pm])
                P.c("act", lambda e, cc=cc, pm=pm: e.activation(out=cv[cc][:], in_=pm[:, :], func=AF.Identity, bias=pcol(0, cc)),
                    reads=[pm, convp], writes=[cv[cc]])
            pmm = bank[6]
            for cc in range(4):
                P.c("pe", lambda e, cc=cc: e.matmul(pmm[:, :], lhsT=onesf[:], rhs=cv[cc][:], start=(cc == 0), stop=(cc == 3)),
                    reads=[onesf, cv[cc]], writes=[pmm])
            for cc in range(4):
                P.c("dve", lambda e, cc=cc: e.scalar_tensor_tensor(out=xc[cc][:], in0=pmm[:, :], scalar=-1.0 / 512, in1=cv[cc][:],
                                                                   op0=ALU.mult, op1=ALU.add), reads=[pmm, cv[cc]], writes=[xc[cc]])
            pmv = bank[7]
            for cc in range(4):
                sq_ = sqc[cc % 2]
                P.c("act", lambda e, cc=cc, sq_=sq_: e.activation(out=sq_[:], in_=xc[cc][:], func=AF.Square), reads=[xc[cc]], writes=[sq_])
                P.c("pe", lambda e, cc=cc, sq_=sq_: e.matmul(pmv[:, :], lhsT=onesf[:], rhs=sq_[:], start=(cc == 0), stop=(cc == 3)),
                    reads=[onesf, sq_], writes=[pmv])
            P.c("act", lambda e: e.activation(out=rsd[:], in_=pmv[:, :], func=AF.Ln, scale=1.0 / 512, bias=cst[:, 0:1]),
                reads=[pmv, cst], writes=[rsd])
            P.c("act", lambda e: e.activation(out=rsd[:], in_=rsd[:], func=AF.Exp, scale=-0.5), reads=[rsd], writes=[rsd])
            os_ = ost[tg % 2]
            for cc in range(4):
                P.c("dve", lambda e, cc=cc: e.tensor_tensor(out=xc[cc][:], in0=xc[cc][:], in1=rsd[:], op=ALU.mult),
                    reads=[xc[cc], rsd], writes=[xc[cc]])
                P.c("dve", lambda e, cc=cc: e.tensor_scalar(out=xc[cc][:], in0=xc[cc][:], scalar1=pcol(1, cc), scalar2=pcol(2, cc),
                                                            op0=ALU.mult, op1=ALU.add), reads=[xc[cc], convp], writes=[xc[cc]])
                P.c("act", lambda e, cc=cc, os_=os_: e.activation(out=os_[:, cc, :], in_=xc[cc][:], func=AF.Silu), reads=[xc[cc]], writes=[os_])
            P.dma("sp", self.oT_d.h[768:1280, tg * 512:(tg + 1) * 512].rearrange("(c p) t -> p c t", p=128), os_[:], os_,
                  reads=[os_], writes=[self.oT_d])

        for tg in range(NG):
            conv_tile(tg)
        P.scope_exit()

    def phase_merge(self, l, x_d, x1_d):
        P, T, NT, NG = self.P, self.T, self.NT, self.NG
        hT, bank = self.hT, self.bank
        P.scope_enter()
        oTt = [P.sb("oTt", [128, 10, 512], BF16) for _ in range(2)]
        sg = [P.sb("sg", [128, 512], F32) for _ in range(3)]
        t1 = [P.sb("t1", [128, 512], F32) for _ in range(3)]
        mst = [P.sb("mst", [128, 512], BF16) for _ in range(2)]
        wbr = [P.sb("wbr", [128, 10, 128], BF16) for _ in range(2)]
        mT_d = self.mT_d
        wsrc = ((self.w_ba, 0, 2), (self.w_bb, 2, 4), (self.w_bc, 6, 4))

        def one(dmc, tg, wb_, wg):
            ot = oTt[tg % 2]
            P.dma("sp", ot[:], self.oT_d.h[:, tg * 512:(tg + 1) * 512].rearrange("(c p) t -> p c t", p=128), ot,
                  reads=[self.oT_d], writes=[ot])
            for b in range(3):
                self.proj_fm(wg[b], bank[b], tg)
                P.c("act", lambda e, b=b: e.activation(out=sg[b][:], in_=bank[b][:, :], func=AF.Sigmoid), reads=[bank[b]], writes=[sg[b]])
            for b, (wt, c0, nchunk) in enumerate(wsrc):
                pm = bank[3 + b]
                for ci in range(nchunk):
                    P.c("pe", lambda e, pm=pm, c0=c0, ci=ci, nchunk=nchunk: e.matmul(pm[:, :], lhsT=wb_[:, c0 + ci, :], rhs=ot[:, c0 + ci, :],
                                                                                   start=(ci == 0), stop=(ci == nchunk - 1)),
                        reads=[wb_, ot], writes=[pm])
                P.c("dve", lambda e, pm=pm, b=b: e.tensor_tensor(out=t1[b][:], in0=pm[:, :], in1=sg[b][:], op=ALU.mult),
                    reads=[pm, sg[b]], writes=[t1[b]])
            P.c("pool", lambda e: e.tensor_tensor(out=t1[0][:], in0=t1[0][:], in1=t1[1][:], op=ALU.add), reads=[t1[0], t1[1]], writes=[t1[0]])
            ms = mst[tg % 2]
            P.c("dve", lambda e, ms=ms: e.tensor_tensor(out=ms[:], in0=t1[0][:], in1=t1[2][:], op=ALU.add), reads=[t1[0], t1[2]], writes=[ms])
            P.dma("act", mT_d[dmc * 128:(dmc + 1) * 128, tg * 512:(tg + 1) * 512], ms[:], ms, reads=[ms], writes=[mT_d])

        for dmc in range(8):
            wb_ = wbr[dmc % 2]
            for (wt, c0, nchunk) in wsrc:
                P.dma("pool", wb_[:, c0:c0 + nchunk, :], wt.h[l].rearrange("(c p) n -> p c n", p=128)[:, :, dmc * 128:(dmc + 1) * 128], wb_,
                      reads=[wt], writes=[wb_])
            wg = [self.wchunk(self.w_in, self.win_cols(l, GATE0 + b * 1024 + dmc * 128)) for b in range(3)]
            for tg in range(NG):
                one(dmc, tg, wb_, wg)
        P.scope_exit()
        P.scope_enter()
        wo = P.sb("wo", [128, 8, 1024], BF16)
        mt = [P.sb("mt", [128, 8, 512], BF16) for _ in range(2)]
        xt = [P.sb("xt2", [128, D], F32) for _ in range(3)]
        for c in range(8):
            P.dma("pool", wo[:, c, :], self.w_out.h[l][c * 128:(c + 1) * 128, :], wo, reads=[self.w_out], writes=[wo])

        def tile(tg, j, mtt):
            i = tg * 4 + j
            x_ = xt[i % 3]
            P.dma("sp", x_[:], x_d[i * 128:(i + 1) * 128, :], x_, reads=[x_d], writes=[x_])
            for half in range(2):
                pm = bank[(2 * i + half) % 4]
                for c in range(8):
                    P.c("pe", lambda e, pm=pm, c=c, half=half: e.matmul(pm[:, :], lhsT=mtt[:, c, j * 128:(j + 1) * 128],
                                                                       rhs=wo[:, c, half * 512:(half + 1) * 512], start=(c == 0), stop=(c == 7)),
                        reads=[mtt, wo], writes=[pm])
                P.c("dve", lambda e, pm=pm, half=half: e.tensor_tensor(out=x_[:, half * 512:(half + 1) * 512], in0=pm[:, :],
                                                                      in1=x_[:, half * 512:(half + 1) * 512], op=ALU.add),
                    reads=[pm, x_], writes=[x_])
            P.dma("act", x1_d[i * 128:(i + 1) * 128, :], x_[:], x_, reads=[x_], writes=[x1_d])

        for tg in range(NG):
            mtt = mt[tg % 2]
            P.dma("sp", mtt[:], mT_d.h[:, tg * 512:(tg + 1) * 512].rearrange("(c p) t -> p c t", p=128), mtt, reads=[mT_d], writes=[mtt])
            for j in range(4):
                tile(tg, j, mtt)
        P.scope_exit()

    def ffn_core(self, xT, xoff, R, wg_src, wu_src, wd_src, sink):
        P, bank = self.P, self.bank
        nf = self.dff // 128
        hid = P.sb("hid", [128, nf, R], BF16)
        wd = [P.sb("wd", [128, nf, 512], BF16) for _ in range(2)]
        sil = [P.sb("sil", [128, 512], F32) for _ in range(2)]
        for half in range(2):
            for f0 in range(0, nf, 11):
                f1 = min(nf, f0 + 11)
                P.dma("pool", wd[half][:, f0:f1, :], wd_src[1][f0 * 128:f1 * 128, half * 512:(half + 1) * 512].rearrange("(f p) n -> p f n", p=128),
                      wd[half], reads=[wd_src[0]], writes=[wd[half]])
        segs = [(r0, min(512, R - r0)) for r0 in range(0, R, 512)]
        cnt = 0
        for f in range(nf):
            wg = self.wchunk(wg_src[0], wg_src[1].rearrange("(c p) n -> p c n", p=128)[:, :, f * 128:(f + 1) * 128])
            wu = self.wchunk(wu_src[0], wu_src[1].rearrange("(c p) n -> p c n", p=128)[:, :, f * 128:(f + 1) * 128])
            for (r0, w) in segs:
                pa, pu = bank[cnt % 2], bank[2 + cnt % 2]
                sl_ = sil[cnt % 2]
                cnt += 1
                for (wt, pm) in ((wg, pa), (wu, pu)):
                    for c in range(8):
                        P.c("pe", lambda e, wt=wt, pm=pm, c=c, r0=r0, w=w: e.matmul(pm[:, 0:w], lhsT=wt[:, c, :], rhs=xT[:, c, xoff + r0:xoff + r0 + w],
                                                                                   start=(c == 0), stop=(c == 7)), reads=[wt, xT], writes=[pm])
                P.c("act", lambda e, pa=pa, sl_=sl_, w=w: e.activation(out=sl_[:, 0:w], in_=pa[:, 0:w], func=AF.Silu), reads=[pa], writes=[sl_])
                P.c("dve", lambda e, pu=pu, sl_=sl_, w=w, r0=r0, f=f: e.tensor_tensor(out=hid[:, f, r0:r0 + w], in0=pu[:, 0:w], in1=sl_[:, 0:w],
                                                                                     op=ALU.mult), reads=[pu, sl_], writes=[hid])
        for j in range(R // 128):
            for half in range(2):
                pm = bank[4 + (2 * j + half) % 4]
                for f in range(nf):
                    P.c("pe", lambda e, pm=pm, f=f, j=j, half=half: e.matmul(pm[:, :], lhsT=hid[:, f, j * 128:(j + 1) * 128], rhs=wd[half][:, f, :],
                                                                            start=(f == 0), stop=(f == nf - 1)), reads=[hid, wd[half]], writes=[pm])
                sink(j, half, pm)

    def phase_ffn_dense(self, x1_d, xo_d):
        P, T = self.P, self.T
        RG = min(1024, T)
        for rg in range(T // RG):
            P.scope_enter()
            xt = [P.sb("xt3", [128, D], F32) for _ in range(3)]

            def sink(j, half, pm, rg=rg, xt=xt):
                i = rg * (RG // 128) + j
                x_ = xt[i % 3]
                if half == 0:
                    P.dma("sp", x_[:], x1_d[i * 128:(i + 1) * 128, :], x_, reads=[x1_d], writes=[x_])
                P.c("dve", lambda e: e.tensor_tensor(out=x_[:, half * 512:(half + 1) * 512], in0=pm[:, :],
                                                     in1=x_[:, half * 512:(half + 1) * 512], op=ALU.add), reads=[pm, x_], writes=[x_])
                if half == 1:
                    P.dma("act", xo_d[i * 128:(i + 1) * 128, :], x_[:], x_, reads=[x_], writes=[xo_d])

            self.ffn_core(self.hT, rg * RG, RG, (self.w_fg, self.w_fg.h[0]), (self.w_fu, self.w_fu.h[0]), (self.w_fd, self.w_fd.h[0]), sink)
            P.scope_exit()

    def phase_moe(self, l, x1_d, xo_d):
        P, T, NT, bank, cst = self.P, self.T, self.NT, self.bank, self.cst
        CAP = self.cfg.get("cap", 1536)
        XS_d = P.dram("XS_d", [NEXP * CAP, D], BF16)
        YS_d = P.dram("YS_d", [NEXP * CAP, D], F32)
        gsm = P.sb("gsm", [128, NT, 2], F32)
        idxs = P.sb("idxs", [128, NT, 2], I32)
        P.scope_enter()
        grow = P.sb("grow", [128, D], F32)
        wrg = P.sb("wrg", [128, NEXP, D], F32)
        brt = P.sb("brt", [128, NEXP], F32)
        ebase = P.sb("ebase", [128, NEXP], F32)
        zt = P.sb("zt", [128, 2, D], BF16)
        xt = [P.sb("xtm", [128, D], F32) for _ in range(2)]
        junk = P.sb("junk", [128, D], F32)
        junk2 = P.sb("junk2", [128, D], F32)
        junk3 = P.sb("junk3", [128, D], F32)
        h2b = [P.sb("h2b", [128, D], BF16) for _ in range(2)]
        sm = [P.sb("smm", [128, 96], F32) for _ in range(2)]
        selb = [P.sb("selb", [128, NEXP], BF16) for _ in range(2)]
        sm2 = [P.sb("smm2", [128, 2], F32) for _ in range(2)]
        selcum = [P.sb("selcum", [128, NEXP], BF16) for _ in range(2)]
        P.dma("sp", grow[:], self.d_grow[:], grow, reads=[self.d_grow], writes=[grow])
        P.dma("sp", wrg[:], self.d_wr.h.rearrange("p (e d) -> p e d", e=NEXP), wrg, reads=[self.d_wr], writes=[wrg])
        P.dma("sp", brt[:], self.d_br[:], brt, reads=[self.d_br], writes=[brt])
        P.dma("sp", ebase[:], self.d_ebase[:], ebase, reads=[self.d_ebase], writes=[ebase])
        for e_ in range(NEXP):
            P.c("pool", lambda e, e_=e_: e.tensor_tensor(out=wrg[:, e_, :], in0=wrg[:, e_, :], in1=grow[:], op=ALU.mult),
                reads=[wrg, grow], writes=[wrg])
        P.c("pool", lambda e: e.memset(zt[:], 0.0), writes=[zt])
        P.c("pool", lambda e: e.memset(selcum[1][:], 0.0), writes=[selcum[1]])
        for a in range(NEXP * CAP // 256):
            P.dma("sp", XS_d.h[a * 256:(a + 1) * 256, :].rearrange("(a p) n -> p a n", p=128), zt[:], zt, reads=[zt], writes=[XS_d])

        def route(i):
            x_, hb_, s_, sb_ = xt[i % 2], h2b[i % 2], sm[i % 2], selb[i % 2]
            s2_ = sm2[i % 2]
            sc_new, sc_old = selcum[i % 2], selcum[(i + 1) % 2]
            col = lambda a, b=None: s_[:, a:(a + 1 if b is None else b)]
            P.dma("sp", x_[:], x1_d[i * 128:(i + 1) * 128, :], x_, reads=[x1_d], writes=[x_])
            P.c("act", lambda e: e.activation(out=junk[:], in_=x_[:], func=AF.Square, accum_out=col(0)), reads=[x_], writes=[junk, s_])
            P.c("act", lambda e: e.activation(out=col(1), in_=col(0), func=AF.Ln, scale=1.0 / D, bias=cst[:, 0:1]), reads=[s_, cst], writes=[s_])
            P.c("act", lambda e: e.activation(out=col(1), in_=col(1), func=AF.Exp, scale=-0.5), reads=[s_], writes=[s_])
            P.c("dve", lambda e: e.scalar_tensor_tensor(out=hb_[:], in0=x_[:], scalar=col(1), in1=grow[:], op0=ALU.mult, op1=ALU.mult),
                reads=[x_, s_, grow], writes=[hb_])
            for e_ in range(NEXP):
                if e_ % 3 == 2:
                    P.c("pool", lambda e, e_=e_: e.tensor_tensor(out=junk2[:], in0=x_[:], in1=wrg[:, e_, :], op=ALU.mult),
                        reads=[x_, wrg], writes=[junk2])
                    P.c("act", lambda e, e_=e_: e.activation(out=junk3[:], in_=junk2[:], func=AF.Identity, accum_out=s2_[:, e_ // 3:e_ // 3 + 1]),
                        reads=[junk2], writes=[junk3, s2_])
                else:
                    P.c("dve", lambda e, e_=e_: e.scalar_tensor_tensor(out=junk[:], in0=x_[:], scalar=col(1), in1=wrg[:, e_, :], op0=ALU.mult,
                                                                       op1=ALU.mult, accum_out=col(8 + e_)), reads=[x_, s_, wrg], writes=[junk, s_])
            P.c("dve", lambda e: e.tensor_scalar(out=s_[:, 10:14:3], in0=s2_[:, 0:2], scalar1=col(1), scalar2=None, op0=ALU.mult),
                reads=[s2_, s_], writes=[s_])
            if True:
                pass
            lg, eq1, lg2, eq2, dest, tmp8 = col(8, 16), col(16, 24), col(24, 32), col(32, 40), col(40, 48), col(48, 56)
            P.c("dve", lambda e: e.tensor_tensor(out=lg, in0=lg, in1=brt[:], op=ALU.add), reads=[s_, brt], writes=[s_])
            P.c("dve", lambda e: e.reduce_max(out=col(2), in_=lg, axis=AX.X), reads=[s_], writes=[s_])
            P.c("dve", lambda e: e.tensor_scalar(out=eq1, in0=lg, scalar1=col(2), scalar2=None, op0=ALU.is_equal), reads=[s_], writes=[s_])
            P.c("dve", lambda e: e.scalar_tensor_tensor(out=lg2, in0=eq1, scalar=-1e30, in1=lg, op0=ALU.mult, op1=ALU.add), reads=[s_], writes=[s_])
            P.c("dve", lambda e: e.reduce_max(out=col(3), in_=lg2, axis=AX.X), reads=[s_], writes=[s_])
            P.c("dve", lambda e: e.tensor_scalar(out=eq2, in0=lg2, scalar1=col(3), scalar2=None, op0=ALU.is_equal), reads=[s_], writes=[s_])
            P.c("dve", lambda e: e.tensor_scalar(out=col(4), in0=col(2), scalar1=-1.0, scalar2=None, op0=ALU.mult), reads=[s_], writes=[s_])
            P.c("act", lambda e: e.activation(out=col(5), in_=col(3), func=AF.Exp, bias=col(4)), reads=[s_], writes=[s_])
            P.c("dve", lambda e: e.tensor_scalar(out=col(6), in0=col(5), scalar1=1.0, scalar2=None, op0=ALU.add), reads=[s_], writes=[s_])
            P.c("dve", lambda e: e.reciprocal(out=col(6), in_=col(6)), reads=[s_], writes=[s_])
            P.c("dve", lambda e: e.tensor_copy(out=gsm[:, i, 0:1], in_=col(6)), reads=[s_], writes=[gsm])
            P.c("dve", lambda e: e.tensor_tensor(out=gsm[:, i, 1:2], in0=col(5), in1=col(6), op=ALU.mult), reads=[s_], writes=[gsm])
            P.c("dve", lambda e: e.tensor_tensor(out=sb_[:], in0=eq1, in1=eq2, op=ALU.add), reads=[s_], writes=[sb_])
            pc = bank[i % 2]
            P.c("pe", lambda e: e.matmul(pc[:, 0:NEXP], lhsT=self.ones(), rhs=sb_[:], start=True, stop=False), reads=[self.cb, sb_], writes=[pc])
            P.c("pe", lambda e: e.matmul(pc[:, 0:NEXP], lhsT=self.tri(), rhs=sb_[:], start=False, stop=False), reads=[self.cb, sb_], writes=[pc])
            P.c("pe", lambda e: e.matmul(pc[:, 0:NEXP], lhsT=self.ones(), rhs=sc_old[:], start=False, stop=True), reads=[self.cb, sc_old], writes=[pc])
            P.c("dve", lambda e: e.tensor_tensor(out=sc_new[:], in0=sc_old[:], in1=sb_[:], op=ALU.add), reads=[sc_old, sb_], writes=[sc_new])
            P.c("dve", lambda e: e.scalar_tensor_tensor(out=dest, in0=pc[:, 0:NEXP], scalar=float(CAP - 1), in1=ebase[:], op0=ALU.min, op1=ALU.add),
                reads=[pc, ebase], writes=[s_])
            for kk, eq in enumerate((eq1, eq2)):
                P.c("dve", lambda e, eq=eq: e.tensor_tensor(out=tmp8, in0=eq, in1=dest, op=ALU.mult), reads=[s_], writes=[s_])
                P.c("dve", lambda e, kk=kk: e.reduce_sum(out=col(56 + kk), in_=tmp8, axis=AX.X), reads=[s_], writes=[s_])
                P.c("dve", lambda e, kk=kk: e.tensor_copy(out=idxs[:, i, kk:kk + 1], in_=col(56 + kk)), reads=[s_], writes=[idxs])
            for kk in range(2):
                P.idma(XS_d[:, :], bass.IndirectOffsetOnAxis(ap=idxs[:, i, kk:kk + 1], axis=0), hb_[:], None, hb_,
                       reads=[hb_, idxs], writes=[XS_d])

        for i in range(NT):
            route(i)
        P.scope_exit()
        for ex in range(NEXP):
            self.moe_expert(ex, CAP, XS_d, YS_d)
        P.scope_enter()
        xt = [P.sb("xtc", [128, D], F32) for _ in range(2)]
        yg = [[P.sb("yg", [128, D], F32) for _ in range(2)] for _ in range(2)]

        def comb(i):
            x_ = xt[i % 2]
            P.dma("sp", x_[:], x1_d[i * 128:(i + 1) * 128, :], x_, reads=[x1_d], writes=[x_])
            for kk in range(2):
                y_ = yg[kk][i % 2]
                P.idma(y_[:], None, YS_d[:, :], bass.IndirectOffsetOnAxis(ap=idxs[:, i, kk:kk + 1], axis=0), y_,
                       reads=[YS_d, idxs], writes=[y_])
                P.c("dve", lambda e, y_=y_, kk=kk: e.scalar_tensor_tensor(out=x_[:], in0=y_[:], scalar=gsm[:, i, kk:kk + 1], in1=x_[:],
                                                                          op0=ALU.mult, op1=ALU.add), reads=[y_, gsm, x_], writes=[x_])
            P.dma("act", xo_d[i * 128:(i + 1) * 128, :], x_[:], x_, reads=[x_], writes=[xo_d])

        for i in range(NT):
            comb(i)
        P.scope_exit()

    def moe_expert(self, ex, CAP, XS_d, YS_d):
        P, bank = self.P, self.bank
        P.scope_enter()
        xsT = P.sb("xsT", [128, 8, CAP], BF16)
        xr = [P.sb("xr", [128, D], BF16) for _ in range(2)]
        ys = [P.sb("ys", [128, D], F32) for _ in range(2)]
        for j in range(CAP // 128):
            x_ = xr[j % 2]
            r0 = ex * CAP + j * 128
            P.dma("sp", x_[:], XS_d[r0:r0 + 128, :], x_, reads=[XS_d], writes=[x_])
            pt = bank[6 + j % 2]
            ptv = pt[:].bitcast(BF16)
            for c in range(8):
                P.c("pe", lambda e, c=c, x_=x_, ptv=ptv: e.transpose(out=ptv[:, c * 128:(c + 1) * 128], in_=x_[:, c * 128:(c + 1) * 128],
                                                                   identity=self.ident()), reads=[x_, self.cb], writes=[pt])
            eng = "dve" if j % 2 == 0 else "act"
            if eng == "dve":
                P.c("dve", lambda e, j=j, ptv=ptv: e.tensor_copy(out=xsT[:, :, j * 128:(j + 1) * 128], in_=ptv[:, :].rearrange("p (c n) -> p c n", c=8)),
                    reads=[pt], writes=[xsT])
            else:
                P.c("act", lambda e, j=j, ptv=ptv: e.copy(out=xsT[:, :, j * 128:(j + 1) * 128], in_=ptv[:, :].rearrange("p (c n) -> p c n", c=8)),
                    reads=[pt], writes=[xsT])

        def sink(j, half, pm):
            y_ = ys[j % 2]
            if half == 0:
                P.c("act", lambda e: e.copy(out=y_[:, 0:512], in_=pm[:, :]), reads=[pm], writes=[y_])
            else:
                P.c("dve", lambda e: e.tensor_copy(out=y_[:, 512:1024], in_=pm[:, :]), reads=[pm], writes=[y_])
                r0 = ex * CAP + j * 128
                P.dma("act", YS_d[r0:r0 + 128, :], y_[:], y_, reads=[y_], writes=[YS_d])

        self.ffn_core(xsT, 0, CAP, (self.w_eg, self.w_eg.h[0][ex]), (self.w_eu, self.w_eu.h[0][ex]), (self.w_ed, self.w_ed.h[0][ex]), sink)
        P.scope_exit()


def _consts():
    i = np.arange(128)
    ident = np.eye(128, dtype=np.float32)
    tri = -(i[:, None] >= i[None, :]).astype(np.float32)
    negones = -np.ones((128, 128), np.float32)
    ones = np.ones((128, 128), np.float32)
    blockones = ((i[:, None] // 64) == (i[None, :] // 64)).astype(np.float32)
    zeros = np.zeros((128, 128), np.float32)
    sbmask = np.where(i[:, None] >= i[None, :], NEG, 0.0).astype(np.float32)
    cb = np.concatenate([ident, tri, negones, ones, blockones, zeros, sbmask], axis=1).astype(ml_dtypes.bfloat16)
    slopes = 2.0 ** (-8.0 * np.arange(1, 13, dtype=np.float32) / 12.0)
    bt = np.zeros((128, 12, 2, 128), np.float32)
    for H in range(12):
        d = DIL[H // 4]
        for half in range(2):
            dist = (half * 128 + i[None, :] - i[:, None]).astype(np.float32)
            valid = (dist >= 0) & (dist <= 128)
            bt[:, H, half, :] = np.where(valid, -slopes[H] * dist * d, NEG)
    return cb, bt.reshape(128, 12 * 256).astype(np.float32)


def host_inputs(inp):
    L = DEPTH
    f = lambda a: np.ascontiguousarray(np.asarray(a, dtype=np.float32))
    g = np.stack([f(inp["attn_norm_g"]), f(inp["ffn_norm_g"])], axis=1)
    gcols = g.reshape(L, 2, 8, 128).transpose(3, 0, 1, 2).reshape(128, L * 2 * 8)
    qk = np.stack([f(inp["q_norm_g"]), f(inp["k_norm_g"])], axis=1)
    qkg = np.concatenate([qk, qk], axis=2).transpose(2, 0, 1).reshape(128, L * 2)
    qkrow = np.broadcast_to(qk.reshape(1, L * 2 * 64), (128, L * 2 * 64))
    cw = f(inp["conv_w"])
    convw = cw.reshape(L, 31, 4, 128).transpose(3, 0, 2, 1).reshape(128, L * 4 * 31)
    cp = np.stack([f(inp["conv_b"]), f(inp["conv_norm_g"]), f(inp["conv_norm_b"])], axis=1)
    convp = cp.reshape(L, 3, 4, 128).transpose(3, 0, 1, 2).reshape(128, L * 3 * 4)
    wr = f(inp["w_router"])[0]
    wr_bc = np.broadcast_to(wr.T.reshape(1, NEXP * D), (128, NEXP * D))
    br_bc = np.broadcast_to(f(inp["b_router"])[0].reshape(1, NEXP), (128, NEXP))
    cb, bt = _consts()
    cap = 1536
    grow = np.broadcast_to(f(inp["ffn_norm_g"])[1].reshape(1, D), (128, D))
    ebase = np.broadcast_to((np.arange(NEXP, dtype=np.float32) * cap).reshape(1, NEXP), (128, NEXP))
    m = dict(grow=grow, ebase=ebase, gcols=gcols, qkg=qkg, qkrow=qkrow, convw=convw, convp=convp, wr_bc=wr_bc, br_bc=br_bc, cbf=cb, btab=bt)
    m = {k: np.ascontiguousarray(v) for k, v in m.items()}
    for k in ("w_in", "w_branch_a", "w_branch_b", "w_branch_c", "w_out", "w_ffn_gate", "w_ffn_up", "w_ffn_down",
              "w_exp_gate", "w_exp_up", "w_exp_down"):
        m[k] = f(inp[k])
    return m


_CACHE = {}


def kernel(**inputs):
    m = host_inputs(inputs)
    x = np.asarray(inputs["x"], dtype=np.float32)
    nb = x.shape[0]
    if "k" not in _CACHE:
        kk = K()
        kk.build_full()
        _CACHE["k"] = kk
    kk = _CACHE["k"]
    in_maps = []
    for b in range(nb):
        mm = dict(m)
        mm["x"] = np.ascontiguousarray(x[b])
        in_maps.append(mm)
    res = run_bass_kernel_spmd(kk.nc, in_maps, core_ids=list(range(nb)))
    return np.stack([np.asarray(r["out"], dtype=np.float32) for r in res.results], axis=0)
```
